# Optimizing a Trainium2 kernel written in Bass

```python
import math
import jax, jax.numpy as jnp
from jax import lax
import numpy as np

D_MODEL = 1024
BATCH = 2
SEQ = 8192
DEPTH = 2

GRID_W = 64
CTX_LEN = 256
HEAD_DIM = 64
NA_HEADS = 4
NA_WIDTH = NA_HEADS * HEAD_DIM
WIN_H = 8
WIN_W = 16
NA_QB = WIN_W
NA_KBW = 2 * WIN_W
DIFF_HEADS = 4
DIFF_QK_WIDTH = 2 * DIFF_HEADS * HEAD_DIM
DIFF_WIDTH = DIFF_HEADS * 2 * HEAD_DIM
CONV_WIDTH = 256
CONV_K = 3
MIX_WIDTH = NA_WIDTH + DIFF_WIDTH + CONV_WIDTH
OFF_K = NA_WIDTH + DIFF_QK_WIDTH
OFF_V = OFF_K + NA_WIDTH + DIFF_QK_WIDTH
OFF_CONV = OFF_V + NA_WIDTH + DIFF_WIDTH
IN_COLS = OFF_CONV + 3 * CONV_WIDTH
SPLIT_IDX = (NA_WIDTH, OFF_K, OFF_K + NA_WIDTH, OFF_V, OFF_V + NA_WIDTH, OFF_CONV, OFF_CONV + CONV_WIDTH, OFF_CONV + 2 * CONV_WIDTH)
KV_SPLIT_IDX = (NA_WIDTH, NA_WIDTH + DIFF_QK_WIDTH, 2 * NA_WIDTH + DIFF_QK_WIDTH)
Q_BLOCK = 128
ROPE_BASE = 10000.0
D_FF = 2816
N_EXPERTS = 8
TOP_K = 2
D_EXPERT = 3584
N_DENSE = (DEPTH + 1) // 2
N_MOE = DEPTH // 2
EPS = 1e-6
NEG_INF = -1e30

kernel_name = 'hybrid_na_diffattn_shortconv_moe_dit'


def rms_norm(x, gain=None):
    xf = x.astype(jnp.float32)
    y = xf * lax.rsqrt(jnp.mean(jnp.square(xf), axis=-1, keepdims=True) + EPS)
    if gain is not None:
        y = y * gain.astype(jnp.float32)
    return y.astype(x.dtype)


def modulate(h, shift, scale):
    return h * (1 + scale) + shift


def axial_rope(n, dtype):
    t = jnp.arange(n, dtype=jnp.int32)
    row = (t // GRID_W).astype(jnp.float32)
    col = (t % GRID_W).astype(jnp.float32)
    n_freq = HEAD_DIM // 4
    inv_freq = ROPE_BASE ** (-jnp.arange(n_freq, dtype=jnp.float32) / n_freq)
    ang = jnp.concatenate([row[:, None] * inv_freq, col[:, None] * inv_freq], axis=-1)
    return jnp.cos(ang).astype(dtype), jnp.sin(ang).astype(dtype)


def apply_rope(x, cos, sin):
    half = HEAD_DIM // 2
    x1, x2 = x[..., :half], x[..., half:]
    c = cos[None, :, None, :]
    s = sin[None, :, None, :]
    return jnp.concatenate([x1 * c - x2 * s, x2 * c + x1 * s], axis=-1)


def ctx_softmax_attention(q, k, v):
    s = jnp.einsum('bqhd,bkhd->bhqk', q, k, preferred_element_type=jnp.float32) * HEAD_DIM ** -0.5
    p = jax.nn.softmax(s, axis=-1).astype(v.dtype)
    return jnp.einsum('bhqk,bkhd->bqhd', p, v)


def neighbourhood_attention(q, k, v, kc, vc, rpb):
    b, n = q.shape[:2]
    rows = n // GRID_W
    kh = min(WIN_H, rows)
    ncb = GRID_W // NA_QB
    r = jnp.arange(rows)
    j = jnp.arange(ncb)
    row_start = jnp.clip(r - kh // 2, 0, rows - kh)
    col_blk = jnp.clip(j * NA_QB - WIN_W // 2, 0, GRID_W - NA_KBW)
    key_rows = row_start[:, None] + jnp.arange(kh)[None, :]
    key_cols = col_blk[:, None] + jnp.arange(NA_KBW)[None, :]
    idx = key_rows[:, None, :, None] * GRID_W + key_cols[None, :, None, :]
    nk = kh * NA_KBW
    k_blk = jnp.take(k, idx.reshape(-1), axis=1).reshape(b, rows, ncb, nk, NA_HEADS, HEAD_DIM)
    v_blk = jnp.take(v, idx.reshape(-1), axis=1).reshape(b, rows, ncb, nk, NA_HEADS, HEAD_DIM)
    q_blk = q.reshape(b, rows, ncb, NA_QB, NA_HEADS, HEAD_DIM)
    q_cols = j[:, None] * NA_QB + jnp.arange(NA_QB)[None, :]
    win_start = jnp.clip(q_cols - WIN_W // 2, 0, GRID_W - WIN_W)
    kc_b = key_cols[:, None, :]
    valid = (kc_b >= win_start[:, :, None]) & (kc_b < win_start[:, :, None] + WIN_W)
    dcol_idx = jnp.clip(kc_b - q_cols[:, :, None] + WIN_W - 1, 0, 2 * WIN_W - 2)
    drow_idx = key_rows - r[:, None] + WIN_H - 1
    bias = rpb[:, drow_idx[:, None, None, :, None], dcol_idx[None, :, :, None, :]].astype(jnp.float32)
    bias = jnp.where(valid[None, None, :, :, None, :], bias, NEG_INF)
    bias = bias.transpose(1, 2, 0, 3, 4, 5).reshape(rows, ncb, NA_HEADS, NA_QB, nk)
    scale = HEAD_DIM ** -0.5
    s_lat = jnp.einsum('brjqhd,brjkhd->brjhqk', q_blk, k_blk, preferred_element_type=jnp.float32) * scale + bias
    s_ctx = jnp.einsum('brjqhd,bkhd->brjhqk', q_blk, kc, preferred_element_type=jnp.float32) * scale
    p = jax.nn.softmax(jnp.concatenate([s_lat, s_ctx], axis=-1), axis=-1).astype(v.dtype)
    out = (jnp.einsum('brjhqk,brjkhd->brjqhd', p[..., :nk], v_blk)
           + jnp.einsum('brjhqk,bkhd->brjqhd', p[..., nk:], vc))
    return out.reshape(b, n, NA_WIDTH)


def diff_attend(q, k, v, lam):
    s = jnp.einsum('bqhd,bkhd->bhqk', q, k, preferred_element_type=jnp.float32) * HEAD_DIM ** -0.5
    p = jax.nn.softmax(s, axis=-1)
    b, _, nq, nkeys = p.shape
    p = p.reshape(b, DIFF_HEADS, 2, nq, nkeys)
    a = (p[:, :, 0] - lam * p[:, :, 1]).astype(v.dtype)
    return jnp.einsum('bhqk,bkhd->bqhd', a, v)


def diff_latent(q, k_all, v_all, lam):
    b, n = q.shape[:2]
    qb = q.reshape(b, n // Q_BLOCK, Q_BLOCK, 2 * DIFF_HEADS, HEAD_DIM).transpose(1, 0, 2, 3, 4)
    out = lax.map(lambda blk: diff_attend(blk, k_all, v_all, lam), qb)
    return out.transpose(1, 0, 2, 3, 4).reshape(b, n, DIFF_HEADS, 2 * HEAD_DIM)


def diff_post(o, subln_gain, lambda_init):
    return rms_norm(o, subln_gain) * (1.0 - lambda_init)


def short_conv(u, w):
    n = u.shape[1]
    up = jnp.pad(u, ((0, 0), (CONV_K // 2, CONV_K // 2), (0, 0)))
    y = w[0] * up[:, 0:n]
    for t in range(1, CONV_K):
        y = y + w[t] * up[:, t:t + n]
    return y


def swiglu(h, w1, w3, w2):
    return (jax.nn.silu(h @ w1) * (h @ w3)) @ w2


def moe_ffn(h, w_router, w1, w3, w2):
    logits = jnp.einsum('...d,de->...e', h.astype(jnp.float32), w_router.astype(jnp.float32))
    top_vals, top_idx = lax.top_k(logits, TOP_K)
    top_w = jax.nn.softmax(top_vals, axis=-1)
    gates = jnp.sum(jax.nn.one_hot(top_idx, N_EXPERTS, dtype=jnp.float32) * top_w[..., None], axis=-2).astype(h.dtype)
    y = jnp.zeros_like(h)
    for e in range(N_EXPERTS):
        y = y + gates[..., e:e + 1] * swiglu(h, w1[e], w3[e], w2[e])
    return y


def token_mixer(h_lat, h_ctx, w_in, w_out, rpb, lam, lambda_init, subln_gain, conv_w, cos, sin, ctx_out):
    b, n, _ = h_lat.shape
    L = h_ctx.shape[1]
    qa, qb, ka, kb, va, vb, xin, bg, cg = jnp.split(h_lat @ w_in, SPLIT_IDX, axis=-1)
    qa = qa.reshape(b, n, NA_HEADS, HEAD_DIM)
    ka = ka.reshape(b, n, NA_HEADS, HEAD_DIM)
    va = va.reshape(b, n, NA_HEADS, HEAD_DIM)
    qb = apply_rope(qb.reshape(b, n, 2 * DIFF_HEADS, HEAD_DIM), cos, sin)
    kb = apply_rope(kb.reshape(b, n, 2 * DIFF_HEADS, HEAD_DIM), cos, sin)
    vb = vb.reshape(b, n, DIFF_HEADS, 2 * HEAD_DIM)
    ka_c, kb_c, va_c, vb_c = jnp.split(h_ctx @ w_in[:, OFF_K:OFF_CONV], KV_SPLIT_IDX, axis=-1)
    ka_c = ka_c.reshape(b, L, NA_HEADS, HEAD_DIM)
    va_c = va_c.reshape(b, L, NA_HEADS, HEAD_DIM)
    kb_c = kb_c.reshape(b, L, 2 * DIFF_HEADS, HEAD_DIM)
    vb_c = vb_c.reshape(b, L, DIFF_HEADS, 2 * HEAD_DIM)
    na = neighbourhood_attention(qa, ka, va, ka_c, va_c, rpb)
    k_all = jnp.concatenate([kb_c, kb], axis=1)
    v_all = jnp.concatenate([vb_c, vb], axis=1)
    dif = diff_post(diff_latent(qb, k_all, v_all, lam), subln_gain, lambda_init).reshape(b, n, DIFF_WIDTH)
    conv = bg * short_conv(cg * xin, conv_w)
    out_lat = jnp.concatenate([na, dif, conv], axis=-1) @ w_out
    if not ctx_out:
        return out_lat, None
    qa_c, qb_c = jnp.split(h_ctx @ w_in[:, :OFF_K], (NA_WIDTH,), axis=-1)
    xin_c, bg_c, cg_c = jnp.split(h_ctx @ w_in[:, OFF_CONV:], (CONV_WIDTH, 2 * CONV_WIDTH), axis=-1)
    na_c = ctx_softmax_attention(qa_c.reshape(b, L, NA_HEADS, HEAD_DIM), ka_c, va_c).reshape(b, L, NA_WIDTH)
    dif_c = diff_post(diff_attend(qb_c.reshape(b, L, 2 * DIFF_HEADS, HEAD_DIM), kb_c, vb_c, lam),
                      subln_gain, lambda_init).reshape(b, L, DIFF_WIDTH)
    conv_c = bg_c * short_conv(cg_c * xin_c, conv_w)
    out_ctx = jnp.concatenate([na_c, dif_c, conv_c], axis=-1) @ w_out
    return out_lat, out_ctx


def setup_inputs(seed: int = 0) -> dict:
    key = jax.random.key(seed)
    ks = jax.random.split(key, 20)
    f32 = jnp.float32

    def nrm(k, shape, scale):
        return jax.random.normal(k, shape, f32) * scale

    return {
        'x': nrm(ks[0], (BATCH, SEQ, D_MODEL), 1.0),
        'c': nrm(ks[1], (BATCH, D_MODEL), 1.0),
        'ctx': nrm(ks[2], (BATCH, CTX_LEN, D_MODEL), 1.0),
        'c_ctx': nrm(ks[3], (D_MODEL,), 1.0),
        'ada_w': nrm(ks[4], (DEPTH, D_MODEL, 6 * D_MODEL), 0.5 * D_MODEL ** -0.5),
        'ada_b': nrm(ks[5], (DEPTH, 6 * D_MODEL), 0.01),
        'w_in': nrm(ks[6], (DEPTH, D_MODEL, IN_COLS), D_MODEL ** -0.5),
        'w_out': nrm(ks[7], (DEPTH, MIX_WIDTH, D_MODEL), MIX_WIDTH ** -0.5),
        'na_rpb': nrm(ks[8], (DEPTH, NA_HEADS, 2 * WIN_H - 1, 2 * WIN_W - 1), 0.1),
        'diff_lambda': nrm(ks[9], (DEPTH, 4, HEAD_DIM), 0.1),
        'diff_subln': 1.0 + nrm(ks[10], (DEPTH, 2 * HEAD_DIM), 0.02),
        'conv_w': nrm(ks[11], (DEPTH, CONV_K, CONV_WIDTH), CONV_K ** -0.5),
        'ffn_w1': nrm(ks[12], (N_DENSE, D_MODEL, D_FF), D_MODEL ** -0.5),
        'ffn_w3': nrm(ks[13], (N_DENSE, D_MODEL, D_FF), D_MODEL ** -0.5),
        'ffn_w2': nrm(ks[14], (N_DENSE, D_FF, D_MODEL), D_FF ** -0.5),
        'router_w': nrm(ks[15], (N_MOE, D_MODEL, N_EXPERTS), D_MODEL ** -0.5),
        'moe_w1': nrm(ks[16], (N_MOE, N_EXPERTS, D_MODEL, D_EXPERT), D_MODEL ** -0.5),
        'moe_w3': nrm(ks[17], (N_MOE, N_EXPERTS, D_MODEL, D_EXPERT), D_MODEL ** -0.5),
        'moe_w2': nrm(ks[18], (N_MOE, N_EXPERTS, D_EXPERT, D_MODEL), D_EXPERT ** -0.5),
        'final_gain': 1.0 + nrm(ks[19], (D_MODEL,), 0.02),
    }


def reference(x, c, ctx, c_ctx, ada_w, ada_b, w_in, w_out, na_rpb, diff_lambda, diff_subln, conv_w,
              ffn_w1, ffn_w3, ffn_w2, router_w, moe_w1, moe_w3, moe_w2, final_gain):
    n = x.shape[1]
    cos, sin = axial_rope(n, x.dtype)
    x_lat, x_ctx = x, ctx
    for i in range(DEPTH):
        ctx_out = i < DEPTH - 1
        mod_lat = (jax.nn.silu(c) @ ada_w[i] + ada_b[i])[:, None, :]
        mod_ctx = jax.nn.silu(c_ctx) @ ada_w[i] + ada_b[i]
        sh_a, sc_a, g_a, sh_f, sc_f, g_f = jnp.split(mod_lat, 6, axis=-1)
        csh_a, csc_a, cg_a, csh_f, csc_f, cg_f = jnp.split(mod_ctx, 6, axis=-1)
        lq1, lk1, lq2, lk2 = [t.astype(jnp.float32) for t in diff_lambda[i]]
        lambda_init = 0.8 - 0.6 * math.exp(-0.3 * i)
        lam = jnp.exp(jnp.sum(lq1 * lk1)) - jnp.exp(jnp.sum(lq2 * lk2)) + lambda_init
        h_lat = modulate(rms_norm(x_lat), sh_a, sc_a)
        h_ctx = modulate(rms_norm(x_ctx), csh_a, csc_a)
        out_lat, out_ctx = token_mixer(h_lat, h_ctx, w_in[i], w_out[i], na_rpb[i], lam, lambda_init,
                                       diff_subln[i], conv_w[i], cos, sin, ctx_out)
        x_lat = x_lat + g_a * out_lat
        if i % 2 == 0:
            m = i // 2
            ffn = functools_partial_swiglu(ffn_w1[m], ffn_w3[m], ffn_w2[m])
        else:
            m = i // 2
            ffn = functools_partial_moe(router_w[m], moe_w1[m], moe_w3[m], moe_w2[m])
        x_lat = x_lat + g_f * ffn(modulate(rms_norm(x_lat), sh_f, sc_f))
        if ctx_out:
            x_ctx = x_ctx + cg_a * out_ctx
            x_ctx = x_ctx + cg_f * ffn(modulate(rms_norm(x_ctx), csh_f, csc_f))
    return rms_norm(x_lat, final_gain)


def functools_partial_swiglu(w1, w3, w2):
    return lambda h: swiglu(h, w1, w3, w2)


def functools_partial_moe(w_router, w1, w3, w2):
    return lambda h: moe_ffn(h, w_router, w1, w3, w2)
```

```python
import contextlib
import math
import numpy as np
import ml_dtypes
import concourse.bass as bass
import concourse.mybir as mybir
from concourse.bass_utils import run_bass_kernel_spmd

F32 = mybir.dt.float32
BF16 = mybir.dt.bfloat16
ALU = mybir.AluOpType
AF = mybir.ActivationFunctionType
AX = mybir.AxisListType
NPBF = ml_dtypes.bfloat16

ENG_NAMES = ("pe", "act", "dve", "pool", "sp")

D = 1024
KC = 8
NL = 2048
LC = 256
NCORES = 8
GRID_W = 64
EPS = 1e-6
D_FF = 2816
D_EXP = 3584
NEXP = 8


class Buf:
    __slots__ = ("name", "w", "r")

    def __init__(self, name=""):
        self.name = name
        self.w = None
        self.r = []


class Prog:
    N_DMA_SEMS = 24

    def __init__(self, nc):
        self.nc = nc
        self.ops = {e: [] for e in ENG_NAMES}
        self.seen = {e: {} for e in ENG_NAMES}
        self.dma_uses = [0] * self.N_DMA_SEMS
        self.dma_rr = {}
        self.out_tokens = []
        self.n_coll = 0

    def _dma_slot(self, q):
        half = self.N_DMA_SEMS // 2
        lo = half if q == "pool" else 0
        k = self.dma_rr.get(q, 0)
        self.dma_rr[q] = k + 1
        return lo + k % half

    def _collect(self, reads, writes):
        deps = []
        for b in reads:
            if b.w is not None:
                deps.append(b.w)
        for b in writes:
            if b.w is not None:
                deps.append(b.w)
            deps.extend(b.r)
        return deps

    def _filter(self, e, deps):
        seen = self.seen[e]
        out = {}
        for t in deps:
            if t[0] == "c":
                _, f, seq = t
                if f == e and e == "pe":
                    continue
                key = ("c", f)
                val = seq
            else:
                _, j, val = t
                key = (t[0], j)
            if seen.get(key, 0) >= val:
                continue
            if out.get(key, 0) < val:
                out[key] = val
        for k, v in out.items():
            seen[k] = v
            if k[0] == "c":
                self.ops[k[1]][v - 1]["inc"] = True
        return list(out.items())

    def op(self, e, meth, reads=(), writes=(), **kw):
        fn = (meth, kw)
        deps = self._collect(reads, writes)
        waits = self._filter(e, deps)
        lst = self.ops[e]
        seq = len(lst) + 1
        lst.append({"fn": fn, "waits": waits, "inc": False, "dma": None})
        tok = ("c", e, seq)
        for b in writes:
            b.w = tok
            b.r = []
        for b in reads:
            b.r.append(tok)
        return tok

    def dma(self, q, out, in_, reads=(), writes=(), is_output=False, **kw):
        deps = self._collect(reads, writes)
        j = self._dma_slot(q)
        prev = self.dma_uses[j] * 16
        if prev:
            deps.append(("d", j, prev))
        waits = self._filter(q, deps)
        self.dma_uses[j] += 1
        val = self.dma_uses[j] * 16
        kw = dict(kw)
        kw["out"] = out
        kw["in_"] = in_
        self.ops[q].append({"fn": ("dma_start", kw),
                            "waits": waits, "inc": False, "dma": j})
        tok = ("d", j, val)
        for b in writes:
            b.w = tok
            b.r = []
        for b in reads:
            b.r.append(tok)
        if is_output:
            self.out_tokens.append(tok)
        return tok

    def custom(self, e, fn, reads=(), writes=(), dma=False):
        if not dma:
            deps = self._collect(reads, writes)
            waits = self._filter(e, deps)
            lst = self.ops[e]
            seq = len(lst) + 1
            lst.append({"fn": ("__custom__", fn), "waits": waits, "inc": False, "dma": None})
            tok = ("c", e, seq)
        else:
            deps = self._collect(reads, writes)
            j = self._dma_slot(e)
            prev = self.dma_uses[j] * 16
            if prev:
                deps.append(("d", j, prev))
            waits = self._filter(e, deps)
            self.dma_uses[j] += 1
            self.ops[e].append({"fn": ("__custom__", fn), "waits": waits, "inc": False, "dma": j})
            tok = ("d", j, self.dma_uses[j] * 16)
        for b in writes:
            b.w = tok
            b.r = []
        for b in reads:
            b.r.append(tok)
        return tok

    def collective(self, reads, writes, **kw):
        deps = self._collect(reads, writes)
        waits = self._filter("pool", deps)
        j = self.n_coll
        self.n_coll += 1
        self.ops["pool"].append({"fn": ("collective_compute", kw), "waits": waits, "inc": False, "dma": None, "coll": j})
        tok = ("x", j, 1)
        for b in writes:
            b.w = tok
            b.r = []
        for b in reads:
            b.r.append(tok)
        return tok

    def finish(self):
        waits = self._filter("sp", list(self.out_tokens))
        self.ops["sp"].append({"fn": None, "waits": waits, "inc": False, "dma": None})

    def emit(self):
        nc = self.nc
        with contextlib.ExitStack() as st:
            csem = {e: st.enter_context(nc.semaphore("c_" + e)) for e in ENG_NAMES}
            dsem = [st.enter_context(nc.semaphore("d%d" % j)) for j in range(self.N_DMA_SEMS)]
            xsem = [st.enter_context(nc.semaphore("x%d" % j)) for j in range(self.n_coll)]
            cum = {}
            for e in ENG_NAMES:
                c = 0
                arr = []
                for o in self.ops[e]:
                    if o["inc"]:
                        c += 1
                    arr.append(c)
                cum[e] = arr

            def run(e, eng):
                for o in self.ops[e]:
                    for key, val in o["waits"]:
                        if key[0] == "c":
                            eng.wait_ge(csem[key[1]], cum[key[1]][val - 1])
                        elif key[0] == "x":
                            eng.wait_ge(xsem[key[1]], val)
                        else:
                            eng.wait_ge(dsem[key[1]], val)
                    if o["fn"] is None:
                        continue
                    if o["fn"][0] == "__custom__":
                        ins = o["fn"][1](eng)
                    else:
                        ins = getattr(eng, o["fn"][0])(**o["fn"][1])
                    if o.get("coll") is not None:
                        ins.then_inc(xsem[o["coll"]], 1)
                    elif o["dma"] is not None:
                        ins.then_inc(dsem[o["dma"]], 16)
                    elif o["inc"]:
                        ins.then_inc(csem[e], 1)

            with nc.Block() as block:
                @block.tensor
                def _(eng):
                    run("pe", eng)

                @block.scalar
                def _(eng):
                    run("act", eng)

                @block.vector
                def _(eng):
                    run("dve", eng)

                @block.gpsimd
                def _(eng):
                    run("pool", eng)

                @block.sync
                def _(eng):
                    run("sp", eng)


class Env:
    def __init__(self):
        self.nc = bass.Bass("TRN2", target_bir_lowering=False)
        self.P = Prog(self.nc)
        self.st = contextlib.ExitStack()
        self.names = set()
        self.psb = None
        self.ps_rr = 0
        self.uid = 0

    def din(self, name, shape, dt=F32):
        return self.nc.dram_tensor(name, list(shape), dt, kind="ExternalInput").ap()

    def dout(self, name, shape, dt=F32):
        return self.nc.dram_tensor(name, list(shape), dt, kind="ExternalOutput").ap()

    def sb(self, name, shape, dt=F32, st=None):
        self.uid += 1
        return (st or self.st).enter_context(self.nc.sbuf_tensor("s_%s_%d" % (name, self.uid), list(shape), dt))

    def dint(self, name, shape, dt=F32):
        return self.nc.dram_tensor(name, list(shape), dt).ap()

    def psum(self):
        self.ps = self.st.enter_context(self.nc.psum_tensor("ps", [128, 8, 512], F32))
        self.psb = [Buf("ps%d" % i) for i in range(8)]
        return self.ps

    def bank(self):
        i = self.ps_rr
        self.ps_rr = (self.ps_rr + 1) % 8
        return i

    def done(self):
        self.P.finish()
        self.P.emit()
        self.st.close()
        return self.nc


def tiles_of(nt):
    out = []
    t = 0
    while t < nt:
        w = min(512, nt - t)
        out.append((t, w))
        t += w
    return out


def emit_consts(E):
    P = E.P
    ones = E.sb("ones_bf", [128, 128], BF16)
    Bones = Buf("ones")
    P.op("pool", "memset", writes=[Bones], ap=ones[:], constant=1.0)
    return ones, Bones


def emit_norm_mod_tile(E, ones, Bones, xsrc, Bx, w, sh, sc1, Bmod, hdst, Bh, scr, h32dst=None, Bh32=None):
    P = E.P
    sq, Bsq = scr["sq"], scr["Bsq"]
    rstd, Brstd = scr["rstd"], scr["Brstd"]
    tmp, Btmp = scr["tmp"], scr["Btmp"]
    P.op("act", "activation", reads=[Bx], writes=[Bsq], out=sq[:, :, 0:w], in_=xsrc, func=AF.Square)
    b = E.bank()
    for k in range(KC):
        P.op("pe", "matmul", reads=[Bones, Bsq], writes=[E.psb[b]], out=E.ps[:, b, 0:w], lhsT=ones[:], rhs=sq[:, k, 0:w], start=(k == 0), stop=(k == KC - 1))
    P.op("act", "activation", reads=[E.psb[b]], writes=[Brstd], out=rstd[:, 0:w], in_=E.ps[:, b, 0:w], func=AF.Sqrt, bias=EPS, scale=1.0 / D)
    P.op("dve", "reciprocal", reads=[Brstd], writes=[Brstd], out=rstd[:, 0:w], in_=rstd[:, 0:w])
    for k in range(KC):
        i = k % 2
        P.op("dve", "scalar_tensor_tensor", reads=[Bx, Brstd, Bmod], writes=[Btmp[i]], out=tmp[i][:, 0:w], in0=xsrc[:, k, :], scalar=sc1[:, k:k + 1], in1=rstd[:, 0:w],
                                                                op0=ALU.mult, op1=ALU.mult)
        if h32dst is not None:
            P.op("pool", "tensor_scalar", reads=[Btmp[i], Bmod], writes=[Bh32], out=h32dst[:, k, :], in0=tmp[i][:, 0:w], scalar1=sh[:, k:k + 1], scalar2=None, op0=ALU.add)
        P.op("act", "activation", reads=[Btmp[i], Bmod], writes=[Bh], out=hdst[:, k, :], in_=tmp[i][:, 0:w], func=AF.Identity, bias=sh[:, k:k + 1], scale=1.0)


def norm_scratch(E):
    scr = {}
    scr["sq"] = E.sb("n_sq", [128, KC, 512], BF16)
    scr["Bsq"] = Buf("sq")
    scr["rstd"] = E.sb("n_rstd", [128, 512], F32)
    scr["Brstd"] = Buf("rstd")
    scr["tmp"] = [E.sb("n_tmp%d" % i, [128, 512], F32) for i in range(2)]
    scr["Btmp"] = [Buf("tmp%d" % i) for i in range(2)]
    return scr


def emit_load_mod(E, mod_d):
    P = E.P
    mod = E.sb("mod", [128, 48, 2], F32)
    sc1a = E.sb("sc1a", [128, 8, 2], F32)
    sc1f = E.sb("sc1f", [128, 8, 2], F32)
    Bmod = Buf("mod")
    P.dma("sp", mod[:], mod_d, writes=[Bmod])
    P.op("dve", "tensor_scalar", reads=[Bmod], writes=[Bmod], out=sc1a[:], in0=mod[:, 8:16, :], scalar1=1.0, scalar2=None, op0=ALU.add)
    P.op("dve", "tensor_scalar", reads=[Bmod], writes=[Bmod], out=sc1f[:], in0=mod[:, 32:40, :], scalar1=1.0, scalar2=None, op0=ALU.add)
    return mod, sc1a, sc1f, Bmod


I32 = mybir.dt.int32
NT = NL + LC
NKB = 66
NKEY = NKB * 128
GROWS = 513


def prog_barrier(P):
    toks = []
    for f in ("pe", "act", "dve", "pool"):
        lst = P.ops[f]
        for idx in range(len(lst) - 1, -1, -1):
            if lst[idx]["fn"] is not None and lst[idx]["dma"] is None and lst[idx].get("coll") is None:
                toks.append(("c", f, idx + 1))
                break
    for j in range(P.N_DMA_SEMS):
        if P.dma_uses[j]:
            toks.append(("d", j, P.dma_uses[j] * 16))
    for e in ENG_NAMES:
        waits = P._filter(e, list(toks))
        P.ops[e].append({"fn": None, "waits": waits, "inc": False, "dma": None})


def build_fused(nstages=6):
    E = Env()
    P = E.P
    nc = E.nc
    xT_d = E.din("xT", [128, KC, NT])
    cT_d = E.din("cT", [128, KC, 2])
    adaw_d = E.din("adaw", [2, 128, KC, 6144])
    adab_d = E.din("adab", [2, 128, 48])
    wall_d = E.din("wall", [2, 128, KC, 4096])
    ropeC_d = E.din("ropeC", [64, NL])
    ropeS_d = E.din("ropeS", [64, NL])
    bc_d = [E.din("nabc%d" % i, [64 * 15, 256]) for i in range(2)]
    idxtab_d = E.din("idxtab", [128, 64], I32)
    selh_d = E.din("selh", [128, 8])
    convw_d = E.din("convw", [2, 128, 2, 3])
    wna_d = E.din("wout_na", [2, 64, 4, D])
    wor_d = E.din("wout_r", [2, 128, 6, D])
    dlam_d = E.din("dlam", [2, 1, 256])
    subln_d = E.din("subln", [2, 128, 1])
    identb_d = E.din("identb", [128, 128], BF16)
    fw1_d = E.din("fw1", [1, 128, KC, D_FF])
    fw3_d = E.din("fw3", [1, 128, KC, D_FF])
    fw2_d = E.din("fw2", [1, 128, D_FF // 128, D])
    NEI = NEXP if nstages >= 6 else 1
    mw1_d = E.din("mw1", [NEI, 128, KC, D_EXP])
    mw3_d = E.din("mw3", [NEI, 128, KC, D_EXP])
    mw2_d = E.din("mw2", [NEI, 128, D_EXP // 128, D])
    wrt_d = E.din("wrt", [128, KC, NEXP])
    sel_d = E.din("sel", [8, 8, 128])
    ident_d = E.din("ident", [128, 128])
    fg_d = E.din("fgain", [128, KC])
    xo_d = E.dout("xo", [128, KC, NL])
    SC = []
    for l in range(2):
        sc = {}
        for nm in ("AK", "AV", "V0", "V1"):
            sc["blob" + nm] = E.dint("blob%s%d" % (nm, l), [NL, 256], BF16)
            sc["G" + nm] = E.dint("G%s%d" % (nm, l), [4 * NL, 256], BF16)
        for nm in ("B0", "B1"):
            sc["blob" + nm] = E.dint("blob%s%d" % (nm, l), [256, NL], BF16)
            sc["G" + nm] = E.dint("G%s%d" % (nm, l), [4 * 256, NL], BF16)
        sc["blobE"] = E.dint("blobE%d" % l, [4, 512], BF16)
        sc["GE"] = E.dint("GE%d" % l, [16, 512], BF16)
        sc["qb"] = E.dint("qb_s%d" % l, [8, 64, NT], BF16)
        sc["qa"] = E.dint("qa_s%d" % l, [128, 2, NT], BF16)
        sc["kvac"] = E.dint("kvac_s%d" % l, [LC, 512], BF16)
        sc["kbc"] = E.dint("kbc_s%d" % l, [8, 64, LC], BF16)
        sc["vbc"] = E.dint("vbc_s%d" % l, [LC, 512], BF16)
        sc["cgx"] = E.dint("cgx_s%d" % l, [128, 2, NT], F32)
        sc["bg"] = E.dint("bg_s%d" % l, [128, 2, NT], F32)
        for k in list(sc.keys()):
            sc["B" + k] = Buf(k)
        SC.append(sc)
    E.psum()
    ps_bf = E.ps.bitcast(BF16)
    ones, Bones = emit_consts(E)
    tl = tiles_of(NT)
    x = E.sb("x", [128, KC, NT], F32)
    Bx = [Buf("x%d" % i) for i in range(len(tl))]
    for ti, (t0, w) in enumerate(tl):
        P.dma("sp", x[:, :, t0:t0 + w], xT_d[:, :, t0:t0 + w], writes=[Bx[ti]])
    mo = E.sb("mo", [128, 2, 48, 2], F32)
    sc1 = E.sb("sc1", [128, 2, 2, 8, 2], F32)
    Bmod = Buf("mod")
    idb = E.sb("idb", [128, 128], BF16)
    Bid = Buf("idb")
    P.dma("sp", idb[:], identb_d, writes=[Bid])

    def phase():
        return contextlib.ExitStack()

    with phase() as ph:
        cf = E.sb("cf", [128, KC, 2], F32, ph)
        cs = E.sb("cs", [128, KC, 2], BF16, ph)
        ab = E.sb("ab", [128, 2, 48], F32, ph)
        slots = [E.sb("aw", [128, KC, 2048], BF16, ph) for i in range(3)]
        Bs = [Buf("aw%d" % i) for i in range(3)]
        Bc, Bab = Buf("c"), Buf("ab")
        P.dma("sp", cf[:], cT_d, writes=[Bc])
        for l in range(2):
            P.dma("sp", ab[:, l, :], adab_d[l], writes=[Bab])
        P.op("act", "activation", reads=[Bc], writes=[Bc], out=cs[:], in_=cf[:], func=AF.Silu)
        for l in range(2):
            b = E.bank()
            for th in range(3):
                s = (l * 3 + th) % 3
                P.dma("pool", slots[s][:], adaw_d[l, :, :, th * 2048:(th + 1) * 2048], writes=[Bs[s]])
                for m in range(16):
                    mc = th * 16 + m
                    for k in range(KC):
                        P.op("pe", "matmul", reads=[Bs[s], Bc], writes=[E.psb[b]], out=E.ps[:, b, 2 * mc:2 * mc + 2], lhsT=slots[s][:, k, m * 128:(m + 1) * 128],
                             rhs=cs[:, k, :], start=(k == 0), stop=(k == KC - 1))
            for j in range(2):
                P.op("dve", "tensor_tensor", reads=[E.psb[b], Bab], writes=[Bmod], out=mo[:, l, :, j], in0=E.ps[:, b, j:96:2], in1=ab[:, l, :], op=ALU.add)
            P.op("dve", "tensor_scalar", reads=[Bmod], writes=[Bmod], out=sc1[:, l, 0, :, :], in0=mo[:, l, 8:16, :], scalar1=1.0, scalar2=None, op0=ALU.add)
            P.op("dve", "tensor_scalar", reads=[Bmod], writes=[Bmod], out=sc1[:, l, 1, :, :], in0=mo[:, l, 32:40, :], scalar1=1.0, scalar2=None, op0=ALU.add)
        prog_barrier(P)

    def norm_scr(ph):
        scr = {}
        scr["sq"] = E.sb("n_sq", [128, KC, 512], BF16, ph)
        scr["Bsq"] = Buf("sq")
        scr["rstd"] = E.sb("n_rstd", [128, 512], F32, ph)
        scr["Brstd"] = Buf("rstd")
        scr["tmp"] = [E.sb("n_tmp", [128, 512], F32, ph) for i in range(2)]
        scr["Btmp"] = [Buf("tmp%d" % i) for i in range(2)]
        return scr

    evac_rr = [0]

    def evac_copy(dst, src, reads, writes):
        if evac_rr[0] % 2 == 0:
            P.op("act", "activation", reads=reads, writes=writes, out=dst, in_=src, func=AF.Copy)
        else:
            P.op("dve", "tensor_copy", reads=reads, writes=writes, out=dst, in_=src)
        evac_rr[0] += 1

    def stage_L1(l):
        sc = SC[l]
        with phase() as ph0:
            hT = E.sb("hT", [128, KC, NT], BF16, ph0)
            Bh = [Buf("h%d" % i) for i in range(len(tl))]
            with phase() as ph:
                scr = norm_scr(ph)
                for ti, (t0, w) in enumerate(tl):
                    j = 0 if t0 < NL else 1
                    emit_norm_mod_tile(E, ones, Bones, x[:, :, t0:t0 + w], Bx[ti], w, mo[:, l, 0:8, j], sc1[:, l, 0, :, j], Bmod,
                                       hT[:, :, t0:t0 + w], Bh[ti], scr)
                prog_barrier(P)
            with phase() as ph:
                rC = E.sb("rC", [64, NL], F32, ph)
                rS = E.sb("rS", [64, NL], F32, ph)
                Brope = Buf("rope")
                P.dma("sp", rC[:], ropeC_d, writes=[Brope])
                P.dma("sp", rS[:], ropeS_d, writes=[Brope])
                NW = 3
                wsl = [E.sb("wsl", [128, KC, 512], BF16, ph) for i in range(NW)]
                Bw = [Buf("wsl%d" % i) for i in range(NW)]
                stg = [E.sb("stg", [128, NT], F32, ph) for i in range(2)]
                Bstg = [Buf("stg%d" % i) for i in range(2)]
                t1 = [E.sb("rt", [64, 512], F32, ph) for i in range(4)]
                Bt1 = [Buf("rt%d" % i) for i in range(4)]
                xin_t = [E.sb("xin", [128, 512], F32, ph) for i in range(2)]
                Bxin = [Buf("xin%d" % i) for i in range(2)]
                kvst = E.sb("kvst", [128, 9, 512], BF16, ph)
                Bkvst = Buf("kvst")
                egw = E.sb("egw", [128, 2, 2], BF16, ph)
                Begw = Buf("egw")
                stg_rr = [0]
                wrr = [0]

                def load_slab(s):
                    slot = wrr[0] % NW
                    wrr[0] += 1
                    P.dma("pool", wsl[slot][:], wall_d[l, :, :, s * 512:(s + 1) * 512], writes=[Bw[slot]])
                    return slot

                def next_stage():
                    i = stg_rr[0]
                    stg_rr[0] = (i + 1) % 2
                    return i

                def roped(sa, sr, dests):
                    for hm in range(8):
                        si = next_stage()
                        sview = stg[si].bitcast(BF16)
                        for ti, (t0, w) in enumerate(tl):
                            ba = E.bank()
                            for k in range(KC):
                                P.op("pe", "matmul", reads=[Bw[sa], Bh[ti]], writes=[E.psb[ba]], out=E.ps[0:64, ba, 0:w], lhsT=wsl[sa][:, k, hm * 64:(hm + 1) * 64],
                                     rhs=hT[:, k, t0:t0 + w], start=(k == 0), stop=(k == KC - 1))
                            if t0 < NL:
                                bb = E.bank()
                                for k in range(KC):
                                    P.op("pe", "matmul", reads=[Bw[sr], Bh[ti]], writes=[E.psb[bb]], out=E.ps[0:64, bb, 0:w], lhsT=wsl[sr][:, k, hm * 64:(hm + 1) * 64],
                                         rhs=hT[:, k, t0:t0 + w], start=(k == 0), stop=(k == KC - 1))
                                ia = (2 * ti) % 4
                                ib = (2 * ti + 1) % 4
                                P.op("dve", "tensor_tensor", reads=[E.psb[ba], Brope], writes=[Bt1[ia]], out=t1[ia][:, 0:w], in0=E.ps[0:64, ba, 0:w], in1=rC[:, t0:t0 + w], op=ALU.mult)
                                P.op("dve", "tensor_tensor", reads=[E.psb[bb], Brope], writes=[Bt1[ib]], out=t1[ib][:, 0:w], in0=E.ps[0:64, bb, 0:w], in1=rS[:, t0:t0 + w], op=ALU.mult)
                                P.op("pool", "tensor_tensor", reads=[Bt1[ia], Bt1[ib]], writes=[Bstg[si]], out=sview[0:64, t0:t0 + w], in0=t1[ia][:, 0:w], in1=t1[ib][:, 0:w], op=ALU.add)
                            else:
                                evac_copy(sview[0:64, t0:t0 + w], E.ps[0:64, ba, 0:w], [E.psb[ba]], [Bstg[si]])
                        for (dap, c0, ncol, Bd) in dests(hm):
                            P.dma("sp", dap, sview[0:64, c0:c0 + ncol], reads=[Bstg[si]], writes=[Bd])

                def plain_chunk(slot, c, dap, Bd, dt):
                    si = next_stage()
                    sview = stg[si].bitcast(BF16) if dt == BF16 else stg[si]
                    for ti, (t0, w) in enumerate(tl):
                        b = E.bank()
                        for k in range(KC):
                            P.op("pe", "matmul", reads=[Bw[slot], Bh[ti]], writes=[E.psb[b]], out=E.ps[:, b, 0:w], lhsT=wsl[slot][:, k, c * 128:(c + 1) * 128],
                                 rhs=hT[:, k, t0:t0 + w], start=(k == 0), stop=(k == KC - 1))
                        evac_copy(sview[:, t0:t0 + w], E.ps[:, b, 0:w], [E.psb[b]], [Bstg[si]])
                    P.dma("sp", dap, sview[:, 0:NT], reads=[Bstg[si]], writes=[Bd])

                def tokmajor(slot, nm0, nm1, ctx_ap, Bctx):
                    bv = [sc["blob" + nm0].rearrange("(j p) c -> p j c", p=128), sc["blob" + nm1].rearrange("(j p) c -> p j c", p=128)]
                    Bb = [sc["Bblob" + nm0], sc["Bblob" + nm1]]
                    ctx_v = ctx_ap.rearrange("(j p) c -> p j c", p=128)
                    for half in range(2):
                        for jj in range(9):
                            blk = half * 9 + jj
                            b = E.bank()
                            ti = min(blk // 4, 4)
                            for k in range(KC):
                                P.op("pe", "matmul", reads=[Bw[slot], Bh[ti]], writes=[E.psb[b]], out=E.ps[:, b, :], lhsT=hT[:, k, blk * 128:(blk + 1) * 128],
                                     rhs=wsl[slot][:, k, :], start=(k == 0), stop=(k == KC - 1))
                            evac_copy(kvst[:, jj, :], E.ps[:, b, :], [E.psb[b]], [Bkvst])
                        for hh in range(2):
                            if half == 0:
                                P.dma("sp", bv[hh][:, 0:9, :], kvst[:, 0:9, hh * 256:(hh + 1) * 256], reads=[Bkvst], writes=[Bb[hh]])
                            else:
                                P.dma("sp", bv[hh][:, 9:16, :], kvst[:, 0:7, hh * 256:(hh + 1) * 256], reads=[Bkvst], writes=[Bb[hh]])
                        if half == 1:
                            P.dma("sp", ctx_v[:, 0:2, :], kvst[:, 7:9, :], reads=[Bkvst], writes=[Bctx])

                sa = load_slab(1)
                sr = load_slab(2)
                roped(sa, sr, lambda hm: [(sc["blobB%d" % (hm // 4)][(hm % 4) * 64:(hm % 4 + 1) * 64, :], 0, NL, sc["BblobB%d" % (hm // 4)]),
                                          (sc["kbc"][hm], NL, LC, sc["Bkbc"])])
                AGKW = dict(kind="AllGather", op=ALU.bypass, replica_groups=[[0, 1, 2, 3], [4, 5, 6, 7]])
                for nm in ("B0", "B1"):
                    P.collective(reads=[sc["Bblob" + nm]], writes=[sc["BG" + nm]], ins=[sc["blob" + nm]], outs=[sc["G" + nm]], **AGKW)
                s7 = load_slab(7)
                for c in range(2):
                    si = next_stage()
                    for ti, (t0, w) in enumerate(tl):
                        bx = E.bank()
                        for k in range(KC):
                            P.op("pe", "matmul", reads=[Bw[s7], Bh[ti]], writes=[E.psb[bx]], out=E.ps[:, bx, 0:w], lhsT=wsl[s7][:, k, c * 128:(c + 1) * 128],
                                 rhs=hT[:, k, t0:t0 + w], start=(k == 0), stop=(k == KC - 1))
                        bc = E.bank()
                        for k in range(KC):
                            P.op("pe", "matmul", reads=[Bw[s7], Bh[ti]], writes=[E.psb[bc]], out=E.ps[:, bc, 0:w], lhsT=wsl[s7][:, k, (2 + c) * 128:(3 + c) * 128],
                                 rhs=hT[:, k, t0:t0 + w], start=(k == 0), stop=(k == KC - 1))
                        xi = ti % 2
                        P.op("act", "activation", reads=[E.psb[bx]], writes=[Bxin[xi]], out=xin_t[xi][:, 0:w], in_=E.ps[:, bx, 0:w], func=AF.Copy)
                        P.op("dve", "tensor_tensor", reads=[E.psb[bc], Bxin[xi]], writes=[Bstg[si]], out=stg[si][:, t0:t0 + w], in0=E.ps[:, bc, 0:w], in1=xin_t[xi][:, 0:w], op=ALU.mult)
                    P.op("dve", "tensor_copy", reads=[Bstg[si]], writes=[Begw], out=egw[:, c, 0:1], in_=stg[si][:, 0:1])
                    P.op("dve", "tensor_copy", reads=[Bstg[si]], writes=[Begw], out=egw[:, c, 1:2], in_=stg[si][:, NL - 1:NL])
                    P.dma("sp", sc["cgx"][:, c, :], stg[si][:, 0:NT], reads=[Bstg[si]], writes=[sc["Bcgx"]])
                P.dma("sp", sc["blobE"][0, 0:512].rearrange("(c p j) -> p c j", c=2, p=128, j=2), egw[:], reads=[Begw], writes=[sc["BblobE"]],
                      allow_slow_non_contiguous=True)
                P.collective(reads=[sc["BblobE"]], writes=[sc["BGE"]], ins=[sc["blobE"]], outs=[sc["GE"]], **AGKW)
                s3 = load_slab(3)
                tokmajor(s3, "V0", "V1", sc["vbc"], sc["Bvbc"])
                for nm in ("V0", "V1"):
                    P.collective(reads=[sc["Bblob" + nm]], writes=[sc["BG" + nm]], ins=[sc["blob" + nm]], outs=[sc["G" + nm]], **AGKW)
                sa = load_slab(4)
                sr = load_slab(5)
                roped(sa, sr, lambda hm: [(sc["qb"][hm], 0, NT, sc["Bqb"])])
                s0 = load_slab(0)
                tokmajor(s0, "AK", "AV", sc["kvac"], sc["Bkvac"])
                for nm in ("AK", "AV"):
                    P.collective(reads=[sc["Bblob" + nm]], writes=[sc["BG" + nm]], ins=[sc["blob" + nm]], outs=[sc["G" + nm]], **AGKW)
                s6 = load_slab(6)
                plain_chunk(s6, 0, sc["qa"][:, 0, :], sc["Bqa"], BF16)
                plain_chunk(s6, 1, sc["qa"][:, 1, :], sc["Bqa"], BF16)
                plain_chunk(s6, 2, sc["bg"][:, 0, :], sc["Bbg"], F32)
                plain_chunk(s6, 3, sc["bg"][:, 1, :], sc["Bbg"], F32)
                prog_barrier(P)

    def stage_L2(l, with_ctx):
        sc = SC[l]
        NQ = NT if with_ctx else NL
        lambda_init = 0.8 - 0.6 * math.exp(-0.3 * l)
        with phase() as ph0:
            mixd = E.sb("mixd", [128, 4, NQ], BF16, ph0)
            Bmixd = [Buf("mixd%d" % i) for i in range(4)]
            dl = E.sb("dl", [1, 256], F32, ph0)
            lt = E.sb("lt", [1, 128], F32, ph0)
            ls = E.sb("ls", [1, 4], F32, ph0)
            ones1 = E.sb("ones1", [1, 128], F32, ph0)
            nl = E.sb("nl", [128, 1], F32, ph0)
            gsc = E.sb("gsc", [128, 1], F32, ph0)
            Bl = Buf("lam")
            P.dma("sp", dl[:], dlam_d[l], writes=[Bl])
            P.dma("sp", gsc[:], subln_d[l], writes=[Bl])
            P.op("pool", "memset", writes=[Bl], ap=ones1[:], constant=1.0)
            P.op("dve", "tensor_tensor", reads=[Bl], writes=[Bl], out=lt[:, 0:64], in0=dl[:, 0:64], in1=dl[:, 64:128], op=ALU.mult)
            P.op("dve", "tensor_tensor", reads=[Bl], writes=[Bl], out=lt[:, 64:128], in0=dl[:, 128:192], in1=dl[:, 192:256], op=ALU.mult)
            P.op("dve", "reduce_sum", reads=[Bl], writes=[Bl], out=ls[:, 0:1], in_=lt[:, 0:64], axis=AX.X)
            P.op("dve", "reduce_sum", reads=[Bl], writes=[Bl], out=ls[:, 1:2], in_=lt[:, 64:128], axis=AX.X)
            P.op("act", "activation", reads=[Bl], writes=[Bl], out=ls[:, 0:2], in_=ls[:, 0:2], func=AF.Exp)
            P.op("dve", "tensor_tensor", reads=[Bl], writes=[Bl], out=ls[:, 2:3], in0=ls[:, 1:2], in1=ls[:, 0:1], op=ALU.subtract)
            P.op("dve", "tensor_scalar", reads=[Bl], writes=[Bl], out=ls[:, 3:4], in0=ls[:, 2:3], scalar1=-lambda_init, scalar2=None, op0=ALU.add)
            b = E.bank()
            P.op("pe", "matmul", reads=[Bl], writes=[E.psb[b]], out=E.ps[:, b, 0:1], lhsT=ones1[:], rhs=ls[:, 3:4], start=True, stop=True)
            P.op("dve", "tensor_copy", reads=[E.psb[b]], writes=[Bl], out=nl[:], in_=E.ps[:, b, 0:1])
            P.op("dve", "tensor_scalar", reads=[Bl], writes=[Bl], out=gsc[:], in0=gsc[:], scalar1=(1.0 - lambda_init), scalar2=None, op0=ALU.mult)

            with phase() as ph:
                NKS = 3
                kt = [E.sb("kt", [65, NKEY], BF16, ph) for i in range(NKS)]
                vt = [E.sb("vt", [128, NKB, 128], BF16, ph) for i in range(2)]
                qt = [E.sb("qt", [65, 2, NQ], BF16, ph) for i in range(1)]
                Bk = [Buf("kt%d" % i) for i in range(NKS)]
                Bv = [Buf("vt%d" % i) for i in range(2)]
                Bq = [Buf("qt%d" % i) for i in range(1)]
                Baug = Buf("aug")
                for i in range(NKS):
                    P.op("pool", "memset", writes=[Baug], ap=kt[i][64:65, :], constant=1.0)
                for i in range(1):
                    P.op("pool", "memset", writes=[Baug], ap=qt[i][64:65, :, :], constant=0.0)
                accD = [E.sb("accD", [128, 512], F32, ph) for i in range(2)]
                accP = [E.sb("accP", [128, 512], F32, ph) for i in range(2)]
                BaD = [Buf("accD%d" % i) for i in range(2)]
                BaP = [Buf("accP%d" % i) for i in range(2)]
                ones32 = E.sb("ones32", [128, 128], F32, ph)
                Bo32 = Buf("ones32")
                P.op("pool", "memset", writes=[Bo32], ap=ones32[:], constant=1.0)
                pT = [E.sb("pT", [128, 512], BF16, ph) for i in range(4)]
                BpT = [Buf("pT%d" % i) for i in range(4)]
                rr = [E.sb("rr", [128, 512], F32, ph) for i in range(2)]
                Brr = [Buf("rr%d" % i) for i in range(2)]
                osq = E.sb("osq", [128, 512], BF16, ph)
                Bosq = Buf("osq")
                GVv = [sc["GV0"].rearrange("(j p) c -> p j c", p=128), sc["GV1"].rearrange("(j p) c -> p j c", p=128)]
                vbcv = sc["vbc"].rearrange("(j p) c -> p j c", p=128)

                def load_map(m):
                    s = m % NKS
                    P.dma("sp", kt[s][0:64, 0:LC], sc["kbc"][m], reads=[sc["Bkbc"]], writes=[Bk[s]])
                    for r in range(4):
                        P.dma("sp", kt[s][0:64, LC + r * NL:LC + (r + 1) * NL], sc["GB%d" % (m // 4)][r * 256 + (m % 4) * 64:r * 256 + (m % 4 + 1) * 64, :],
                              reads=[sc["BGB%d" % (m // 4)]], writes=[Bk[s]])

                def load_vq(dh):
                    s = dh % 2
                    P.dma("sp", vt[s][:, 0:2, :], vbcv[:, :, dh * 128:(dh + 1) * 128], reads=[sc["Bvbc"]], writes=[Bv[s]])
                    P.dma("sp", vt[s][:, 2:NKB, :], GVv[dh // 2][:, :, (dh % 2) * 128:(dh % 2 + 1) * 128], reads=[sc["BGV%d" % (dh // 2)]], writes=[Bv[s]])

                def load_q(dh):
                    for mi in range(2):
                        P.dma("sp", qt[0][0:64, mi, :], sc["qb"][2 * dh + mi][:, 0:NQ], reads=[sc["Bqb"]], writes=[Bq[0]])

                qtiles = [(t0, w, NKB) for (t0, w) in tiles_of(NL)] + ([(NL, LC, 2)] if with_ctx else [])
                OB = (0, 1)
                ZB = (2, 3)
                SBK = (4, 5, 6, 7)
                load_map(0)
                load_map(1)
                load_vq(0)
                load_q(0)
                for dh in range(4):
                    s = dh % 2
                    if dh + 1 < 4:
                        load_map(2 * dh + 2)
                        load_vq(dh + 1)
                    for qi, (q0, qw, nkb) in enumerate(qtiles):
                        if dh + 1 < 4 and qi == len(qtiles) - 1:
                            pass
                        steps = [(kb, mi) for kb in range(nkb) for mi in range(2)]

                        def emit_S(i):
                            kb, mi = steps[i]
                            sbk = SBK[i % 4]
                            ks = (2 * dh + mi) % NKS
                            P.op("pe", "matmul", reads=[Bk[ks], Bq[0], Baug], writes=[E.psb[sbk]], out=E.ps[:, sbk, 0:qw], lhsT=kt[ks][0:65, kb * 128:(kb + 1) * 128],
                                 rhs=qt[0][0:65, mi, q0:q0 + qw], start=True, stop=True)

                        emit_S(0)
                        emit_S(1)
                        for i, (kb, mi) in enumerate(steps):
                            sbk = SBK[i % 4]
                            pi = i % 4
                            P.op("act", "activation", reads=[E.psb[sbk]], writes=[BpT[pi]], out=pT[pi][:, 0:qw], in_=E.ps[:, sbk, 0:qw], func=AF.Exp, scale=0.125)
                            if i + 2 < len(steps):
                                emit_S(i + 2)
                            first = (kb == 0)
                            last = (kb == nkb - 1)
                            P.op("pe", "matmul", reads=[Bv[s], BpT[pi]], writes=[E.psb[OB[mi]]], out=E.ps[:, OB[mi], 0:qw], lhsT=vt[s][:, kb, :], rhs=pT[pi][:, 0:qw], start=first, stop=last)
                            if kb % 2 == 0:
                                if kb == 0:
                                    P.op("dve", "tensor_copy", reads=[BpT[pi]], writes=[BaD[mi]], out=accD[mi][:, 0:qw], in_=pT[pi][:, 0:qw])
                                else:
                                    P.op("dve", "tensor_tensor", reads=[BpT[pi], BaD[mi]], writes=[BaD[mi]], out=accD[mi][:, 0:qw], in0=accD[mi][:, 0:qw], in1=pT[pi][:, 0:qw], op=ALU.add)
                            else:
                                P.op("pe", "matmul", reads=[Bones, BpT[pi]], writes=[E.psb[ZB[mi]]], out=E.ps[:, ZB[mi], 0:qw], lhsT=ones[:], rhs=pT[pi][:, 0:qw], start=(kb == 1), stop=False)
                        for mi in range(2):
                            P.op("pe", "matmul", reads=[Bo32, BaD[mi]], writes=[E.psb[ZB[mi]]], out=E.ps[:, ZB[mi], 0:qw], lhsT=ones32[:], rhs=accD[mi][:, 0:qw], start=False, stop=True)
                        for mi in range(2):
                            P.op("dve", "reciprocal", reads=[E.psb[ZB[mi]]], writes=[Brr[mi]], out=rr[mi][:, 0:qw], in_=E.ps[:, ZB[mi], 0:qw])
                            P.op("dve", "tensor_tensor", reads=[E.psb[OB[mi]], Brr[mi]], writes=[Brr[mi]], out=rr[mi][:, 0:qw], in0=E.ps[:, OB[mi], 0:qw], in1=rr[mi][:, 0:qw], op=ALU.mult)
                        P.op("dve", "scalar_tensor_tensor", reads=[Brr[0], Brr[1], Bl], writes=[Brr[0]], out=rr[0][:, 0:qw], in0=rr[1][:, 0:qw], scalar=nl[:, 0:1], in1=rr[0][:, 0:qw],
                             op0=ALU.mult, op1=ALU.add)
                        P.op("act", "activation", reads=[Brr[0]], writes=[Bosq], out=osq[:, 0:qw], in_=rr[0][:, 0:qw], func=AF.Square)
                        bz = ZB[0]
                        P.op("pe", "matmul", reads=[Bones, Bosq], writes=[E.psb[bz]], out=E.ps[:, bz, 0:qw], lhsT=ones[:], rhs=osq[:, 0:qw], start=True, stop=True)
                        P.op("act", "activation", reads=[E.psb[bz]], writes=[Brr[1]], out=rr[1][:, 0:qw], in_=E.ps[:, bz, 0:qw], func=AF.Sqrt, bias=EPS, scale=1.0 / 128)
                        P.op("dve", "reciprocal", reads=[Brr[1]], writes=[Brr[1]], out=rr[1][:, 0:qw], in_=rr[1][:, 0:qw])
                        P.op("dve", "scalar_tensor_tensor", reads=[Brr[0], Brr[1], Bl], writes=[Bmixd[dh]], out=mixd[:, dh, q0:q0 + qw], in0=rr[0][:, 0:qw], scalar=gsc[:, 0:1], in1=rr[1][:, 0:qw],
                             op0=ALU.mult, op1=ALU.mult)
                    if 2 * dh + 3 < 8:
                        load_map(2 * dh + 3)
                    if dh + 1 < 4:
                        load_q(dh + 1)
                prog_barrier(P)

            mixn = E.sb("mixn", [64, 4, NQ], BF16, ph0)
            Bmixn = [Buf("mixn%d" % i) for i in range(4)]
            with phase() as ph:
                qa = E.sb("qa", [128, 2, NQ], BF16, ph)
                kvac = E.sb("kvac", [128, 2, 512], BF16, ph)
                kacT = E.sb("kacT", [128, 2, LC], BF16, ph)
                Bna, Bkvac, BkacT = Buf("na_in"), Buf("kvac"), Buf("kacT")
                P.dma("sp", qa[:], sc["qa"][:, :, 0:NQ], reads=[sc["Bqa"]], writes=[Bna])
                P.dma("sp", kvac[:], sc["kvac"].rearrange("(j p) c -> p j c", p=128), reads=[sc["Bkvac"]], writes=[Bkvac])
                bt = E.bank()
                for j in range(2):
                    for hp in range(2):
                        P.op("pe", "transpose", reads=[Bkvac, Bid], writes=[E.psb[bt]], out=ps_bf[:, bt, hp * 256 + j * 128:hp * 256 + (j + 1) * 128], in_=kvac[:, j, hp * 128:(hp + 1) * 128], identity=idb[:])
                P.op("act", "activation", reads=[E.psb[bt]], writes=[BkacT], out=kacT[:].rearrange("p a b -> p (a b)"), in_=ps_bf[:, bt, 0:512], func=AF.Copy)
                kvwin = [E.sb("kwin", [128, 4, 256], BF16, ph) for i in range(2)]
                vwin = [E.sb("vwin", [128, 4, 256], BF16, ph) for i in range(2)]
                kwT = [E.sb("kwT", [128, 2, 512], BF16, ph) for i in range(2)]
                nab = [E.sb("nab", [64, 8, 4, 64], F32, ph) for i in range(2)]
                Bkv = [Buf("kvwin%d" % i) for i in range(2)]
                BkwT = [Buf("kwT%d" % i) for i in range(2)]
                Bnab = [Buf("nab%d" % i) for i in range(2)]
                sbuf_s = [E.sb("nas", [64, 768], F32, ph) for i in range(2)]
                Bs = [Buf("nas%d" % i) for i in range(2)]
                p32 = [E.sb("nap", [64, 768], F32, ph) for i in range(2)]
                Bp32 = [Buf("nap%d" % i) for i in range(2)]
                pn = [E.sb("napn", [64, 768], BF16, ph) for i in range(2)]
                Bpn = [Buf("napn%d" % i) for i in range(2)]
                ptsb = [E.sb("napt", [128, 384], BF16, ph) for i in range(2)]
                Bpt = [Buf("napt%d" % i) for i in range(2)]
                st4 = [E.sb("nast", [64, 4], F32, ph) for i in range(2)]
                Bst4 = [Buf("nast%d" % i) for i in range(2)]
                GAK, GAV = sc["GAK"], sc["GAV"]
                idxt = E.sb("idxt", [128, 64], I32, ph)
                Bidx = Buf("idxt")
                P.dma("sp", idxt[:], idxtab_d, writes=[Bidx])

                def load_row(rl):
                    s = rl % 2
                    P.custom("pool", (lambda eng, rl=rl, s=s: eng.indirect_dma_start(
                        out=kvwin[s][:].rearrange("p a b -> p (a b)"), out_offset=None, in_=GAK[:, :],
                        in_offset=bass.IndirectOffsetOnAxis(ap=idxt[:, rl:rl + 1], axis=0))), reads=[sc["BGAK"], Bidx], writes=[Bkv[s]], dma=True)
                    P.custom("pool", (lambda eng, rl=rl, s=s: eng.indirect_dma_start(
                        out=vwin[s][:].rearrange("p a b -> p (a b)"), out_offset=None, in_=GAV[:, :],
                        in_offset=bass.IndirectOffsetOnAxis(ap=idxt[:, rl:rl + 1], axis=0))), reads=[sc["BGAV"], Bidx], writes=[Bkv[s]], dma=True)
                    P.custom("pool", (lambda eng, rl=rl, s=s: eng.indirect_dma_start(
                        out=nab[s][:].rearrange("p a b c -> p (a b c)"), out_offset=None, in_=bc_d[l][:, :],
                        in_offset=bass.IndirectOffsetOnAxis(ap=idxt[0:64, 32 + rl:32 + rl + 1], axis=0))), reads=[Bidx], writes=[Bnab[s]], dma=True)
                    bt = E.bank()
                    for j in range(4):
                        for hp in range(2):
                            P.op("pe", "transpose", reads=[Bkv[s], Bid], writes=[E.psb[bt]], out=ps_bf[:, bt, hp * 512 + j * 128:hp * 512 + (j + 1) * 128], in_=kvwin[s][:, j, hp * 128:(hp + 1) * 128], identity=idb[:])
                    P.op("act", "activation", reads=[E.psb[bt]], writes=[BkwT[s]], out=kwT[s][:].rearrange("p a b -> p (a b)"), in_=ps_bf[:, bt, 0:1024], func=AF.Copy)

                it = [0]

                def na_block(q0, h, rl):
                    i = it[0] % 2
                    it[0] += 1
                    hp, hb = h // 2, (h % 2) * 64
                    nk = 768 if rl is not None else 256
                    lhs = qa[hb:hb + 64, hp, q0:q0 + 64]
                    if rl is not None:
                        s = rl % 2
                        ba = E.bank()
                        P.op("pe", "matmul", reads=[Bna, BkwT[s]], writes=[E.psb[ba]], out=E.ps[0:64, ba, 0:512], lhsT=lhs, rhs=kwT[s][hb:hb + 64, hp, :], start=True, stop=True)
                        for jj in range(4):
                            P.op("dve", "scalar_tensor_tensor", reads=[E.psb[ba], Bnab[s]], writes=[Bs[i]],
                                 out=sbuf_s[i][:, jj * 128:(jj + 1) * 128].rearrange("p (a m) -> p a m", a=8),
                                 in0=E.ps[0:64, ba, jj * 128:(jj + 1) * 128].rearrange("p (a m) -> p a m", a=8), scalar=0.125,
                                 in1=nab[s][:, :, h, jj:64:4], op0=ALU.mult, op1=ALU.add)
                        c0 = 512
                    else:
                        c0 = 0
                    bb = E.bank()
                    P.op("pe", "matmul", reads=[Bna, BkacT], writes=[E.psb[bb]], out=E.ps[0:64, bb, 0:256], lhsT=lhs, rhs=kacT[hb:hb + 64, hp, :], start=True, stop=True)
                    P.op("act", "activation", reads=[E.psb[bb]], writes=[Bs[i]], out=sbuf_s[i][:, c0:c0 + 256], in_=E.ps[0:64, bb, 0:256], func=AF.Copy, scale=0.125)
                    P.op("dve", "reduce_max", reads=[Bs[i]], writes=[Bst4[i]], out=st4[i][:, 0:1], in_=sbuf_s[i][:, 0:nk], axis=AX.X)
                    P.op("dve", "tensor_scalar", reads=[Bst4[i]], writes=[Bst4[i]], out=st4[i][:, 1:2], in0=st4[i][:, 0:1], scalar1=-1.0, scalar2=None, op0=ALU.mult)
                    P.op("act", "activation", reads=[Bs[i], Bst4[i]], writes=[Bp32[i]], out=p32[i][:, 0:nk], in_=sbuf_s[i][:, 0:nk], func=AF.Exp, bias=st4[i][:, 1:2], scale=1.0)
                    P.op("dve", "reduce_sum", reads=[Bp32[i]], writes=[Bst4[i]], out=st4[i][:, 2:3], in_=p32[i][:, 0:nk], axis=AX.X)
                    P.op("dve", "reciprocal", reads=[Bst4[i]], writes=[Bst4[i]], out=st4[i][:, 3:4], in_=st4[i][:, 2:3])
                    P.op("dve", "tensor_scalar", reads=[Bp32[i], Bst4[i]], writes=[Bpn[i]], out=pn[i][:, 0:nk], in0=p32[i][:, 0:nk], scalar1=st4[i][:, 3:4], scalar2=None, op0=ALU.mult)
                    nb = nk // 128
                    bt = E.bank()
                    for j in range(nb):
                        P.op("pe", "transpose", reads=[Bpn[i], Bid], writes=[E.psb[bt]], out=ps_bf[:, bt, j * 64:(j + 1) * 64], in_=pn[i][:, j * 128:(j + 1) * 128], identity=idb[0:64, 0:64])
                    P.op("act", "activation", reads=[E.psb[bt]], writes=[Bpt[i]], out=ptsb[i][:, 0:nb * 64], in_=ps_bf[:, bt, 0:nb * 64], func=AF.Copy)
                    bo = E.bank()
                    for j in range(nb):
                        if rl is not None and j < 4:
                            lv = vwin[rl % 2][:, j, h * 64:(h + 1) * 64]
                            rd = [Bkv[rl % 2], Bpt[i]]
                        else:
                            lv = kvac[:, j - (4 if rl is not None else 0), 256 + h * 64:256 + (h + 1) * 64]
                            rd = [Bkvac, Bpt[i]]
                        P.op("pe", "matmul", reads=rd, writes=[E.psb[bo]], out=E.ps[0:64, bo, 0:64], lhsT=lv, rhs=ptsb[i][:, j * 64:(j + 1) * 64], start=(j == 0), stop=(j == nb - 1))
                    P.op("dve", "tensor_copy", reads=[E.psb[bo]], writes=[Bmixn[h]], out=mixn[0:64, h, q0:q0 + 64], in_=E.ps[0:64, bo, 0:64])

                load_row(0)
                for rl in range(32):
                    if rl + 1 < 32:
                        load_row(rl + 1)
                    for h in range(4):
                        na_block(rl * 64, h, rl)
                if with_ctx:
                    for qb in range(4):
                        for h in range(4):
                            na_block(NL + qb * 64, h, None)
                prog_barrier(P)

            with phase() as ph:
                mixc = E.sb("mixc", [128, 2, NQ], BF16, ph)
                Bmixc = [Buf("mixc%d" % i) for i in range(2)]
                cg = E.sb("cgx", [128, 2, NT + 4], F32, ph)
                bg = E.sb("bg", [128, 2, NT], F32, ph)
                cw = E.sb("cw", [128, 2, 3], F32, ph)
                ca = E.sb("ca", [128, NL], F32, ph)
                eg = E.sb("eg", [128, 4, 2, 2], BF16, ph)
                selh = E.sb("selh", [128, 8], F32, ph)
                egt = E.sb("egt", [128, 4], F32, ph)
                Bcv, Bca, Beg = Buf("conv_in"), Buf("ca"), Buf("eg")
                P.op("pool", "memset", writes=[Bcv], ap=cg[:], constant=0.0)
                P.dma("sp", cg[:, :, 1:NL + 1], sc["cgx"][:, :, 0:NL], reads=[sc["Bcgx"]], writes=[Bcv])
                P.dma("sp", cg[:, :, NL + 3:NL + 3 + LC], sc["cgx"][:, :, NL:NT], reads=[sc["Bcgx"]], writes=[Bcv])
                P.dma("sp", bg[:], sc["bg"], reads=[sc["Bbg"]], writes=[Bcv])
                P.dma("sp", cw[:], convw_d[l], writes=[Bcv])
                P.dma("sp", selh[:], selh_d, writes=[Beg])
                for r in range(4):
                    P.dma("sp", eg[:, r, :, :], sc["GE"][r * 4, 0:512].rearrange("(c p j) -> p c j", c=2, p=128, j=2), reads=[sc["BGE"]], writes=[Beg],
                          allow_slow_non_contiguous=True)
                for c in range(2):
                    P.op("dve", "tensor_tensor", reads=[Beg], writes=[Beg], out=egt[:], in0=eg[:, :, c, 1], in1=selh[:, 0:4], op=ALU.mult)
                    P.op("dve", "reduce_sum", reads=[Beg, Bcv], writes=[Bcv], out=cg[:, c, 0:1], in_=egt[:], axis=AX.X)
                    P.op("dve", "tensor_tensor", reads=[Beg], writes=[Beg], out=egt[:], in0=eg[:, :, c, 0], in1=selh[:, 4:8], op=ALU.mult)
                    P.op("dve", "reduce_sum", reads=[Beg, Bcv], writes=[Bcv], out=cg[:, c, NL + 1:NL + 2], in_=egt[:], axis=AX.X)
                segs = [(0, 0, NL)] + ([(NL + 2, NL, LC)] if with_ctx else [])
                for c in range(2):
                    for (u0, o0, n) in segs:
                        P.op("dve", "tensor_scalar", reads=[Bcv], writes=[Bca], out=ca[:, 0:n], in0=cg[:, c, u0 + 1:u0 + 1 + n], scalar1=cw[:, c, 1:2], scalar2=None, op0=ALU.mult)
                        P.op("dve", "scalar_tensor_tensor", reads=[Bcv, Bca], writes=[Bca], out=ca[:, 0:n], in0=cg[:, c, u0:u0 + n], scalar=cw[:, c, 0:1], in1=ca[:, 0:n],
                             op0=ALU.mult, op1=ALU.add)
                        P.op("dve", "scalar_tensor_tensor", reads=[Bcv, Bca], writes=[Bca], out=ca[:, 0:n], in0=cg[:, c, u0 + 2:u0 + 2 + n], scalar=cw[:, c, 2:3], in1=ca[:, 0:n],
                             op0=ALU.mult, op1=ALU.add)
                        P.op("dve", "tensor_tensor", reads=[Bcv, Bca], writes=[Bmixc[c]], out=mixc[:, c, o0:o0 + n], in0=ca[:, 0:n], in1=bg[:, c, o0:o0 + n], op=ALU.mult)
                wna = E.sb("wna", [64, 4, D], BF16, ph)
                wr = E.sb("wr", [128, 6, D], BF16, ph)
                Bw = Buf("wout")
                P.dma("pool", wna[:], wna_d[l], writes=[Bw])
                P.dma("pool", wr[:], wor_d[l], writes=[Bw])
                for ti, (t0, w) in enumerate(tiles_of(NQ)):
                    j = 0 if t0 < NL else 1
                    for m in range(KC):
                        b = E.bank()
                        for kc in range(10):
                            if kc < 4:
                                lh, rh, Br = wna[:, kc, m * 128:(m + 1) * 128], mixn[0:64, kc, t0:t0 + w], Bmixn[kc]
                            elif kc < 8:
                                lh, rh, Br = wr[:, kc - 4, m * 128:(m + 1) * 128], mixd[:, kc - 4, t0:t0 + w], Bmixd[kc - 4]
                            else:
                                lh, rh, Br = wr[:, kc - 4, m * 128:(m + 1) * 128], mixc[:, kc - 8, t0:t0 + w], Bmixc[kc - 8]
                            P.op("pe", "matmul", reads=[Bw, Br], writes=[E.psb[b]], out=E.ps[:, b, 0:w], lhsT=lh, rhs=rh, start=(kc == 0), stop=(kc == 9))
                        P.op("dve", "scalar_tensor_tensor", reads=[E.psb[b], Bmod, Bx[ti]], writes=[Bx[ti]], out=x[:, m, t0:t0 + w], in0=E.ps[:, b, 0:w], scalar=mo[:, l, 16 + m, j:j + 1],
                             in1=x[:, m, t0:t0 + w], op0=ALU.mult, op1=ALU.add)
                prog_barrier(P)

    def stage_L3(l, NTK, moe, final):
        G = 2
        F = D_EXP if moe else D_FF
        NE = NEXP if moe else 1
        w1, w3, w2 = (mw1_d, mw3_d, mw2_d) if moe else (fw1_d, fw3_d, fw2_d)
        tlk = tiles_of(NTK)
        with phase() as ph0:
            hT = E.sb("hT3", [128, KC, NTK], BF16, ph0)
            Bh = [Buf("h%d" % i) for i in range(len(tlk))]
            if moe:
                gatesT = E.sb("gatesT", [8, NTK], F32, ph0)
                Bgt = Buf("gatesT")
                sel = E.sb("sel", [8, 8, 128], F32, ph0)
                Bc = Buf("moeconst")
                P.dma("sp", sel[:], sel_d, writes=[Bc])
            with phase() as ph:
                scr = norm_scr(ph)
                if moe:
                    h32 = E.sb("h32", [128, KC, 512], F32, ph)
                    Bh32 = Buf("h32")
                    wr = E.sb("wrt", [128, KC, NEXP], F32, ph)
                    ident = E.sb("ident", [128, 128], F32, ph)
                    P.dma("sp", wr[:], wrt_d, writes=[Bc])
                    P.dma("sp", ident[:], ident_d, writes=[Bc])
                    lg = E.sb("lg", [128, 8], F32, ph)
                    l2 = E.sb("l2", [128, 8], F32, ph)
                    ew = E.sb("ew", [128, 8], F32, ph)
                    sm = E.sb("sm", [128, 8], F32, ph)
                    Bg = Buf("gate_scr")
                for ti, (t0, w) in enumerate(tlk):
                    j = 0 if t0 < NL else 1
                    emit_norm_mod_tile(E, ones, Bones, x[:, :, t0:t0 + w], Bx[ti], w, mo[:, l, 24:32, j], sc1[:, l, 1, :, j], Bmod,
                                       hT[:, :, t0:t0 + w], Bh[ti], scr,
                                       h32dst=(h32[:, :, 0:w] if moe else None), Bh32=(Bh32 if moe else None))
                    if moe:
                        for blk in range(w // 128):
                            b = E.bank()
                            for k in range(KC):
                                P.op("pe", "matmul", reads=[Bh32, Bc], writes=[E.psb[b]], out=E.ps[:, b, 0:8], lhsT=h32[:, k, blk * 128:(blk + 1) * 128], rhs=wr[:, k, :],
                                     start=(k == 0), stop=(k == KC - 1))
                            P.op("dve", "tensor_copy", reads=[E.psb[b]], writes=[Bg], out=lg[:], in_=E.ps[:, b, 0:8])
                            P.op("dve", "reduce_max", reads=[Bg], writes=[Bg], out=sm[:, 0:1], in_=lg[:], axis=AX.X)
                            P.op("dve", "tensor_scalar", reads=[Bg], writes=[Bg], out=sm[:, 1:2], in0=sm[:, 0:1], scalar1=-1.0, scalar2=None, op0=ALU.mult)
                            P.op("dve", "tensor_scalar", reads=[Bg], writes=[Bg], out=l2[:], in0=lg[:], scalar1=sm[:, 0:1], scalar2=-1e30, op0=ALU.is_equal, op1=ALU.mult)
                            P.op("dve", "tensor_tensor", reads=[Bg], writes=[Bg], out=l2[:], in0=l2[:], in1=lg[:], op=ALU.add)
                            P.op("dve", "reduce_max", reads=[Bg], writes=[Bg], out=sm[:, 2:3], in_=l2[:], axis=AX.X)
                            P.op("dve", "tensor_scalar", reads=[Bg], writes=[Bg], out=l2[:], in0=lg[:], scalar1=sm[:, 2:3], scalar2=None, op0=ALU.is_ge)
                            P.op("act", "activation", reads=[Bg], writes=[Bg], out=ew[:], in_=lg[:], func=AF.Exp, bias=sm[:, 1:2], scale=1.0)
                            P.op("dve", "tensor_tensor", reads=[Bg], writes=[Bg], out=ew[:], in0=ew[:], in1=l2[:], op=ALU.mult)
                            P.op("dve", "reduce_sum", reads=[Bg], writes=[Bg], out=sm[:, 3:4], in_=ew[:], axis=AX.X)
                            P.op("dve", "reciprocal", reads=[Bg], writes=[Bg], out=sm[:, 4:5], in_=sm[:, 3:4])
                            P.op("dve", "tensor_scalar", reads=[Bg], writes=[Bg], out=ew[:], in0=ew[:], scalar1=sm[:, 4:5], scalar2=None, op0=ALU.mult)
                            b2 = E.bank()
                            P.op("pe", "transpose", reads=[Bg, Bc], writes=[E.psb[b2]], out=E.ps[0:8, b2, 0:128], in_=ew[:], identity=ident[:])
                            c0 = t0 + blk * 128
                            P.op("dve", "tensor_copy", reads=[E.psb[b2]], writes=[Bgt], out=gatesT[:, c0:c0 + 128], in_=E.ps[0:8, b2, 0:128])
                prog_barrier(P)
            with phase() as ph:
                NS = 3
                w1s = [E.sb("w1s", [128, KC, 128 * G], BF16, ph) for i in range(NS)]
                w3s = [E.sb("w3s", [128, KC, 128 * G], BF16, ph) for i in range(NS)]
                w2s = [E.sb("w2s", [128, G, D], BF16, ph) for i in range(NS)]
                Bws = [Buf("ws%d" % i) for i in range(NS)]
                ut = [E.sb("ut", [128, G, 512], BF16, ph) for i in range(2)]
                But = [Buf("ut%d" % i) for i in range(2)]
                st_ = [E.sb("st", [128, 512], F32, ph) for i in range(2)]
                Bst = [Buf("st%d" % i) for i in range(2)]
                tt_ = [E.sb("tt", [128, 512], F32, ph) for i in range(2)]
                Btt = [Buf("tt%d" % i) for i in range(2)]
                if moe:
                    gbc = [E.sb("gbc", [128, NTK], F32, ph) for i in range(2)]
                    Bgbc = [Buf("gbc%d" % i) for i in range(2)]
                ngrp = F // (128 * G)
                groups = [(ex, g) for ex in range(NE) for g in range(ngrp)]

                def load_group(gi):
                    ex, g = groups[gi]
                    s = gi % NS
                    P.dma("pool", w1s[s][:], w1[ex, :, :, g * 128 * G:(g + 1) * 128 * G], writes=[Bws[s]])
                    P.dma("pool", w3s[s][:], w3[ex, :, :, g * 128 * G:(g + 1) * 128 * G], writes=[Bws[s]])
                    P.dma("pool", w2s[s][:], w2[ex, :, g * G:(g + 1) * G, :], writes=[Bws[s]])

                load_group(0)
                if len(groups) > 1:
                    load_group(1)
                cnt = 0
                for gi, (ex, g) in enumerate(groups):
                    s = gi % NS
                    if gi + 2 < len(groups):
                        load_group(gi + 2)
                    if moe and g == 0:
                        gs = ex % 2
                        for ti, (t0, w) in enumerate(tlk):
                            b = E.bank()
                            P.op("pe", "matmul", reads=[Bc, Bgt], writes=[E.psb[b]], out=E.ps[:, b, 0:w], lhsT=sel[:, ex, :], rhs=gatesT[:, t0:t0 + w], start=True, stop=True)
                            P.op("act", "activation", reads=[E.psb[b]], writes=[Bgbc[gs]], out=gbc[gs][:, t0:t0 + w], in_=E.ps[:, b, 0:w], func=AF.Copy)
                    for ti, (t0, w) in enumerate(tlk):
                        ui = cnt % 2
                        cnt += 1
                        for fc in range(G):
                            ba = E.bank()
                            for k in range(KC):
                                P.op("pe", "matmul", reads=[Bws[s], Bh[ti]], writes=[E.psb[ba]], out=E.ps[:, ba, 0:w], lhsT=w1s[s][:, k, fc * 128:(fc + 1) * 128],
                                     rhs=hT[:, k, t0:t0 + w], start=(k == 0), stop=(k == KC - 1))
                            bb = E.bank()
                            for k in range(KC):
                                P.op("pe", "matmul", reads=[Bws[s], Bh[ti]], writes=[E.psb[bb]], out=E.ps[:, bb, 0:w], lhsT=w3s[s][:, k, fc * 128:(fc + 1) * 128],
                                     rhs=hT[:, k, t0:t0 + w], start=(k == 0), stop=(k == KC - 1))
                            si = fc % 2
                            P.op("act", "activation", reads=[E.psb[ba]], writes=[Bst[si]], out=st_[si][:, 0:w], in_=E.ps[:, ba, 0:w], func=AF.Silu)
                            if moe:
                                gs = ex % 2
                                P.op("dve", "tensor_tensor", reads=[E.psb[bb], Bgbc[gs]], writes=[Btt[si]], out=tt_[si][:, 0:w], in0=E.ps[:, bb, 0:w], in1=gbc[gs][:, t0:t0 + w], op=ALU.mult)
                                P.op("dve", "tensor_tensor", reads=[Btt[si], Bst[si]], writes=[But[ui]], out=ut[ui][:, fc, 0:w], in0=tt_[si][:, 0:w], in1=st_[si][:, 0:w], op=ALU.mult)
                            else:
                                P.op("dve", "tensor_tensor", reads=[E.psb[bb], Bst[si]], writes=[But[ui]], out=ut[ui][:, fc, 0:w], in0=E.ps[:, bb, 0:w], in1=st_[si][:, 0:w], op=ALU.mult)
                        j = 0 if t0 < NL else 1
                        for m in range(KC):
                            bc = E.bank()
                            for fc in range(G):
                                P.op("pe", "matmul", reads=[Bws[s], But[ui]], writes=[E.psb[bc]], out=E.ps[:, bc, 0:w], lhsT=w2s[s][:, fc, m * 128:(m + 1) * 128],
                                     rhs=ut[ui][:, fc, 0:w], start=(fc == 0), stop=(fc == G - 1))
                            P.op("dve", "scalar_tensor_tensor", reads=[E.psb[bc], Bmod, Bx[ti]], writes=[Bx[ti]], out=x[:, m, t0:t0 + w], in0=E.ps[:, bc, 0:w], scalar=mo[:, l, 40 + m, j:j + 1],
                                 in1=x[:, m, t0:t0 + w], op0=ALU.mult, op1=ALU.add)
                prog_barrier(P)
        if final:
            with phase() as ph:
                fg = E.sb("fg", [128, KC], F32, ph)
                sq = E.sb("fsq", [128, KC, 512], BF16, ph)
                rstd = E.sb("frstd", [128, 512], F32, ph)
                Bfg, Bsq, Brstd = Buf("fg"), Buf("fsq"), Buf("frstd")
                P.dma("sp", fg[:], fg_d, writes=[Bfg])
                for ti, (t0, w) in enumerate(tlk):
                    P.op("act", "activation", reads=[Bx[ti]], writes=[Bsq], out=sq[:, :, 0:w], in_=x[:, :, t0:t0 + w], func=AF.Square)
                    b = E.bank()
                    for k in range(KC):
                        P.op("pe", "matmul", reads=[Bones, Bsq], writes=[E.psb[b]], out=E.ps[:, b, 0:w], lhsT=ones[:], rhs=sq[:, k, 0:w], start=(k == 0), stop=(k == KC - 1))
                    P.op("act", "activation", reads=[E.psb[b]], writes=[Brstd], out=rstd[:, 0:w], in_=E.ps[:, b, 0:w], func=AF.Sqrt, bias=EPS, scale=1.0 / D)
                    P.op("dve", "reciprocal", reads=[Brstd], writes=[Brstd], out=rstd[:, 0:w], in_=rstd[:, 0:w])
                    for k in range(KC):
                        P.op("dve", "scalar_tensor_tensor", reads=[Bx[ti], Brstd, Bfg], writes=[Bx[ti]], out=x[:, k, t0:t0 + w], in0=x[:, k, t0:t0 + w], scalar=fg[:, k:k + 1], in1=rstd[:, 0:w],
                             op0=ALU.mult, op1=ALU.mult)
                    P.dma("sp", xo_d[:, :, t0:t0 + w], x[:, :, t0:t0 + w], reads=[Bx[ti]], is_output=True)

    stages = [lambda: stage_L1(0), lambda: stage_L2(0, True), lambda: stage_L3(0, NT, False, False),
              lambda: stage_L1(1), lambda: stage_L2(1, False), lambda: stage_L3(1, NL, True, True)]
    for i in range(nstages):
        stages[i]()
    if nstages < 6:
        for ti, (t0, w) in enumerate(tiles_of(NL)):
            P.dma("sp", xo_d[:, :, t0:t0 + w], x[:, :, t0:t0 + w], reads=[Bx[ti]], is_output=True)
    return E.done()


def fm(a):
    T = a.shape[0]
    return np.ascontiguousarray(a.T.reshape(-1, 128, T).transpose(1, 0, 2))


def unfm(a):
    return np.ascontiguousarray(a.transpose(1, 0, 2).reshape(-1, a.shape[2]).T)


def wl(w):
    K, C = w.shape
    return np.ascontiguousarray(w.reshape(K // 128, 128, C).transpose(1, 0, 2))


def rope_tables(q):
    t = np.arange(q * NL, (q + 1) * NL, dtype=np.int32)
    row = (t // GRID_W).astype(np.float32)
    col = (t % GRID_W).astype(np.float32)
    inv = (np.float32(10000.0) ** (-np.arange(16, dtype=np.float32) / np.float32(16))).astype(np.float32)
    ang = np.concatenate([row[:, None] * inv, col[:, None] * inv], axis=-1).astype(np.float32)
    c = np.cos(ang).astype(np.float32).T
    s = np.sin(ang).astype(np.float32).T
    return np.ascontiguousarray(np.concatenate([c, c], 0)), np.ascontiguousarray(np.concatenate([-s, s], 0))


def rot_cols(w):
    return np.ascontiguousarray(w.reshape(w.shape[0], -1, 2, 32)[:, :, ::-1, :].reshape(w.shape[0], -1))


def make_wall(W):
    qB, kB = W[:, 256:768], W[:, 1024:1536]
    return wl(np.concatenate([W[:, 768:1024], W[:, 1536:1792], kB, rot_cols(kB), W[:, 1792:2304], qB, rot_cols(qB),
                              W[:, 0:256], W[:, 2560:2816], W[:, 2304:2560], W[:, 2816:3072]], axis=1))


def na_bias_table(rpb):
    qc = np.arange(64)
    kc = np.arange(64)
    win_start = np.clip(qc - 8, 0, 48)
    valid = (kc[None, :] >= win_start[:, None]) & (kc[None, :] < win_start[:, None] + 16)
    dcol = np.clip(kc[None, :] - qc[:, None] + 15, 0, 30)
    t = rpb[:, :, dcol]
    t = np.where(valid[None, None], t, np.float32(-1e30)).astype(np.float32)
    return np.ascontiguousarray(t.transpose(2, 1, 0, 3).reshape(64 * 15, 256))


def build_in_maps(inp, nstages=6):
    adaw = np.ascontiguousarray(inp["ada_w"].reshape(2, 8, 128, 6144).transpose(0, 2, 1, 3))
    adab = np.ascontiguousarray(inp["ada_b"].reshape(2, 48, 128).transpose(0, 2, 1))
    wall = np.stack([make_wall(inp["w_in"][l]) for l in range(2)])
    nabc = np.stack([na_bias_table(inp["na_rpb"][l]) for l in range(2)])
    convw = np.stack([np.ascontiguousarray(inp["conv_w"][l].T.reshape(2, 128, 3).transpose(1, 0, 2)) for l in range(2)])
    wna = np.stack([np.ascontiguousarray(inp["w_out"][l][0:256].reshape(4, 64, D).transpose(1, 0, 2)) for l in range(2)])
    wor = np.stack([wl(inp["w_out"][l][256:]) for l in range(2)])
    dlam = np.ascontiguousarray(inp["diff_lambda"].reshape(2, 1, 256))
    subln = np.ascontiguousarray(inp["diff_subln"].reshape(2, 128, 1))
    identb = np.eye(128, dtype=np.float32).astype(NPBF)
    fw1 = wl(inp["ffn_w1"][0])[None]
    fw3 = wl(inp["ffn_w3"][0])[None]
    fw2 = wl(inp["ffn_w2"][0])[None]
    NEI = NEXP if nstages >= 6 else 1
    mw1 = np.stack([wl(inp["moe_w1"][0, e]) for e in range(NEI)])
    mw3 = np.stack([wl(inp["moe_w3"][0, e]) for e in range(NEI)])
    mw2 = np.stack([wl(inp["moe_w2"][0, e]) for e in range(NEI)])
    wrt = wl(inp["router_w"][0])
    sel = np.zeros((8, 8, 128), np.float32)
    for e in range(8):
        sel[e, e, :] = 1.0
    ident = np.eye(128, dtype=np.float32)
    fg = np.ascontiguousarray(inp["final_gain"].reshape(8, 128).T)
    ropes = [rope_tables(q) for q in range(4)]
    maps = []
    for core in range(NCORES):
        b, q = core // 4, core % 4
        cT = np.ascontiguousarray(np.stack([inp["c"][b], inp["c_ctx"]], -1).reshape(8, 128, 2).transpose(1, 0, 2))
        r = 32 * q + np.arange(32)
        rs_ = np.clip(r - 4, 0, 120)
        idxtab = np.zeros((128, 64), np.int32)
        pp = np.arange(128)
        for rl in range(32):
            idxtab[:, rl] = rs_[rl] * 64 + 4 * pp
            idxtab[:, 32 + rl] = (pp % 64) * 15 + (rs_[rl] - r[rl] + 7)
        selh = np.zeros((128, 8), np.float32)
        if q > 0:
            selh[:, q - 1] = 1.0
        if q < 3:
            selh[:, 4 + q + 1] = 1.0
        maps.append({
            "xT": fm(np.concatenate([inp["x"][b, q * NL:(q + 1) * NL], inp["ctx"][b]], 0)), "cT": cT, "adaw": adaw, "adab": adab, "wall": wall,
            "ropeC": ropes[q][0], "ropeS": ropes[q][1], "nabc0": nabc[0], "nabc1": nabc[1], "idxtab": idxtab, "selh": selh, "convw": convw, "wout_na": wna, "wout_r": wor,
            "dlam": dlam, "subln": subln, "identb": identb, "fw1": fw1, "fw3": fw3, "fw2": fw2, "mw1": mw1, "mw3": mw3, "mw2": mw2, "wrt": wrt,
            "sel": sel, "ident": ident, "fgain": fg,
        })
    return maps


_NC = []


def kernel(x, c, ctx, c_ctx, ada_w, ada_b, w_in, w_out, na_rpb, diff_lambda, diff_subln, conv_w,
           ffn_w1, ffn_w3, ffn_w2, router_w, moe_w1, moe_w3, moe_w2, final_gain):
    inp = {k: np.asarray(v) for k, v in dict(
        x=x, c=c, ctx=ctx, c_ctx=c_ctx, ada_w=ada_w, ada_b=ada_b, w_in=w_in, w_out=w_out, na_rpb=na_rpb,
        diff_lambda=diff_lambda, diff_subln=diff_subln, conv_w=conv_w, ffn_w1=ffn_w1, ffn_w3=ffn_w3, ffn_w2=ffn_w2,
        router_w=router_w, moe_w1=moe_w1, moe_w3=moe_w3, moe_w2=moe_w2, final_gain=final_gain).items()}
    import os
    nst = int(os.environ.get("KF_NSTAGES", "6"))
    if not _NC:
        _NC.append(build_fused(nst))
    res = run_bass_kernel_spmd(_NC[0], build_in_maps(inp, nst), core_ids=list(range(NCORES)))
    out = np.zeros((2, 4 * NL, D), np.float32)
    for core in range(NCORES):
        b, q = core // 4, core % 4
        out[b, q * NL:(q + 1) * NL] = unfm(np.asarray(res.results[core]["xo"]))
    return out
```

```python
import contextlib
import math
import numpy as np
import ml_dtypes
import concourse.bass as bass
import concourse.mybir as mybir
from concourse.bass_utils import run_bass_kernel_spmd

F32 = mybir.dt.float32
BF16 = mybir.dt.bfloat16
ALU = mybir.AluOpType
AF = mybir.ActivationFunctionType
AX = mybir.AxisListType
NPBF = ml_dtypes.bfloat16

ENG_NAMES = ("pe", "act", "dve", "pool", "sp")

D = 1024
KC = 8
NL = 2048
LC = 256
NCORES = 8
GRID_W = 64
EPS = 1e-6
D_FF = 2816
D_EXP = 3584
NEXP = 8


class Buf:
    __slots__ = ("name", "w", "r")

    def __init__(self, name=""):
        self.name = name
        self.w = None
        self.r = []


class Prog:
    N_DMA_SEMS = 24

    def __init__(self, nc):
        self.nc = nc
        self.ops = {e: [] for e in ENG_NAMES}
        self.seen = {e: {} for e in ENG_NAMES}
        self.dma_uses = [0] * self.N_DMA_SEMS
        self.dma_rr = {}
        self.out_tokens = []
        self.n_coll = 0

    def _dma_slot(self, q):
        half = self.N_DMA_SEMS // 2
        lo = half if q == "pool" else 0
        k = self.dma_rr.get(q, 0)
        self.dma_rr[q] = k + 1
        return lo + k % half

    def _collect(self, reads, writes):
        deps = []
        for b in reads:
            if b.w is not None:
                deps.append(b.w)
        for b in writes:
            if b.w is not None:
                deps.append(b.w)
            deps.extend(b.r)
        return deps

    def _filter(self, e, deps):
        seen = self.seen[e]
        out = {}
        for t in deps:
            if t[0] == "c":
                _, f, seq = t
                if f == e and e == "pe":
                    continue
                key = ("c", f)
                val = seq
            else:
                _, j, val = t
                key = (t[0], j)
            if seen.get(key, 0) >= val:
                continue
            if out.get(key, 0) < val:
                out[key] = val
        for k, v in out.items():
            seen[k] = v
            if k[0] == "c":
                self.ops[k[1]][v - 1]["inc"] = True
        return list(out.items())

    def op(self, e, meth, reads=(), writes=(), **kw):
        fn = (meth, kw)
        deps = self._collect(reads, writes)
        waits = self._filter(e, deps)
        lst = self.ops[e]
        seq = len(lst) + 1
        lst.append({"fn": fn, "waits": waits, "inc": False, "dma": None})
        tok = ("c", e, seq)
        for b in writes:
            b.w = tok
            b.r = []
        for b in reads:
            b.r.append(tok)
        return tok

    def dma(self, q, out, in_, reads=(), writes=(), is_output=False, **kw):
        deps = self._collect(reads, writes)
        j = self._dma_slot(q)
        prev = self.dma_uses[j] * 16
        if prev:
            deps.append(("d", j, prev))
        waits = self._filter(q, deps)
        self.dma_uses[j] += 1
        val = self.dma_uses[j] * 16
        kw = dict(kw)
        kw["out"] = out
        kw["in_"] = in_
        self.ops[q].append({"fn": ("dma_start", kw),
                            "waits": waits, "inc": False, "dma": j})
        tok = ("d", j, val)
        for b in writes:
            b.w = tok
            b.r = []
        for b in reads:
            b.r.append(tok)
        if is_output:
            self.out_tokens.append(tok)
        return tok

    def custom(self, e, fn, reads=(), writes=(), dma=False):
        if not dma:
            deps = self._collect(reads, writes)
            waits = self._filter(e, deps)
            lst = self.ops[e]
            seq = len(lst) + 1
            lst.append({"fn": ("__custom__", fn), "waits": waits, "inc": False, "dma": None})
            tok = ("c", e, seq)
        else:
            deps = self._collect(reads, writes)
            j = self._dma_slot(e)
            prev = self.dma_uses[j] * 16
            if prev:
                deps.append(("d", j, prev))
            waits = self._filter(e, deps)
            self.dma_uses[j] += 1
            self.ops[e].append({"fn": ("__custom__", fn), "waits": waits, "inc": False, "dma": j})
            tok = ("d", j, self.dma_uses[j] * 16)
        for b in writes:
            b.w = tok
            b.r = []
        for b in reads:
            b.r.append(tok)
        return tok

    def collective(self, reads, writes, **kw):
        deps = self._collect(reads, writes)
        waits = self._filter("pool", deps)
        j = self.n_coll
        self.n_coll += 1
        self.ops["pool"].append({"fn": ("collective_compute", kw), "waits": waits, "inc": False, "dma": None, "coll": j})
        tok = ("x", j, 1)
        for b in writes:
            b.w = tok
            b.r = []
        for b in reads:
            b.r.append(tok)
        return tok

    def finish(self):
        waits = self._filter("sp", list(self.out_tokens))
        self.ops["sp"].append({"fn": None, "waits": waits, "inc": False, "dma": None})

    def emit(self):
        nc = self.nc
        with contextlib.ExitStack() as st:
            csem = {e: st.enter_context(nc.semaphore("c_" + e)) for e in ENG_NAMES}
            dsem = [st.enter_context(nc.semaphore("d%d" % j)) for j in range(self.N_DMA_SEMS)]
            xsem = [st.enter_context(nc.semaphore("x%d" % j)) for j in range(self.n_coll)]
            cum = {}
            for e in ENG_NAMES:
                c = 0
                arr = []
                for o in self.ops[e]:
                    if o["inc"]:
                        c += 1
                    arr.append(c)
                cum[e] = arr

            def run(e, eng):
                for o in self.ops[e]:
                    for key, val in o["waits"]:
                        if key[0] == "c":
                            eng.wait_ge(csem[key[1]], cum[key[1]][val - 1])
                        elif key[0] == "x":
                            eng.wait_ge(xsem[key[1]], val)
                        else:
                            eng.wait_ge(dsem[key[1]], val)
                    if o["fn"] is None:
                        continue
                    if o["fn"][0] == "__custom__":
                        ins = o["fn"][1](eng)
                    else:
                        ins = getattr(eng, o["fn"][0])(**o["fn"][1])
                    if o.get("coll") is not None:
                        ins.then_inc(xsem[o["coll"]], 1)
                    elif o["dma"] is not None:
                        ins.then_inc(dsem[o["dma"]], 16)
                    elif o["inc"]:
                        ins.then_inc(csem[e], 1)

            with nc.Block() as block:
                @block.tensor
                def _(eng):
                    run("pe", eng)

                @block.scalar
                def _(eng):
                    run("act", eng)

                @block.vector
                def _(eng):
                    run("dve", eng)

                @block.gpsimd
                def _(eng):
                    run("pool", eng)

                @block.sync
                def _(eng):
                    run("sp", eng)


class Env:
    def __init__(self):
        self.nc = bass.Bass("TRN2", target_bir_lowering=False)
        self.P = Prog(self.nc)
        self.st = contextlib.ExitStack()
        self.names = set()
        self.psb = None
        self.ps_rr = 0
        self.uid = 0

    def din(self, name, shape, dt=F32):
        return self.nc.dram_tensor(name, list(shape), dt, kind="ExternalInput").ap()

    def dout(self, name, shape, dt=F32):
        return self.nc.dram_tensor(name, list(shape), dt, kind="ExternalOutput").ap()

    def sb(self, name, shape, dt=F32, st=None):
        self.uid += 1
        return (st or self.st).enter_context(self.nc.sbuf_tensor("s_%s_%d" % (name, self.uid), list(shape), dt))

    def dint(self, name, shape, dt=F32):
        return self.nc.dram_tensor(name, list(shape), dt).ap()

    def psum(self):
        self.ps = self.st.enter_context(self.nc.psum_tensor("ps", [128, 8, 512], F32))
        self.psb = [Buf("ps%d" % i) for i in range(8)]
        return self.ps

    def bank(self):
        i = self.ps_rr
        self.ps_rr = (self.ps_rr + 1) % 8
        return i

    def done(self):
        self.P.finish()
        self.P.emit()
        self.st.close()
        return self.nc


def tiles_of(nt):
    out = []
    t = 0
    while t < nt:
        w = min(512, nt - t)
        out.append((t, w))
        t += w
    return out


def emit_consts(E):
    P = E.P
    ones = E.sb("ones_bf", [128, 128], BF16)
    Bones = Buf("ones")
    P.op("pool", "memset", writes=[Bones], ap=ones[:], constant=1.0)
    return ones, Bones


def emit_norm_mod_tile(E, ones, Bones, xsrc, Bx, w, sh, sc1, Bmod, hdst, Bh, scr, h32dst=None, Bh32=None):
    P = E.P
    sq, Bsq = scr["sq"], scr["Bsq"]
    rstd, Brstd = scr["rstd"], scr["Brstd"]
    tmp, Btmp = scr["tmp"], scr["Btmp"]
    P.op("act", "activation", reads=[Bx], writes=[Bsq], out=sq[:, :, 0:w], in_=xsrc, func=AF.Square)
    b = E.bank()
    for k in range(KC):
        P.op("pe", "matmul", reads=[Bones, Bsq], writes=[E.psb[b]], out=E.ps[:, b, 0:w], lhsT=ones[:], rhs=sq[:, k, 0:w], start=(k == 0), stop=(k == KC - 1))
    P.op("act", "activation", reads=[E.psb[b]], writes=[Brstd], out=rstd[:, 0:w], in_=E.ps[:, b, 0:w], func=AF.Sqrt, bias=EPS, scale=1.0 / D)
    P.op("dve", "reciprocal", reads=[Brstd], writes=[Brstd], out=rstd[:, 0:w], in_=rstd[:, 0:w])
    for k in range(KC):
        i = k % 2
        P.op("dve", "scalar_tensor_tensor", reads=[Bx, Brstd, Bmod], writes=[Btmp[i]], out=tmp[i][:, 0:w], in0=xsrc[:, k, :], scalar=sc1[:, k:k + 1], in1=rstd[:, 0:w],
                                                                op0=ALU.mult, op1=ALU.mult)
        if h32dst is not None:
            P.op("pool", "tensor_scalar", reads=[Btmp[i], Bmod], writes=[Bh32], out=h32dst[:, k, :], in0=tmp[i][:, 0:w], scalar1=sh[:, k:k + 1], scalar2=None, op0=ALU.add)
        P.op("act", "activation", reads=[Btmp[i], Bmod], writes=[Bh], out=hdst[:, k, :], in_=tmp[i][:, 0:w], func=AF.Identity, bias=sh[:, k:k + 1], scale=1.0)


def norm_scratch(E):
    scr = {}
    scr["sq"] = E.sb("n_sq", [128, KC, 512], BF16)
    scr["Bsq"] = Buf("sq")
    scr["rstd"] = E.sb("n_rstd", [128, 512], F32)
    scr["Brstd"] = Buf("rstd")
    scr["tmp"] = [E.sb("n_tmp%d" % i, [128, 512], F32) for i in range(2)]
    scr["Btmp"] = [Buf("tmp%d" % i) for i in range(2)]
    return scr


def emit_load_mod(E, mod_d):
    P = E.P
    mod = E.sb("mod", [128, 48, 2], F32)
    sc1a = E.sb("sc1a", [128, 8, 2], F32)
    sc1f = E.sb("sc1f", [128, 8, 2], F32)
    Bmod = Buf("mod")
    P.dma("sp", mod[:], mod_d, writes=[Bmod])
    P.op("dve", "tensor_scalar", reads=[Bmod], writes=[Bmod], out=sc1a[:], in0=mod[:, 8:16, :], scalar1=1.0, scalar2=None, op0=ALU.add)
    P.op("dve", "tensor_scalar", reads=[Bmod], writes=[Bmod], out=sc1f[:], in0=mod[:, 32:40, :], scalar1=1.0, scalar2=None, op0=ALU.add)
    return mod, sc1a, sc1f, Bmod


I32 = mybir.dt.int32
NT = NL + LC
NKB = 66
NKEY = NKB * 128
GROWS = 513


def prog_barrier(P):
    toks = []
    for f in ("pe", "act", "dve", "pool"):
        lst = P.ops[f]
        for idx in range(len(lst) - 1, -1, -1):
            if lst[idx]["fn"] is not None and lst[idx]["dma"] is None and lst[idx].get("coll") is None:
                toks.append(("c", f, idx + 1))
                break
    for j in range(P.N_DMA_SEMS):
        if P.dma_uses[j]:
            toks.append(("d", j, P.dma_uses[j] * 16))
    for e in ENG_NAMES:
        waits = P._filter(e, list(toks))
        P.ops[e].append({"fn": None, "waits": waits, "inc": False, "dma": None})


def build_fused(nstages=6):
    E = Env()
    P = E.P
    nc = E.nc
    xT_d = E.din("xT", [128, KC, NT])
    cT_d = E.din("cT", [128, KC, 2])
    adaw_d = E.din("adaw", [2, 128, KC, 6144])
    adab_d = E.din("adab", [2, 128, 48])
    wall_d = E.din("wall", [2, 128, KC, 4096])
    ropeC_d = E.din("ropeC", [64, NL])
    ropeS_d = E.din("ropeS", [64, NL])
    bc_d = [E.din("nabc%d" % i, [64 * 15, 256]) for i in range(2)]
    idxtab_d = E.din("idxtab", [128, 64], I32)
    selh_d = E.din("selh", [128, 8])
    convw_d = E.din("convw", [2, 128, 2, 3])
    wna_d = E.din("wout_na", [2, 64, 4, D])
    wor_d = E.din("wout_r", [2, 128, 6, D])
    dlam_d = E.din("dlam", [2, 1, 256])
    subln_d = E.din("subln", [2, 128, 1])
    identb_d = E.din("identb", [128, 128], BF16)
    fw1_d = E.din("fw1", [1, 128, KC, D_FF])
    fw3_d = E.din("fw3", [1, 128, KC, D_FF])
    fw2_d = E.din("fw2", [1, 128, D_FF // 128, D])
    NEI = NEXP if nstages >= 6 else 1
    mw1_d = E.din("mw1", [NEI, 128, KC, D_EXP])
    mw3_d = E.din("mw3", [NEI, 128, KC, D_EXP])
    mw2_d = E.din("mw2", [NEI, 128, D_EXP // 128, D])
    wrt_d = E.din("wrt", [128, KC, NEXP])
    sel_d = E.din("sel", [8, 8, 128])
    ident_d = E.din("ident", [128, 128])
    fg_d = E.din("fgain", [128, KC])
    xo_d = E.dout("xo", [128, KC, NL])
    SC = []
    for l in range(2):
        sc = {}
        for nm in ("AK", "AV", "V0", "V1"):
            sc["blob" + nm] = E.dint("blob%s%d" % (nm, l), [NL, 256], BF16)
            sc["G" + nm] = E.dint("G%s%d" % (nm, l), [4 * NL, 256], BF16)
        for nm in ("B0", "B1"):
            sc["blob" + nm] = E.dint("blob%s%d" % (nm, l), [256, NL], BF16)
            sc["G" + nm] = E.dint("G%s%d" % (nm, l), [4 * 256, NL], BF16)
        sc["blobE"] = E.dint("blobE%d" % l, [4, 512], BF16)
        sc["GE"] = E.dint("GE%d" % l, [16, 512], BF16)
        sc["qb"] = E.dint("qb_s%d" % l, [8, 64, NT], BF16)
        sc["qa"] = E.dint("qa_s%d" % l, [128, 2, NT], BF16)
        sc["kvac"] = E.dint("kvac_s%d" % l, [LC, 512], BF16)
        sc["kbc"] = E.dint("kbc_s%d" % l, [8, 64, LC], BF16)
        sc["vbc"] = E.dint("vbc_s%d" % l, [LC, 512], BF16)
        sc["cgx"] = E.dint("cgx_s%d" % l, [128, 2, NT], F32)
        sc["bg"] = E.dint("bg_s%d" % l, [128, 2, NT], F32)
        for k in list(sc.keys()):
            sc["B" + k] = Buf(k)
        SC.append(sc)
    E.psum()
    ps_bf = E.ps.bitcast(BF16)
    ones, Bones = emit_consts(E)
    tl = tiles_of(NT)
    x = E.sb("x", [128, KC, NT], F32)
    Bx = [Buf("x%d" % i) for i in range(len(tl))]
    for ti, (t0, w) in enumerate(tl):
        P.dma("sp", x[:, :, t0:t0 + w], xT_d[:, :, t0:t0 + w], writes=[Bx[ti]])
    mo = E.sb("mo", [128, 2, 48, 2], F32)
    sc1 = E.sb("sc1", [128, 2, 2, 8, 2], F32)
    Bmod = Buf("mod")
    idb = E.sb("idb", [128, 128], BF16)
    Bid = Buf("idb")
    P.dma("sp", idb[:], identb_d, writes=[Bid])

    def phase():
        return contextlib.ExitStack()

    with phase() as ph:
        cf = E.sb("cf", [128, KC, 2], F32, ph)
        cs = E.sb("cs", [128, KC, 2], BF16, ph)
        ab = E.sb("ab", [128, 2, 48], F32, ph)
        slots = [E.sb("aw", [128, KC, 2048], BF16, ph) for i in range(3)]
        Bs = [Buf("aw%d" % i) for i in range(3)]
        Bc, Bab = Buf("c"), Buf("ab")
        P.dma("sp", cf[:], cT_d, writes=[Bc])
        for l in range(2):
            P.dma("sp", ab[:, l, :], adab_d[l], writes=[Bab])
        P.op("act", "activation", reads=[Bc], writes=[Bc], out=cs[:], in_=cf[:], func=AF.Silu)
        for l in range(2):
            b = E.bank()
            for th in range(3):
                s = (l * 3 + th) % 3
                P.dma("pool", slots[s][:], adaw_d[l, :, :, th * 2048:(th + 1) * 2048], writes=[Bs[s]])
                for m in range(16):
                    mc = th * 16 + m
                    for k in range(KC):
                        P.op("pe", "matmul", reads=[Bs[s], Bc], writes=[E.psb[b]], out=E.ps[:, b, 2 * mc:2 * mc + 2], lhsT=slots[s][:, k, m * 128:(m + 1) * 128],
                             rhs=cs[:, k, :], start=(k == 0), stop=(k == KC - 1))
            for j in range(2):
                P.op("dve", "tensor_tensor", reads=[E.psb[b], Bab], writes=[Bmod], out=mo[:, l, :, j], in0=E.ps[:, b, j:96:2], in1=ab[:, l, :], op=ALU.add)
            P.op("dve", "tensor_scalar", reads=[Bmod], writes=[Bmod], out=sc1[:, l, 0, :, :], in0=mo[:, l, 8:16, :], scalar1=1.0, scalar2=None, op0=ALU.add)
            P.op("dve", "tensor_scalar", reads=[Bmod], writes=[Bmod], out=sc1[:, l, 1, :, :], in0=mo[:, l, 32:40, :], scalar1=1.0, scalar2=None, op0=ALU.add)
        prog_barrier(P)

    def norm_scr(ph):
        scr = {}
        scr["sq"] = E.sb("n_sq", [128, KC, 512], BF16, ph)
        scr["Bsq"] = Buf("sq")
        scr["rstd"] = E.sb("n_rstd", [128, 512], F32, ph)
        scr["Brstd"] = Buf("rstd")
        scr["tmp"] = [E.sb("n_tmp", [128, 512], F32, ph) for i in range(2)]
        scr["Btmp"] = [Buf("tmp%d" % i) for i in range(2)]
        return scr

    evac_rr = [0]

    def evac_copy(dst, src, reads, writes):
        if evac_rr[0] % 2 == 0:
            P.op("act", "activation", reads=reads, writes=writes, out=dst, in_=src, func=AF.Copy)
        else:
            P.op("dve", "tensor_copy", reads=reads, writes=writes, out=dst, in_=src)
        evac_rr[0] += 1

    def stage_L1(l):
        sc = SC[l]
        with phase() as ph0:
            hT = E.sb("hT", [128, KC, NT], BF16, ph0)
            Bh = [Buf("h%d" % i) for i in range(len(tl))]
            with phase() as ph:
                scr = norm_scr(ph)
                for ti, (t0, w) in enumerate(tl):
                    j = 0 if t0 < NL else 1
                    emit_norm_mod_tile(E, ones, Bones, x[:, :, t0:t0 + w], Bx[ti], w, mo[:, l, 0:8, j], sc1[:, l, 0, :, j], Bmod,
                                       hT[:, :, t0:t0 + w], Bh[ti], scr)
                prog_barrier(P)
            with phase() as ph:
                rC = E.sb("rC", [64, NL], F32, ph)
                rS = E.sb("rS", [64, NL], F32, ph)
                Brope = Buf("rope")
                P.dma("sp", rC[:], ropeC_d, writes=[Brope])
                P.dma("sp", rS[:], ropeS_d, writes=[Brope])
                NW = 3
                wsl = [E.sb("wsl", [128, KC, 512], BF16, ph) for i in range(NW)]
                Bw = [Buf("wsl%d" % i) for i in range(NW)]
                stg = [E.sb("stg", [128, NT], F32, ph) for i in range(2)]
                Bstg = [Buf("stg%d" % i) for i in range(2)]
                t1 = [E.sb("rt", [64, 512], F32, ph) for i in range(4)]
                Bt1 = [Buf("rt%d" % i) for i in range(4)]
                xin_t = [E.sb("xin", [128, 512], F32, ph) for i in range(2)]
                Bxin = [Buf("xin%d" % i) for i in range(2)]
                kvst = E.sb("kvst", [128, 9, 512], BF16, ph)
                Bkvst = Buf("kvst")
                egw = E.sb("egw", [128, 2, 2], BF16, ph)
                Begw = Buf("egw")
                stg_rr = [0]
                wrr = [0]

                def load_slab(s):
                    slot = wrr[0] % NW
                    wrr[0] += 1
                    P.dma("pool", wsl[slot][:], wall_d[l, :, :, s * 512:(s + 1) * 512], writes=[Bw[slot]])
                    return slot

                def next_stage():
                    i = stg_rr[0]
                    stg_rr[0] = (i + 1) % 2
                    return i

                def roped(sa, sr, dests):
                    for hm in range(8):
                        si = next_stage()
                        sview = stg[si].bitcast(BF16)
                        for ti, (t0, w) in enumerate(tl):
                            ba = E.bank()
                            for k in range(KC):
                                P.op("pe", "matmul", reads=[Bw[sa], Bh[ti]], writes=[E.psb[ba]], out=E.ps[0:64, ba, 0:w], lhsT=wsl[sa][:, k, hm * 64:(hm + 1) * 64],
                                     rhs=hT[:, k, t0:t0 + w], start=(k == 0), stop=(k == KC - 1))
                            if t0 < NL:
                                bb = E.bank()
                                for k in range(KC):
                                    P.op("pe", "matmul", reads=[Bw[sr], Bh[ti]], writes=[E.psb[bb]], out=E.ps[0:64, bb, 0:w], lhsT=wsl[sr][:, k, hm * 64:(hm + 1) * 64],
                                         rhs=hT[:, k, t0:t0 + w], start=(k == 0), stop=(k == KC - 1))
                                ia = (2 * ti) % 4
                                ib = (2 * ti + 1) % 4
                                P.op("dve", "tensor_tensor", reads=[E.psb[ba], Brope], writes=[Bt1[ia]], out=t1[ia][:, 0:w], in0=E.ps[0:64, ba, 0:w], in1=rC[:, t0:t0 + w], op=ALU.mult)
                                P.op("dve", "tensor_tensor", reads=[E.psb[bb], Brope], writes=[Bt1[ib]], out=t1[ib][:, 0:w], in0=E.ps[0:64, bb, 0:w], in1=rS[:, t0:t0 + w], op=ALU.mult)
                                P.op("pool", "tensor_tensor", reads=[Bt1[ia], Bt1[ib]], writes=[Bstg[si]], out=sview[0:64, t0:t0 + w], in0=t1[ia][:, 0:w], in1=t1[ib][:, 0:w], op=ALU.add)
                            else:
                                evac_copy(sview[0:64, t0:t0 + w], E.ps[0:64, ba, 0:w], [E.psb[ba]], [Bstg[si]])
                        for (dap, c0, ncol, Bd) in dests(hm):
                            P.dma("sp", dap, sview[0:64, c0:c0 + ncol], reads=[Bstg[si]], writes=[Bd])

                def plain_chunk(slot, c, dap, Bd, dt):
                    si = next_stage()
                    sview = stg[si].bitcast(BF16) if dt == BF16 else stg[si]
                    for ti, (t0, w) in enumerate(tl):
                        b = E.bank()
                        for k in range(KC):
                            P.op("pe", "matmul", reads=[Bw[slot], Bh[ti]], writes=[E.psb[b]], out=E.ps[:, b, 0:w], lhsT=wsl[slot][:, k, c * 128:(c + 1) * 128],
                                 rhs=hT[:, k, t0:t0 + w], start=(k == 0), stop=(k == KC - 1))
                        evac_copy(sview[:, t0:t0 + w], E.ps[:, b, 0:w], [E.psb[b]], [Bstg[si]])
                    P.dma("sp", dap, sview[:, 0:NT], reads=[Bstg[si]], writes=[Bd])

                def tokmajor(slot, nm0, nm1, ctx_ap, Bctx):
                    bv = [sc["blob" + nm0].rearrange("(j p) c -> p j c", p=128), sc["blob" + nm1].rearrange("(j p) c -> p j c", p=128)]
                    Bb = [sc["Bblob" + nm0], sc["Bblob" + nm1]]
                    ctx_v = ctx_ap.rearrange("(j p) c -> p j c", p=128)
                    for half in range(2):
                        for jj in range(9):
                            blk = half * 9 + jj
                            b = E.bank()
                            ti = min(blk // 4, 4)
                            for k in range(KC):
                                P.op("pe", "matmul", reads=[Bw[slot], Bh[ti]], writes=[E.psb[b]], out=E.ps[:, b, :], lhsT=hT[:, k, blk * 128:(blk + 1) * 128],
                                     rhs=wsl[slot][:, k, :], start=(k == 0), stop=(k == KC - 1))
                            evac_copy(kvst[:, jj, :], E.ps[:, b, :], [E.psb[b]], [Bkvst])
                        for hh in range(2):
                            if half == 0:
                                P.dma("sp", bv[hh][:, 0:9, :], kvst[:, 0:9, hh * 256:(hh + 1) * 256], reads=[Bkvst], writes=[Bb[hh]])
                            else:
                                P.dma("sp", bv[hh][:, 9:16, :], kvst[:, 0:7, hh * 256:(hh + 1) * 256], reads=[Bkvst], writes=[Bb[hh]])
                        if half == 1:
                            P.dma("sp", ctx_v[:, 0:2, :], kvst[:, 7:9, :], reads=[Bkvst], writes=[Bctx])

                sa = load_slab(1)
                sr = load_slab(2)
                roped(sa, sr, lambda hm: [(sc["blobB%d" % (hm // 4)][(hm % 4) * 64:(hm % 4 + 1) * 64, :], 0, NL, sc["BblobB%d" % (hm // 4)]),
                                          (sc["kbc"][hm], NL, LC, sc["Bkbc"])])
                AGKW = dict(kind="AllGather", op=ALU.bypass, replica_groups=[[0, 1, 2, 3], [4, 5, 6, 7]])
                for nm in ("B0", "B1"):
                    P.collective(reads=[sc["Bblob" + nm]], writes=[sc["BG" + nm]], ins=[sc["blob" + nm]], outs=[sc["G" + nm]], **AGKW)
                s7 = load_slab(7)
                for c in range(2):
                    si = next_stage()
                    for ti, (t0, w) in enumerate(tl):
                        bx = E.bank()
                        for k in range(KC):
                            P.op("pe", "matmul", reads=[Bw[s7], Bh[ti]], writes=[E.psb[bx]], out=E.ps[:, bx, 0:w], lhsT=wsl[s7][:, k, c * 128:(c + 1) * 128],
                                 rhs=hT[:, k, t0:t0 + w], start=(k == 0), stop=(k == KC - 1))
                        bc = E.bank()
                        for k in range(KC):
                            P.op("pe", "matmul", reads=[Bw[s7], Bh[ti]], writes=[E.psb[bc]], out=E.ps[:, bc, 0:w], lhsT=wsl[s7][:, k, (2 + c) * 128:(3 + c) * 128],
                                 rhs=hT[:, k, t0:t0 + w], start=(k == 0), stop=(k == KC - 1))
                        xi = ti % 2
                        P.op("act", "activation", reads=[E.psb[bx]], writes=[Bxin[xi]], out=xin_t[xi][:, 0:w], in_=E.ps[:, bx, 0:w], func=AF.Copy)
                        P.op("dve", "tensor_tensor", reads=[E.psb[bc], Bxin[xi]], writes=[Bstg[si]], out=stg[si][:, t0:t0 + w], in0=E.ps[:, bc, 0:w], in1=xin_t[xi][:, 0:w], op=ALU.mult)
                    P.op("dve", "tensor_copy", reads=[Bstg[si]], writes=[Begw], out=egw[:, c, 0:1], in_=stg[si][:, 0:1])
                    P.op("dve", "tensor_copy", reads=[Bstg[si]], writes=[Begw], out=egw[:, c, 1:2], in_=stg[si][:, NL - 1:NL])
                    P.dma("sp", sc["cgx"][:, c, :], stg[si][:, 0:NT], reads=[Bstg[si]], writes=[sc["Bcgx"]])
                P.dma("sp", sc["blobE"][0, 0:512].rearrange("(c p j) -> p c j", c=2, p=128, j=2), egw[:], reads=[Begw], writes=[sc["BblobE"]],
                      allow_slow_non_contiguous=True)
                P.collective(reads=[sc["BblobE"]], writes=[sc["BGE"]], ins=[sc["blobE"]], outs=[sc["GE"]], **AGKW)
                s3 = load_slab(3)
                tokmajor(s3, "V0", "V1", sc["vbc"], sc["Bvbc"])
                for nm in ("V0", "V1"):
                    P.collective(reads=[sc["Bblob" + nm]], writes=[sc["BG" + nm]], ins=[sc["blob" + nm]], outs=[sc["G" + nm]], **AGKW)
                sa = load_slab(4)
                sr = load_slab(5)
                roped(sa, sr, lambda hm: [(sc["qb"][hm], 0, NT, sc["Bqb"])])
                s0 = load_slab(0)
                tokmajor(s0, "AK", "AV", sc["kvac"], sc["Bkvac"])
                for nm in ("AK", "AV"):
                    P.collective(reads=[sc["Bblob" + nm]], writes=[sc["BG" + nm]], ins=[sc["blob" + nm]], outs=[sc["G" + nm]], **AGKW)
                s6 = load_slab(6)
                plain_chunk(s6, 0, sc["qa"][:, 0, :], sc["Bqa"], BF16)
                plain_chunk(s6, 1, sc["qa"][:, 1, :], sc["Bqa"], BF16)
                plain_chunk(s6, 2, sc["bg"][:, 0, :], sc["Bbg"], F32)
                plain_chunk(s6, 3, sc["bg"][:, 1, :], sc["Bbg"], F32)
                prog_barrier(P)

    def stage_L2(l, with_ctx):
        sc = SC[l]
        NQ = NT if with_ctx else NL
        lambda_init = 0.8 - 0.6 * math.exp(-0.3 * l)
        with phase() as ph0:
            mixd = E.sb("mixd", [128, 4, NQ], BF16, ph0)
            Bmixd = [Buf("mixd%d" % i) for i in range(4)]
            dl = E.sb("dl", [1, 256], F32, ph0)
            lt = E.sb("lt", [1, 128], F32, ph0)
            ls = E.sb("ls", [1, 4], F32, ph0)
            ones1 = E.sb("ones1", [1, 128], F32, ph0)
            nl = E.sb("nl", [128, 1], F32, ph0)
            gsc = E.sb("gsc", [128, 1], F32, ph0)
            Bl = Buf("lam")
            P.dma("sp", dl[:], dlam_d[l], writes=[Bl])
            P.dma("sp", gsc[:], subln_d[l], writes=[Bl])
            P.op("pool", "memset", writes=[Bl], ap=ones1[:], constant=1.0)
            P.op("dve", "tensor_tensor", reads=[Bl], writes=[Bl], out=lt[:, 0:64], in0=dl[:, 0:64], in1=dl[:, 64:128], op=ALU.mult)
            P.op("dve", "tensor_tensor", reads=[Bl], writes=[Bl], out=lt[:, 64:128], in0=dl[:, 128:192], in1=dl[:, 192:256], op=ALU.mult)
            P.op("dve", "reduce_sum", reads=[Bl], writes=[Bl], out=ls[:, 0:1], in_=lt[:, 0:64], axis=AX.X)
            P.op("dve", "reduce_sum", reads=[Bl], writes=[Bl], out=ls[:, 1:2], in_=lt[:, 64:128], axis=AX.X)
            P.op("act", "activation", reads=[Bl], writes=[Bl], out=ls[:, 0:2], in_=ls[:, 0:2], func=AF.Exp)
            P.op("dve", "tensor_tensor", reads=[Bl], writes=[Bl], out=ls[:, 2:3], in0=ls[:, 1:2], in1=ls[:, 0:1], op=ALU.subtract)
            P.op("dve", "tensor_scalar", reads=[Bl], writes=[Bl], out=ls[:, 3:4], in0=ls[:, 2:3], scalar1=-lambda_init, scalar2=None, op0=ALU.add)
            b = E.bank()
            P.op("pe", "matmul", reads=[Bl], writes=[E.psb[b]], out=E.ps[:, b, 0:1], lhsT=ones1[:], rhs=ls[:, 3:4], start=True, stop=True)
            P.op("dve", "tensor_copy", reads=[E.psb[b]], writes=[Bl], out=nl[:], in_=E.ps[:, b, 0:1])
            P.op("dve", "tensor_scalar", reads=[Bl], writes=[Bl], out=gsc[:], in0=gsc[:], scalar1=(1.0 - lambda_init), scalar2=None, op0=ALU.mult)

            with phase() as ph:
                NKS = 3
                kt = [E.sb("kt", [65, NKEY], BF16, ph) for i in range(NKS)]
                vt = [E.sb("vt", [128, NKB, 128], BF16, ph) for i in range(2)]
                qt = [E.sb("qt", [65, 2, NQ], BF16, ph) for i in range(1)]
                Bk = [Buf("kt%d" % i) for i in range(NKS)]
                Bv = [Buf("vt%d" % i) for i in range(2)]
                Bq = [Buf("qt%d" % i) for i in range(1)]
                Baug = Buf("aug")
                for i in range(NKS):
                    P.op("pool", "memset", writes=[Baug], ap=kt[i][64:65, :], constant=1.0)
                for i in range(1):
                    P.op("pool", "memset", writes=[Baug], ap=qt[i][64:65, :, :], constant=0.0)
                accD = [E.sb("accD", [128, 512], F32, ph) for i in range(2)]
                accP = [E.sb("accP", [128, 512], F32, ph) for i in range(2)]
                BaD = [Buf("accD%d" % i) for i in range(2)]
                BaP = [Buf("accP%d" % i) for i in range(2)]
                ones32 = E.sb("ones32", [128, 128], F32, ph)
                Bo32 = Buf("ones32")
                P.op("pool", "memset", writes=[Bo32], ap=ones32[:], constant=1.0)
                pT = [E.sb("pT", [128, 512], BF16, ph) for i in range(4)]
                BpT = [Buf("pT%d" % i) for i in range(4)]
                rr = [E.sb("rr", [128, 512], F32, ph) for i in range(2)]
                Brr = [Buf("rr%d" % i) for i in range(2)]
                osq = E.sb("osq", [128, 512], BF16, ph)
                Bosq = Buf("osq")
                GVv = [sc["GV0"].rearrange("(j p) c -> p j c", p=128), sc["GV1"].rearrange("(j p) c -> p j c", p=128)]
                vbcv = sc["vbc"].rearrange("(j p) c -> p j c", p=128)

                def load_map(m):
                    s = m % NKS
                    P.dma("sp", kt[s][0:64, 0:LC], sc["kbc"][m], reads=[sc["Bkbc"]], writes=[Bk[s]])
                    for r in range(4):
                        P.dma("sp", kt[s][0:64, LC + r * NL:LC + (r + 1) * NL], sc["GB%d" % (m // 4)][r * 256 + (m % 4) * 64:r * 256 + (m % 4 + 1) * 64, :],
                              reads=[sc["BGB%d" % (m // 4)]], writes=[Bk[s]])

                def load_vq(dh):
                    s = dh % 2
                    P.dma("sp", vt[s][:, 0:2, :], vbcv[:, :, dh * 128:(dh + 1) * 128], reads=[sc["Bvbc"]], writes=[Bv[s]])
                    P.dma("sp", vt[s][:, 2:NKB, :], GVv[dh // 2][:, :, (dh % 2) * 128:(dh % 2 + 1) * 128], reads=[sc["BGV%d" % (dh // 2)]], writes=[Bv[s]])

                def load_q(dh):
                    for mi in range(2):
                        P.dma("sp", qt[0][0:64, mi, :], sc["qb"][2 * dh + mi][:, 0:NQ], reads=[sc["Bqb"]], writes=[Bq[0]])

                qtiles = [(t0, w, NKB) for (t0, w) in tiles_of(NL)] + ([(NL, LC, 2)] if with_ctx else [])
                OB = (0, 1)
                ZB = (2, 3)
                SBK = (4, 5, 6, 7)
                load_map(0)
                load_map(1)
                load_vq(0)
                load_q(0)
                for dh in range(4):
                    s = dh % 2
                    if dh + 1 < 4:
                        load_map(2 * dh + 2)
                        load_vq(dh + 1)
                    for qi, (q0, qw, nkb) in enumerate(qtiles):
                        if dh + 1 < 4 and qi == len(qtiles) - 1:
                            pass
                        steps = [(kb, mi) for kb in range(nkb) for mi in range(2)]

                        def emit_S(i):
                            kb, mi = steps[i]
                            sbk = SBK[i % 4]
                            ks = (2 * dh + mi) % NKS
                            P.op("pe", "matmul", reads=[Bk[ks], Bq[0], Baug], writes=[E.psb[sbk]], out=E.ps[:, sbk, 0:qw], lhsT=kt[ks][0:65, kb * 128:(kb + 1) * 128],
                                 rhs=qt[0][0:65, mi, q0:q0 + qw], start=True, stop=True)

                        emit_S(0)
                        emit_S(1)
                        for i, (kb, mi) in enumerate(steps):
                            sbk = SBK[i % 4]
                            pi = i % 4
                            P.op("act", "activation", reads=[E.psb[sbk]], writes=[BpT[pi]], out=pT[pi][:, 0:qw], in_=E.ps[:, sbk, 0:qw], func=AF.Exp, scale=0.125)
                            if i + 2 < len(steps):
                                emit_S(i + 2)
                            first = (kb == 0)
                            last = (kb == nkb - 1)
                            P.op("pe", "matmul", reads=[Bv[s], BpT[pi]], writes=[E.psb[OB[mi]]], out=E.ps[:, OB[mi], 0:qw], lhsT=vt[s][:, kb, :], rhs=pT[pi][:, 0:qw], start=first, stop=last)
                            if kb % 2 == 0:
                                if kb == 0:
                                    P.op("dve", "tensor_copy", reads=[BpT[pi]], writes=[BaD[mi]], out=accD[mi][:, 0:qw], in_=pT[pi][:, 0:qw])
                                else:
                                    P.op("dve", "tensor_tensor", reads=[BpT[pi], BaD[mi]], writes=[BaD[mi]], out=accD[mi][:, 0:qw], in0=accD[mi][:, 0:qw], in1=pT[pi][:, 0:qw], op=ALU.add)
                            else:
                                P.op("pe", "matmul", reads=[Bones, BpT[pi]], writes=[E.psb[ZB[mi]]], out=E.ps[:, ZB[mi], 0:qw], lhsT=ones[:], rhs=pT[pi][:, 0:qw], start=(kb == 1), stop=False)
                        for mi in range(2):
                            P.op("pe", "matmul", reads=[Bo32, BaD[mi]], writes=[E.psb[ZB[mi]]], out=E.ps[:, ZB[mi], 0:qw], lhsT=ones32[:], rhs=accD[mi][:, 0:qw], start=False, stop=True)
                        for mi in range(2):
                            P.op("dve", "reciprocal", reads=[E.psb[ZB[mi]]], writes=[Brr[mi]], out=rr[mi][:, 0:qw], in_=E.ps[:, ZB[mi], 0:qw])
                            P.op("dve", "tensor_tensor", reads=[E.psb[OB[mi]], Brr[mi]], writes=[Brr[mi]], out=rr[mi][:, 0:qw], in0=E.ps[:, OB[mi], 0:qw], in1=rr[mi][:, 0:qw], op=ALU.mult)
                        P.op("dve", "scalar_tensor_tensor", reads=[Brr[0], Brr[1], Bl], writes=[Brr[0]], out=rr[0][:, 0:qw], in0=rr[1][:, 0:qw], scalar=nl[:, 0:1], in1=rr[0][:, 0:qw],
                             op0=ALU.mult, op1=ALU.add)
                        P.op("act", "activation", reads=[Brr[0]], writes=[Bosq], out=osq[:, 0:qw], in_=rr[0][:, 0:qw], func=AF.Square)
                        bz = ZB[0]
                        P.op("pe", "matmul", reads=[Bones, Bosq], writes=[E.psb[bz]], out=E.ps[:, bz, 0:qw], lhsT=ones[:], rhs=osq[:, 0:qw], start=True, stop=True)
                        P.op("act", "activation", reads=[E.psb[bz]], writes=[Brr[1]], out=rr[1][:, 0:qw], in_=E.ps[:, bz, 0:qw], func=AF.Sqrt, bias=EPS, scale=1.0 / 128)
                        P.op("dve", "reciprocal", reads=[Brr[1]], writes=[Brr[1]], out=rr[1][:, 0:qw], in_=rr[1][:, 0:qw])
                        P.op("dve", "scalar_tensor_tensor", reads=[Brr[0], Brr[1], Bl], writes=[Bmixd[dh]], out=mixd[:, dh, q0:q0 + qw], in0=rr[0][:, 0:qw], scalar=gsc[:, 0:1], in1=rr[1][:, 0:qw],
                             op0=ALU.mult, op1=ALU.mult)
                    if 2 * dh + 3 < 8:
                        load_map(2 * dh + 3)
                    if dh + 1 < 4:
                        load_q(dh + 1)
                prog_barrier(P)

            mixn = E.sb("mixn", [64, 4, NQ], BF16, ph0)
            Bmixn = [Buf("mixn%d" % i) for i in range(4)]
            with phase() as ph:
                qa = E.sb("qa", [128, 2, NQ], BF16, ph)
                kvac = E.sb("kvac", [128, 2, 512], BF16, ph)
                kacT = E.sb("kacT", [128, 2, LC], BF16, ph)
                Bna, Bkvac, BkacT = Buf("na_in"), Buf("kvac"), Buf("kacT")
                P.dma("sp", qa[:], sc["qa"][:, :, 0:NQ], reads=[sc["Bqa"]], writes=[Bna])
                P.dma("sp", kvac[:], sc["kvac"].rearrange("(j p) c -> p j c", p=128), reads=[sc["Bkvac"]], writes=[Bkvac])
                bt = E.bank()
                for j in range(2):
                    for hp in range(2):
                        P.op("pe", "transpose", reads=[Bkvac, Bid], writes=[E.psb[bt]], out=ps_bf[:, bt, hp * 256 + j * 128:hp * 256 + (j + 1) * 128], in_=kvac[:, j, hp * 128:(hp + 1) * 128], identity=idb[:])
                P.op("act", "activation", reads=[E.psb[bt]], writes=[BkacT], out=kacT[:].rearrange("p a b -> p (a b)"), in_=ps_bf[:, bt, 0:512], func=AF.Copy)
                kvwin = [E.sb("kwin", [128, 4, 256], BF16, ph) for i in range(2)]
                vwin = [E.sb("vwin", [128, 4, 256], BF16, ph) for i in range(2)]
                kwT = [E.sb("kwT", [128, 2, 512], BF16, ph) for i in range(2)]
                nab = [E.sb("nab", [64, 8, 4, 64], F32, ph) for i in range(2)]
                Bkv = [Buf("kvwin%d" % i) for i in range(2)]
                BkwT = [Buf("kwT%d" % i) for i in range(2)]
                Bnab = [Buf("nab%d" % i) for i in range(2)]
                sbuf_s = [E.sb("nas", [64, 768], F32, ph) for i in range(2)]
                Bs = [Buf("nas%d" % i) for i in range(2)]
                p32 = [E.sb("nap", [64, 768], F32, ph) for i in range(2)]
                Bp32 = [Buf("nap%d" % i) for i in range(2)]
                pn = [E.sb("napn", [64, 768], BF16, ph) for i in range(2)]
                Bpn = [Buf("napn%d" % i) for i in range(2)]
                ptsb = [E.sb("napt", [128, 384], BF16, ph) for i in range(2)]
                Bpt = [Buf("napt%d" % i) for i in range(2)]
                st4 = [E.sb("nast", [64, 4], F32, ph) for i in range(2)]
                Bst4 = [Buf("nast%d" % i) for i in range(2)]
                GAK, GAV = sc["GAK"], sc["GAV"]
                idxt = E.sb("idxt", [128, 64], I32, ph)
                Bidx = Buf("idxt")
                P.dma("sp", idxt[:], idxtab_d, writes=[Bidx])

                def load_row(rl):
                    s = rl % 2
                    P.custom("pool", (lambda eng, rl=rl, s=s: eng.indirect_dma_start(
                        out=kvwin[s][:].rearrange("p a b -> p (a b)"), out_offset=None, in_=GAK[:, :],
                        in_offset=bass.IndirectOffsetOnAxis(ap=idxt[:, rl:rl + 1], axis=0))), reads=[sc["BGAK"], Bidx], writes=[Bkv[s]], dma=True)
                    P.custom("pool", (lambda eng, rl=rl, s=s: eng.indirect_dma_start(
                        out=vwin[s][:].rearrange("p a b -> p (a b)"), out_offset=None, in_=GAV[:, :],
                        in_offset=bass.IndirectOffsetOnAxis(ap=idxt[:, rl:rl + 1], axis=0))), reads=[sc["BGAV"], Bidx], writes=[Bkv[s]], dma=True)
                    P.custom("pool", (lambda eng, rl=rl, s=s: eng.indirect_dma_start(
                        out=nab[s][:].rearrange("p a b c -> p (a b c)"), out_offset=None, in_=bc_d[l][:, :],
                        in_offset=bass.IndirectOffsetOnAxis(ap=idxt[0:64, 32 + rl:32 + rl + 1], axis=0))), reads=[Bidx], writes=[Bnab[s]], dma=True)
                    bt = E.bank()
                    for j in range(4):
                        for hp in range(2):
                            P.op("pe", "transpose", reads=[Bkv[s], Bid], writes=[E.psb[bt]], out=ps_bf[:, bt, hp * 512 + j * 128:hp * 512 + (j + 1) * 128], in_=kvwin[s][:, j, hp * 128:(hp + 1) * 128], identity=idb[:])
                    P.op("act", "activation", reads=[E.psb[bt]], writes=[BkwT[s]], out=kwT[s][:].rearrange("p a b -> p (a b)"), in_=ps_bf[:, bt, 0:1024], func=AF.Copy)

                it = [0]

                def na_block(q0, h, rl):
                    i = it[0] % 2
                    it[0] += 1
                    hp, hb = h // 2, (h % 2) * 64
                    nk = 768 if rl is not None else 256
                    lhs = qa[hb:hb + 64, hp, q0:q0 + 64]
                    if rl is not None:
                        s = rl % 2
                        ba = E.bank()
                        P.op("pe", "matmul", reads=[Bna, BkwT[s]], writes=[E.psb[ba]], out=E.ps[0:64, ba, 0:512], lhsT=lhs, rhs=kwT[s][hb:hb + 64, hp, :], start=True, stop=True)
                        for jj in range(4):
                            P.op("dve", "scalar_tensor_tensor", reads=[E.psb[ba], Bnab[s]], writes=[Bs[i]],
                                 out=sbuf_s[i][:, jj * 128:(jj + 1) * 128].rearrange("p (a m) -> p a m", a=8),
                                 in0=E.ps[0:64, ba, jj * 128:(jj + 1) * 128].rearrange("p (a m) -> p a m", a=8), scalar=0.125,
                                 in1=nab[s][:, :, h, jj:64:4], op0=ALU.mult, op1=ALU.add)
                        c0 = 512
                    else:
                        c0 = 0
                    bb = E.bank()
                    P.op("pe", "matmul", reads=[Bna, BkacT], writes=[E.psb[bb]], out=E.ps[0:64, bb, 0:256], lhsT=lhs, rhs=kacT[hb:hb + 64, hp, :], start=True, stop=True)
                    P.op("act", "activation", reads=[E.psb[bb]], writes=[Bs[i]], out=sbuf_s[i][:, c0:c0 + 256], in_=E.ps[0:64, bb, 0:256], func=AF.Copy, scale=0.125)
                    P.op("dve", "reduce_max", reads=[Bs[i]], writes=[Bst4[i]], out=st4[i][:, 0:1], in_=sbuf_s[i][:, 0:nk], axis=AX.X)
                    P.op("dve", "tensor_scalar", reads=[Bst4[i]], writes=[Bst4[i]], out=st4[i][:, 1:2], in0=st4[i][:, 0:1], scalar1=-1.0, scalar2=None, op0=ALU.mult)
                    P.op("act", "activation", reads=[Bs[i], Bst4[i]], writes=[Bp32[i]], out=p32[i][:, 0:nk], in_=sbuf_s[i][:, 0:nk], func=AF.Exp, bias=st4[i][:, 1:2], scale=1.0)
                    P.op("dve", "reduce_sum", reads=[Bp32[i]], writes=[Bst4[i]], out=st4[i][:, 2:3], in_=p32[i][:, 0:nk], axis=AX.X)
                    P.op("dve", "reciprocal", reads=[Bst4[i]], writes=[Bst4[i]], out=st4[i][:, 3:4], in_=st4[i][:, 2:3])
                    P.op("dve", "tensor_scalar", reads=[Bp32[i], Bst4[i]], writes=[Bpn[i]], out=pn[i][:, 0:nk], in0=p32[i][:, 0:nk], scalar1=st4[i][:, 3:4], scalar2=None, op0=ALU.mult)
                    nb = nk // 128
                    bt = E.bank()
                    for j in range(nb):
                        P.op("pe", "transpose", reads=[Bpn[i], Bid], writes=[E.psb[bt]], out=ps_bf[:, bt, j * 64:(j + 1) * 64], in_=pn[i][:, j * 128:(j + 1) * 128], identity=idb[0:64, 0:64])
                    P.op("act", "activation", reads=[E.psb[bt]], writes=[Bpt[i]], out=ptsb[i][:, 0:nb * 64], in_=ps_bf[:, bt, 0:nb * 64], func=AF.Copy)
                    bo = E.bank()
                    for j in range(nb):
                        if rl is not None and j < 4:
                            lv = vwin[rl % 2][:, j, h * 64:(h + 1) * 64]
                            rd = [Bkv[rl % 2], Bpt[i]]
                        else:
                            lv = kvac[:, j - (4 if rl is not None else 0), 256 + h * 64:256 + (h + 1) * 64]
                            rd = [Bkvac, Bpt[i]]
                        P.op("pe", "matmul", reads=rd, writes=[E.psb[bo]], out=E.ps[0:64, bo, 0:64], lhsT=lv, rhs=ptsb[i][:, j * 64:(j + 1) * 64], start=(j == 0), stop=(j == nb - 1))
                    P.op("dve", "tensor_copy", reads=[E.psb[bo]], writes=[Bmixn[h]], out=mixn[0:64, h, q0:q0 + 64], in_=E.ps[0:64, bo, 0:64])

                load_row(0)
                for rl in range(32):
                    if rl + 1 < 32:
                        load_row(rl + 1)
                    for h in range(4):
                        na_block(rl * 64, h, rl)
                if with_ctx:
                    for qb in range(4):
                        for h in range(4):
                            na_block(NL + qb * 64, h, None)
                prog_barrier(P)

            with phase() as ph:
                mixc = E.sb("mixc", [128, 2, NQ], BF16, ph)
                Bmixc = [Buf("mixc%d" % i) for i in range(2)]
                cg = E.sb("cgx", [128, 2, NT + 4], F32, ph)
                bg = E.sb("bg", [128, 2, NT], F32, ph)
                cw = E.sb("cw", [128, 2, 3], F32, ph)
                ca = E.sb("ca", [128, NL], F32, ph)
                eg = E.sb("eg", [128, 4, 2, 2], BF16, ph)
                selh = E.sb("selh", [128, 8], F32, ph)
                egt = E.sb("egt", [128, 4], F32, ph)
                Bcv, Bca, Beg = Buf("conv_in"), Buf("ca"), Buf("eg")
                P.op("pool", "memset", writes=[Bcv], ap=cg[:], constant=0.0)
                P.dma("sp", cg[:, :, 1:NL + 1], sc["cgx"][:, :, 0:NL], reads=[sc["Bcgx"]], writes=[Bcv])
                P.dma("sp", cg[:, :, NL + 3:NL + 3 + LC], sc["cgx"][:, :, NL:NT], reads=[sc["Bcgx"]], writes=[Bcv])
                P.dma("sp", bg[:], sc["bg"], reads=[sc["Bbg"]], writes=[Bcv])
                P.dma("sp", cw[:], convw_d[l], writes=[Bcv])
                P.dma("sp", selh[:], selh_d, writes=[Beg])
                for r in range(4):
                    P.dma("sp", eg[:, r, :, :], sc["GE"][r * 4, 0:512].rearrange("(c p j) -> p c j", c=2, p=128, j=2), reads=[sc["BGE"]], writes=[Beg],
                          allow_slow_non_contiguous=True)
                for c in range(2):
                    P.op("dve", "tensor_tensor", reads=[Beg], writes=[Beg], out=egt[:], in0=eg[:, :, c, 1], in1=selh[:, 0:4], op=ALU.mult)
                    P.op("dve", "reduce_sum", reads=[Beg, Bcv], writes=[Bcv], out=cg[:, c, 0:1], in_=egt[:], axis=AX.X)
                    P.op("dve", "tensor_tensor", reads=[Beg], writes=[Beg], out=egt[:], in0=eg[:, :, c, 0], in1=selh[:, 4:8], op=ALU.mult)
                    P.op("dve", "reduce_sum", reads=[Beg, Bcv], writes=[Bcv], out=cg[:, c, NL + 1:NL + 2], in_=egt[:], axis=AX.X)
                segs = [(0, 0, NL)] + ([(NL + 2, NL, LC)] if with_ctx else [])
                for c in range(2):
                    for (u0, o0, n) in segs:
                        P.op("dve", "tensor_scalar", reads=[Bcv], writes=[Bca], out=ca[:, 0:n], in0=cg[:, c, u0 + 1:u0 + 1 + n], scalar1=cw[:, c, 1:2], scalar2=None, op0=ALU.mult)
                        P.op("dve", "scalar_tensor_tensor", reads=[Bcv, Bca], writes=[Bca], out=ca[:, 0:n], in0=cg[:, c, u0:u0 + n], scalar=cw[:, c, 0:1], in1=ca[:, 0:n],
                             op0=ALU.mult, op1=ALU.add)
                        P.op("dve", "scalar_tensor_tensor", reads=[Bcv, Bca], writes=[Bca], out=ca[:, 0:n], in0=cg[:, c, u0 + 2:u0 + 2 + n], scalar=cw[:, c, 2:3], in1=ca[:, 0:n],
                             op0=ALU.mult, op1=ALU.add)
                        P.op("dve", "tensor_tensor", reads=[Bcv, Bca], writes=[Bmixc[c]], out=mixc[:, c, o0:o0 + n], in0=ca[:, 0:n], in1=bg[:, c, o0:o0 + n], op=ALU.mult)
                wna = E.sb("wna", [64, 4, D], BF16, ph)
                wr = E.sb("wr", [128, 6, D], BF16, ph)
                Bw = Buf("wout")
                P.dma("pool", wna[:], wna_d[l], writes=[Bw])
                P.dma("pool", wr[:], wor_d[l], writes=[Bw])
                for ti, (t0, w) in enumerate(tiles_of(NQ)):
                    j = 0 if t0 < NL else 1
                    for m in range(KC):
                        b = E.bank()
                        for kc in range(10):
                            if kc < 4:
                                lh, rh, Br = wna[:, kc, m * 128:(m + 1) * 128], mixn[0:64, kc, t0:t0 + w], Bmixn[kc]
                            elif kc < 8:
                                lh, rh, Br = wr[:, kc - 4, m * 128:(m + 1) * 128], mixd[:, kc - 4, t0:t0 + w], Bmixd[kc - 4]
                            else:
                                lh, rh, Br = wr[:, kc - 4, m * 128:(m + 1) * 128], mixc[:, kc - 8, t0:t0 + w], Bmixc[kc - 8]
                            P.op("pe", "matmul", reads=[Bw, Br], writes=[E.psb[b]], out=E.ps[:, b, 0:w], lhsT=lh, rhs=rh, start=(kc == 0), stop=(kc == 9))
                        P.op("dve", "scalar_tensor_tensor", reads=[E.psb[b], Bmod, Bx[ti]], writes=[Bx[ti]], out=x[:, m, t0:t0 + w], in0=E.ps[:, b, 0:w], scalar=mo[:, l, 16 + m, j:j + 1],
                             in1=x[:, m, t0:t0 + w], op0=ALU.mult, op1=ALU.add)
                prog_barrier(P)

    def stage_L3(l, NTK, moe, final):
        G = 2
        F = D_EXP if moe else D_FF
        NE = NEXP if moe else 1
        w1, w3, w2 = (mw1_d, mw3_d, mw2_d) if moe else (fw1_d, fw3_d, fw2_d)
        tlk = tiles_of(NTK)
        with phase() as ph0:
            hT = E.sb("hT3", [128, KC, NTK], BF16, ph0)
            Bh = [Buf("h%d" % i) for i in range(len(tlk))]
            if moe:
                gatesT = E.sb("gatesT", [8, NTK], F32, ph0)
                Bgt = Buf("gatesT")
                sel = E.sb("sel", [8, 8, 128], F32, ph0)
                Bc = Buf("moeconst")
                P.dma("sp", sel[:], sel_d, writes=[Bc])
            with phase() as ph:
                scr = norm_scr(ph)
                if moe:
                    h32 = E.sb("h32", [128, KC, 512], F32, ph)
                    Bh32 = Buf("h32")
                    wr = E.sb("wrt", [128, KC, NEXP], F32, ph)
                    ident = E.sb("ident", [128, 128], F32, ph)
                    P.dma("sp", wr[:], wrt_d, writes=[Bc])
                    P.dma("sp", ident[:], ident_d, writes=[Bc])
                    lg = E.sb("lg", [128, 8], F32, ph)
                    l2 = E.sb("l2", [128, 8], F32, ph)
                    ew = E.sb("ew", [128, 8], F32, ph)
                    sm = E.sb("sm", [128, 8], F32, ph)
                    Bg = Buf("gate_scr")
                for ti, (t0, w) in enumerate(tlk):
                    j = 0 if t0 < NL else 1
                    emit_norm_mod_tile(E, ones, Bones, x[:, :, t0:t0 + w], Bx[ti], w, mo[:, l, 24:32, j], sc1[:, l, 1, :, j], Bmod,
                                       hT[:, :, t0:t0 + w], Bh[ti], scr,
                                       h32dst=(h32[:, :, 0:w] if moe else None), Bh32=(Bh32 if moe else None))
                    if moe:
                        for blk in range(w // 128):
                            b = E.bank()
                            for k in range(KC):
                                P.op("pe", "matmul", reads=[Bh32, Bc], writes=[E.psb[b]], out=E.ps[:, b, 0:8], lhsT=h32[:, k, blk * 128:(blk + 1) * 128], rhs=wr[:, k, :],
                                     start=(k == 0), stop=(k == KC - 1))
                            P.op("dve", "tensor_copy", reads=[E.psb[b]], writes=[Bg], out=lg[:], in_=E.ps[:, b, 0:8])
                            P.op("dve", "reduce_max", reads=[Bg], writes=[Bg], out=sm[:, 0:1], in_=lg[:], axis=AX.X)
                            P.op("dve", "tensor_scalar", reads=[Bg], writes=[Bg], out=sm[:, 1:2], in0=sm[:, 0:1], scalar1=-1.0, scalar2=None, op0=ALU.mult)
                            P.op("dve", "tensor_scalar", reads=[Bg], writes=[Bg], out=l2[:], in0=lg[:], scalar1=sm[:, 0:1], scalar2=-1e30, op0=ALU.is_equal, op1=ALU.mult)
                            P.op("dve", "tensor_tensor", reads=[Bg], writes=[Bg], out=l2[:], in0=l2[:], in1=lg[:], op=ALU.add)
                            P.op("dve", "reduce_max", reads=[Bg], writes=[Bg], out=sm[:, 2:3], in_=l2[:], axis=AX.X)
                            P.op("dve", "tensor_scalar", reads=[Bg], writes=[Bg], out=l2[:], in0=lg[:], scalar1=sm[:, 2:3], scalar2=None, op0=ALU.is_ge)
                            P.op("act", "activation", reads=[Bg], writes=[Bg], out=ew[:], in_=lg[:], func=AF.Exp, bias=sm[:, 1:2], scale=1.0)
                            P.op("dve", "tensor_tensor", reads=[Bg], writes=[Bg], out=ew[:], in0=ew[:], in1=l2[:], op=ALU.mult)
                            P.op("dve", "reduce_sum", reads=[Bg], writes=[Bg], out=sm[:, 3:4], in_=ew[:], axis=AX.X)
                            P.op("dve", "reciprocal", reads=[Bg], writes=[Bg], out=sm[:, 4:5], in_=sm[:, 3:4])
                            P.op("dve", "tensor_scalar", reads=[Bg], writes=[Bg], out=ew[:], in0=ew[:], scalar1=sm[:, 4:5], scalar2=None, op0=ALU.mult)
                            b2 = E.bank()
                            P.op("pe", "transpose", reads=[Bg, Bc], writes=[E.psb[b2]], out=E.ps[0:8, b2, 0:128], in_=ew[:], identity=ident[:])
                            c0 = t0 + blk * 128
                            P.op("dve", "tensor_copy", reads=[E.psb[b2]], writes=[Bgt], out=gatesT[:, c0:c0 + 128], in_=E.ps[0:8, b2, 0:128])
                prog_barrier(P)
            with phase() as ph:
                NS = 3
                w1s = [E.sb("w1s", [128, KC, 128 * G], BF16, ph) for i in range(NS)]
                w3s = [E.sb("w3s", [128, KC, 128 * G], BF16, ph) for i in range(NS)]
                w2s = [E.sb("w2s", [128, G, D], BF16, ph) for i in range(NS)]
                Bws = [Buf("ws%d" % i) for i in range(NS)]
                ut = [E.sb("ut", [128, G, 512], BF16, ph) for i in range(2)]
                But = [Buf("ut%d" % i) for i in range(2)]
                st_ = [E.sb("st", [128, 512], F32, ph) for i in range(4)]
                Bst = [Buf("st%d" % i) for i in range(4)]
                tt_ = [E.sb("tt", [128, 512], F32, ph) for i in range(4)]
                Btt = [Buf("tt%d" % i) for i in range(4)]
                if moe:
                    gbc = [E.sb("gbc", [128, NTK], F32, ph) for i in range(2)]
                    Bgbc = [Buf("gbc%d" % i) for i in range(2)]
                ngrp = F // (128 * G)
                groups = [(ex, g) for ex in range(NE) for g in range(ngrp)]

                def load_group(gi):
                    ex, g = groups[gi]
                    s = gi % NS
                    P.dma("pool", w1s[s][:], w1[ex, :, :, g * 128 * G:(g + 1) * 128 * G], writes=[Bws[s]])
                    P.dma("pool", w3s[s][:], w3[ex, :, :, g * 128 * G:(g + 1) * 128 * G], writes=[Bws[s]])
                    P.dma("pool", w2s[s][:], w2[ex, :, g * G:(g + 1) * G, :], writes=[Bws[s]])

                load_group(0)
                if len(groups) > 1:
                    load_group(1)
                items = [(gi, ti) for gi in range(len(groups)) for ti in range(len(tlk))]
                dn_rr = [0]

                def up(ii):
                    gi, ti = items[ii]
                    ex, g = groups[gi]
                    s = gi % NS
                    t0, w = tlk[ti]
                    ui = ii % 2
                    if ti == 0:
                        if moe and g == 0:
                            gs = ex % 2
                            for tj, (u0, uw) in enumerate(tlk):
                                b_ = 4 + dn_rr[0] % 4
                                dn_rr[0] += 1
                                P.op("pe", "matmul", reads=[Bc, Bgt], writes=[E.psb[b_]], out=E.ps[:, b_, 0:uw], lhsT=sel[:, ex, :], rhs=gatesT[:, u0:u0 + uw], start=True, stop=True)
                                P.op("act", "activation", reads=[E.psb[b_]], writes=[Bgbc[gs]], out=gbc[gs][:, u0:u0 + uw], in_=E.ps[:, b_, 0:uw], func=AF.Copy)
                    for fc in range(G):
                        ba, bb = 2 * fc, 2 * fc + 1
                        for k in range(KC):
                            P.op("pe", "matmul", reads=[Bws[s], Bh[ti]], writes=[E.psb[ba]], out=E.ps[:, ba, 0:w], lhsT=w1s[s][:, k, fc * 128:(fc + 1) * 128],
                                 rhs=hT[:, k, t0:t0 + w], start=(k == 0), stop=(k == KC - 1))
                        for k in range(KC):
                            P.op("pe", "matmul", reads=[Bws[s], Bh[ti]], writes=[E.psb[bb]], out=E.ps[:, bb, 0:w], lhsT=w3s[s][:, k, fc * 128:(fc + 1) * 128],
                                 rhs=hT[:, k, t0:t0 + w], start=(k == 0), stop=(k == KC - 1))
                        si = (ii % 2) * 2 + fc
                        P.op("act", "activation", reads=[E.psb[ba]], writes=[Bst[si]], out=st_[si][:, 0:w], in_=E.ps[:, ba, 0:w], func=AF.Silu)
                        if moe:
                            gs = ex % 2
                            P.op("dve", "tensor_tensor", reads=[E.psb[bb], Bgbc[gs]], writes=[Btt[si]], out=tt_[si][:, 0:w], in0=E.ps[:, bb, 0:w], in1=gbc[gs][:, t0:t0 + w], op=ALU.mult)
                            P.op("dve", "tensor_tensor", reads=[Btt[si], Bst[si]], writes=[But[ui]], out=ut[ui][:, fc, 0:w], in0=tt_[si][:, 0:w], in1=st_[si][:, 0:w], op=ALU.mult)
                        else:
                            P.op("dve", "tensor_tensor", reads=[E.psb[bb], Bst[si]], writes=[But[ui]], out=ut[ui][:, fc, 0:w], in0=E.ps[:, bb, 0:w], in1=st_[si][:, 0:w], op=ALU.mult)

                def down(ii):
                    gi, ti = items[ii]
                    s = gi % NS
                    t0, w = tlk[ti]
                    ui = ii % 2
                    j = 0 if t0 < NL else 1
                    for m in range(KC):
                        bc = 4 + dn_rr[0] % 4
                        dn_rr[0] += 1
                        for fc in range(G):
                            P.op("pe", "matmul", reads=[Bws[s], But[ui]], writes=[E.psb[bc]], out=E.ps[:, bc, 0:w], lhsT=w2s[s][:, fc, m * 128:(m + 1) * 128],
                                 rhs=ut[ui][:, fc, 0:w], start=(fc == 0), stop=(fc == G - 1))
                        P.op("dve", "scalar_tensor_tensor", reads=[E.psb[bc], Bmod, Bx[ti]], writes=[Bx[ti]], out=x[:, m, t0:t0 + w], in0=E.ps[:, bc, 0:w], scalar=mo[:, l, 40 + m, j:j + 1],
                             in1=x[:, m, t0:t0 + w], op0=ALU.mult, op1=ALU.add)
                    if ti == len(tlk) - 1 and gi + 3 < len(groups):
                        load_group(gi + 3)

                if len(groups) > 2:
                    load_group(2)
                up(0)
                for ii in range(len(items)):
                    if ii + 1 < len(items):
                        up(ii + 1)
                    down(ii)
                prog_barrier(P)
        if final:
            with phase() as ph:
                fg = E.sb("fg", [128, KC], F32, ph)
                sq = E.sb("fsq", [128, KC, 512], BF16, ph)
                rstd = E.sb("frstd", [128, 512], F32, ph)
                Bfg, Bsq, Brstd = Buf("fg"), Buf("fsq"), Buf("frstd")
                P.dma("sp", fg[:], fg_d, writes=[Bfg])
                for ti, (t0, w) in enumerate(tlk):
                    P.op("act", "activation", reads=[Bx[ti]], writes=[Bsq], out=sq[:, :, 0:w], in_=x[:, :, t0:t0 + w], func=AF.Square)
                    b = E.bank()
                    for k in range(KC):
                        P.op("pe", "matmul", reads=[Bones, Bsq], writes=[E.psb[b]], out=E.ps[:, b, 0:w], lhsT=ones[:], rhs=sq[:, k, 0:w], start=(k == 0), stop=(k == KC - 1))
                    P.op("act", "activation", reads=[E.psb[b]], writes=[Brstd], out=rstd[:, 0:w], in_=E.ps[:, b, 0:w], func=AF.Sqrt, bias=EPS, scale=1.0 / D)
                    P.op("dve", "reciprocal", reads=[Brstd], writes=[Brstd], out=rstd[:, 0:w], in_=rstd[:, 0:w])
                    for k in range(KC):
                        P.op("dve", "scalar_tensor_tensor", reads=[Bx[ti], Brstd, Bfg], writes=[Bx[ti]], out=x[:, k, t0:t0 + w], in0=x[:, k, t0:t0 + w], scalar=fg[:, k:k + 1], in1=rstd[:, 0:w],
                             op0=ALU.mult, op1=ALU.mult)
                    P.dma("sp", xo_d[:, :, t0:t0 + w], x[:, :, t0:t0 + w], reads=[Bx[ti]], is_output=True)

    stages = [lambda: stage_L1(0), lambda: stage_L2(0, True), lambda: stage_L3(0, NT, False, False),
              lambda: stage_L1(1), lambda: stage_L2(1, False), lambda: stage_L3(1, NL, True, True)]
    for i in range(nstages):
        stages[i]()
    if nstages < 6:
        for ti, (t0, w) in enumerate(tiles_of(NL)):
            P.dma("sp", xo_d[:, :, t0:t0 + w], x[:, :, t0:t0 + w], reads=[Bx[ti]], is_output=True)
    return E.done()


def fm(a):
    T = a.shape[0]
    return np.ascontiguousarray(a.T.reshape(-1, 128, T).transpose(1, 0, 2))


def unfm(a):
    return np.ascontiguousarray(a.transpose(1, 0, 2).reshape(-1, a.shape[2]).T)


def wl(w):
    K, C = w.shape
    return np.ascontiguousarray(w.reshape(K // 128, 128, C).transpose(1, 0, 2))


def rope_tables(q):
    t = np.arange(q * NL, (q + 1) * NL, dtype=np.int32)
    row = (t // GRID_W).astype(np.float32)
    col = (t % GRID_W).astype(np.float32)
    inv = (np.float32(10000.0) ** (-np.arange(16, dtype=np.float32) / np.float32(16))).astype(np.float32)
    ang = np.concatenate([row[:, None] * inv, col[:, None] * inv], axis=-1).astype(np.float32)
    c = np.cos(ang).astype(np.float32).T
    s = np.sin(ang).astype(np.float32).T
    return np.ascontiguousarray(np.concatenate([c, c], 0)), np.ascontiguousarray(np.concatenate([-s, s], 0))


def rot_cols(w):
    return np.ascontiguousarray(w.reshape(w.shape[0], -1, 2, 32)[:, :, ::-1, :].reshape(w.shape[0], -1))


def make_wall(W):
    qB, kB = W[:, 256:768], W[:, 1024:1536]
    return wl(np.concatenate([W[:, 768:1024], W[:, 1536:1792], kB, rot_cols(kB), W[:, 1792:2304], qB, rot_cols(qB),
                              W[:, 0:256], W[:, 2560:2816], W[:, 2304:2560], W[:, 2816:3072]], axis=1))


def na_bias_table(rpb):
    qc = np.arange(64)
    kc = np.arange(64)
    win_start = np.clip(qc - 8, 0, 48)
    valid = (kc[None, :] >= win_start[:, None]) & (kc[None, :] < win_start[:, None] + 16)
    dcol = np.clip(kc[None, :] - qc[:, None] + 15, 0, 30)
    t = rpb[:, :, dcol]
    t = np.where(valid[None, None], t, np.float32(-1e30)).astype(np.float32)
    return np.ascontiguousarray(t.transpose(2, 1, 0, 3).reshape(64 * 15, 256))


def build_in_maps(inp, nstages=6):
    adaw = np.ascontiguousarray(inp["ada_w"].reshape(2, 8, 128, 6144).transpose(0, 2, 1, 3))
    adab = np.ascontiguousarray(inp["ada_b"].reshape(2, 48, 128).transpose(0, 2, 1))
    wall = np.stack([make_wall(inp["w_in"][l]) for l in range(2)])
    nabc = np.stack([na_bias_table(inp["na_rpb"][l]) for l in range(2)])
    convw = np.stack([np.ascontiguousarray(inp["conv_w"][l].T.reshape(2, 128, 3).transpose(1, 0, 2)) for l in range(2)])
    wna = np.stack([np.ascontiguousarray(inp["w_out"][l][0:256].reshape(4, 64, D).transpose(1, 0, 2)) for l in range(2)])
    wor = np.stack([wl(inp["w_out"][l][256:]) for l in range(2)])
    dlam = np.ascontiguousarray(inp["diff_lambda"].reshape(2, 1, 256))
    subln = np.ascontiguousarray(inp["diff_subln"].reshape(2, 128, 1))
    identb = np.eye(128, dtype=np.float32).astype(NPBF)
    fw1 = wl(inp["ffn_w1"][0])[None]
    fw3 = wl(inp["ffn_w3"][0])[None]
    fw2 = wl(inp["ffn_w2"][0])[None]
    NEI = NEXP if nstages >= 6 else 1
    mw1 = np.stack([wl(inp["moe_w1"][0, e]) for e in range(NEI)])
    mw3 = np.stack([wl(inp["moe_w3"][0, e]) for e in range(NEI)])
    mw2 = np.stack([wl(inp["moe_w2"][0, e]) for e in range(NEI)])
    wrt = wl(inp["router_w"][0])
    sel = np.zeros((8, 8, 128), np.float32)
    for e in range(8):
        sel[e, e, :] = 1.0
    ident = np.eye(128, dtype=np.float32)
    fg = np.ascontiguousarray(inp["final_gain"].reshape(8, 128).T)
    ropes = [rope_tables(q) for q in range(4)]
    maps = []
    for core in range(NCORES):
        b, q = core // 4, core % 4
        cT = np.ascontiguousarray(np.stack([inp["c"][b], inp["c_ctx"]], -1).reshape(8, 128, 2).transpose(1, 0, 2))
        r = 32 * q + np.arange(32)
        rs_ = np.clip(r - 4, 0, 120)
        idxtab = np.zeros((128, 64), np.int32)
        pp = np.arange(128)
        for rl in range(32):
            idxtab[:, rl] = rs_[rl] * 64 + 4 * pp
            idxtab[:, 32 + rl] = (pp % 64) * 15 + (rs_[rl] - r[rl] + 7)
        selh = np.zeros((128, 8), np.float32)
        if q > 0:
            selh[:, q - 1] = 1.0
        if q < 3:
            selh[:, 4 + q + 1] = 1.0
        maps.append({
            "xT": fm(np.concatenate([inp["x"][b, q * NL:(q + 1) * NL], inp["ctx"][b]], 0)), "cT": cT, "adaw": adaw, "adab": adab, "wall": wall,
            "ropeC": ropes[q][0], "ropeS": ropes[q][1], "nabc0": nabc[0], "nabc1": nabc[1], "idxtab": idxtab, "selh": selh, "convw": convw, "wout_na": wna, "wout_r": wor,
            "dlam": dlam, "subln": subln, "identb": identb, "fw1": fw1, "fw3": fw3, "fw2": fw2, "mw1": mw1, "mw3": mw3, "mw2": mw2, "wrt": wrt,
            "sel": sel, "ident": ident, "fgain": fg,
        })
    return maps


_NC = []


def kernel(x, c, ctx, c_ctx, ada_w, ada_b, w_in, w_out, na_rpb, diff_lambda, diff_subln, conv_w,
           ffn_w1, ffn_w3, ffn_w2, router_w, moe_w1, moe_w3, moe_w2, final_gain):
    inp = {k: np.asarray(v) for k, v in dict(
        x=x, c=c, ctx=ctx, c_ctx=c_ctx, ada_w=ada_w, ada_b=ada_b, w_in=w_in, w_out=w_out, na_rpb=na_rpb,
        diff_lambda=diff_lambda, diff_subln=diff_subln, conv_w=conv_w, ffn_w1=ffn_w1, ffn_w3=ffn_w3, ffn_w2=ffn_w2,
        router_w=router_w, moe_w1=moe_w1, moe_w3=moe_w3, moe_w2=moe_w2, final_gain=final_gain).items()}
    import os
    nst = int(os.environ.get("KF_NSTAGES", "6"))
    if not _NC:
        _NC.append(build_fused(nst))
    res = run_bass_kernel_spmd(_NC[0], build_in_maps(inp, nst), core_ids=list(range(NCORES)))
    out = np.zeros((2, 4 * NL, D), np.float32)
    for core in range(NCORES):
        b, q = core // 4, core % 4
        out[b, q * NL:(q + 1) * NL] = unfm(np.asarray(res.results[core]["xo"]))
    return out
```

```python
import contextlib
import math
import numpy as np
import ml_dtypes
import concourse.bass as bass
import concourse.mybir as mybir
from concourse.bass_utils import run_bass_kernel_spmd

F32 = mybir.dt.float32
BF16 = mybir.dt.bfloat16
ALU = mybir.AluOpType
AF = mybir.ActivationFunctionType
AX = mybir.AxisListType
NPBF = ml_dtypes.bfloat16

ENG_NAMES = ("pe", "act", "dve", "pool", "sp")

D = 1024
KC = 8
NL = 2048
LC = 256
NCORES = 8
GRID_W = 64
EPS = 1e-6
D_FF = 2816
D_EXP = 3584
NEXP = 8


class Buf:
    __slots__ = ("name", "w", "r")

    def __init__(self, name=""):
        self.name = name
        self.w = None
        self.r = []


class Prog:
    N_DMA_SEMS = 24

    def __init__(self, nc):
        self.nc = nc
        self.ops = {e: [] for e in ENG_NAMES}
        self.seen = {e: {} for e in ENG_NAMES}
        self.dma_uses = [0] * self.N_DMA_SEMS
        self.dma_rr = {}
        self.out_tokens = []
        self.n_coll = 0

    def _dma_slot(self, q):
        half = self.N_DMA_SEMS // 2
        lo = half if q == "pool" else 0
        k = self.dma_rr.get(q, 0)
        self.dma_rr[q] = k + 1
        return lo + k % half

    def _collect(self, reads, writes):
        deps = []
        for b in reads:
            if b.w is not None:
                deps.append(b.w)
        for b in writes:
            if b.w is not None:
                deps.append(b.w)
            deps.extend(b.r)
        return deps

    def _filter(self, e, deps):
        seen = self.seen[e]
        out = {}
        for t in deps:
            if t[0] == "c":
                _, f, seq = t
                if f == e and e == "pe":
                    continue
                key = ("c", f)
                val = seq
            else:
                _, j, val = t
                key = (t[0], j)
            if seen.get(key, 0) >= val:
                continue
            if out.get(key, 0) < val:
                out[key] = val
        for k, v in out.items():
            seen[k] = v
            if k[0] == "c":
                self.ops[k[1]][v - 1]["inc"] = True
        return list(out.items())

    def op(self, e, meth, reads=(), writes=(), **kw):
        fn = (meth, kw)
        deps = self._collect(reads, writes)
        waits = self._filter(e, deps)
        lst = self.ops[e]
        seq = len(lst) + 1
        lst.append({"fn": fn, "waits": waits, "inc": False, "dma": None})
        tok = ("c", e, seq)
        for b in writes:
            b.w = tok
            b.r = []
        for b in reads:
            b.r.append(tok)
        return tok

    def dma(self, q, out, in_, reads=(), writes=(), is_output=False, **kw):
        deps = self._collect(reads, writes)
        j = self._dma_slot(q)
        prev = self.dma_uses[j] * 16
        if prev:
            deps.append(("d", j, prev))
        waits = self._filter(q, deps)
        self.dma_uses[j] += 1
        val = self.dma_uses[j] * 16
        kw = dict(kw)
        kw["out"] = out
        kw["in_"] = in_
        self.ops[q].append({"fn": ("dma_start", kw),
                            "waits": waits, "inc": False, "dma": j})
        tok = ("d", j, val)
        for b in writes:
            b.w = tok
            b.r = []
        for b in reads:
            b.r.append(tok)
        if is_output:
            self.out_tokens.append(tok)
        return tok

    def custom(self, e, fn, reads=(), writes=(), dma=False):
        if not dma:
            deps = self._collect(reads, writes)
            waits = self._filter(e, deps)
            lst = self.ops[e]
            seq = len(lst) + 1
            lst.append({"fn": ("__custom__", fn), "waits": waits, "inc": False, "dma": None})
            tok = ("c", e, seq)
        else:
            deps = self._collect(reads, writes)
            j = self._dma_slot(e)
            prev = self.dma_uses[j] * 16
            if prev:
                deps.append(("d", j, prev))
            waits = self._filter(e, deps)
            self.dma_uses[j] += 1
            self.ops[e].append({"fn": ("__custom__", fn), "waits": waits, "inc": False, "dma": j})
            tok = ("d", j, self.dma_uses[j] * 16)
        for b in writes:
            b.w = tok
            b.r = []
        for b in reads:
            b.r.append(tok)
        return tok

    def collective(self, reads, writes, **kw):
        deps = self._collect(reads, writes)
        waits = self._filter("pool", deps)
        j = self.n_coll
        self.n_coll += 1
        self.ops["pool"].append({"fn": ("collective_compute", kw), "waits": waits, "inc": False, "dma": None, "coll": j})
        tok = ("x", j, 1)
        for b in writes:
            b.w = tok
            b.r = []
        for b in reads:
            b.r.append(tok)
        return tok

    def finish(self):
        waits = self._filter("sp", list(self.out_tokens))
        self.ops["sp"].append({"fn": None, "waits": waits, "inc": False, "dma": None})

    def emit(self):
        nc = self.nc
        with contextlib.ExitStack() as st:
            csem = {e: st.enter_context(nc.semaphore("c_" + e)) for e in ENG_NAMES}
            dsem = [st.enter_context(nc.semaphore("d%d" % j)) for j in range(self.N_DMA_SEMS)]
            xsem = [st.enter_context(nc.semaphore("x%d" % j)) for j in range(self.n_coll)]
            cum = {}
            for e in ENG_NAMES:
                c = 0
                arr = []
                for o in self.ops[e]:
                    if o["inc"]:
                        c += 1
                    arr.append(c)
                cum[e] = arr

            def run(e, eng):
                for o in self.ops[e]:
                    for key, val in o["waits"]:
                        if key[0] == "c":
                            eng.wait_ge(csem[key[1]], cum[key[1]][val - 1])
                        elif key[0] == "x":
                            eng.wait_ge(xsem[key[1]], val)
                        else:
                            eng.wait_ge(dsem[key[1]], val)
                    if o["fn"] is None:
                        continue
                    if o["fn"][0] == "__custom__":
                        ins = o["fn"][1](eng)
                    else:
                        ins = getattr(eng, o["fn"][0])(**o["fn"][1])
                    if o.get("coll") is not None:
                        ins.then_inc(xsem[o["coll"]], 1)
                    elif o["dma"] is not None:
                        ins.then_inc(dsem[o["dma"]], 16)
                    elif o["inc"]:
                        ins.then_inc(csem[e], 1)

            with nc.Block() as block:
                @block.tensor
                def _(eng):
                    run("pe", eng)

                @block.scalar
                def _(eng):
                    run("act", eng)

                @block.vector
                def _(eng):
                    run("dve", eng)

                @block.gpsimd
                def _(eng):
                    run("pool", eng)

                @block.sync
                def _(eng):
                    run("sp", eng)


class Env:
    def __init__(self):
        self.nc = bass.Bass("TRN2", target_bir_lowering=False)
        self.P = Prog(self.nc)
        self.st = contextlib.ExitStack()
        self.names = set()
        self.psb = None
        self.ps_rr = 0
        self.uid = 0

    def din(self, name, shape, dt=F32):
        return self.nc.dram_tensor(name, list(shape), dt, kind="ExternalInput").ap()

    def dout(self, name, shape, dt=F32):
        return self.nc.dram_tensor(name, list(shape), dt, kind="ExternalOutput").ap()

    def sb(self, name, shape, dt=F32, st=None):
        self.uid += 1
        return (st or self.st).enter_context(self.nc.sbuf_tensor("s_%s_%d" % (name, self.uid), list(shape), dt))

    def dint(self, name, shape, dt=F32):
        return self.nc.dram_tensor(name, list(shape), dt).ap()

    def psum(self):
        self.ps = self.st.enter_context(self.nc.psum_tensor("ps", [128, 8, 512], F32))
        self.psb = [Buf("ps%d" % i) for i in range(8)]
        return self.ps

    def bank(self):
        i = self.ps_rr
        self.ps_rr = (self.ps_rr + 1) % 8
        return i

    def done(self):
        self.P.finish()
        self.P.emit()
        self.st.close()
        return self.nc


def tiles_of(nt):
    out = []
    t = 0
    while t < nt:
        w = min(512, nt - t)
        out.append((t, w))
        t += w
    return out


def emit_consts(E):
    P = E.P
    ones = E.sb("ones_bf", [128, 128], BF16)
    Bones = Buf("ones")
    P.op("pool", "memset", writes=[Bones], ap=ones[:], constant=1.0)
    return ones, Bones


def emit_norm_mod_tile(E, ones, Bones, xsrc, Bx, w, sh, sc1, Bmod, hdst, Bh, scr, h32dst=None, Bh32=None):
    P = E.P
    sq, Bsq = scr["sq"], scr["Bsq"]
    rstd, Brstd = scr["rstd"], scr["Brstd"]
    tmp, Btmp = scr["tmp"], scr["Btmp"]
    P.op("act", "activation", reads=[Bx], writes=[Bsq], out=sq[:, :, 0:w], in_=xsrc, func=AF.Square)
    b = E.bank()
    for k in range(KC):
        P.op("pe", "matmul", reads=[Bones, Bsq], writes=[E.psb[b]], out=E.ps[:, b, 0:w], lhsT=ones[:], rhs=sq[:, k, 0:w], start=(k == 0), stop=(k == KC - 1))
    P.op("act", "activation", reads=[E.psb[b]], writes=[Brstd], out=rstd[:, 0:w], in_=E.ps[:, b, 0:w], func=AF.Sqrt, bias=EPS, scale=1.0 / D)
    P.op("dve", "reciprocal", reads=[Brstd], writes=[Brstd], out=rstd[:, 0:w], in_=rstd[:, 0:w])
    for k in range(KC):
        i = k % 2
        P.op("dve", "scalar_tensor_tensor", reads=[Bx, Brstd, Bmod], writes=[Btmp[i]], out=tmp[i][:, 0:w], in0=xsrc[:, k, :], scalar=sc1[:, k:k + 1], in1=rstd[:, 0:w],
                                                                op0=ALU.mult, op1=ALU.mult)
        if h32dst is not None:
            P.op("pool", "tensor_scalar", reads=[Btmp[i], Bmod], writes=[Bh32], out=h32dst[:, k, :], in0=tmp[i][:, 0:w], scalar1=sh[:, k:k + 1], scalar2=None, op0=ALU.add)
        P.op("act", "activation", reads=[Btmp[i], Bmod], writes=[Bh], out=hdst[:, k, :], in_=tmp[i][:, 0:w], func=AF.Identity, bias=sh[:, k:k + 1], scale=1.0)


def norm_scratch(E):
    scr = {}
    scr["sq"] = E.sb("n_sq", [128, KC, 512], BF16)
    scr["Bsq"] = Buf("sq")
    scr["rstd"] = E.sb("n_rstd", [128, 512], F32)
    scr["Brstd"] = Buf("rstd")
    scr["tmp"] = [E.sb("n_tmp%d" % i, [128, 512], F32) for i in range(2)]
    scr["Btmp"] = [Buf("tmp%d" % i) for i in range(2)]
    return scr


def emit_load_mod(E, mod_d):
    P = E.P
    mod = E.sb("mod", [128, 48, 2], F32)
    sc1a = E.sb("sc1a", [128, 8, 2], F32)
    sc1f = E.sb("sc1f", [128, 8, 2], F32)
    Bmod = Buf("mod")
    P.dma("sp", mod[:], mod_d, writes=[Bmod])
    P.op("dve", "tensor_scalar", reads=[Bmod], writes=[Bmod], out=sc1a[:], in0=mod[:, 8:16, :], scalar1=1.0, scalar2=None, op0=ALU.add)
    P.op("dve", "tensor_scalar", reads=[Bmod], writes=[Bmod], out=sc1f[:], in0=mod[:, 32:40, :], scalar1=1.0, scalar2=None, op0=ALU.add)
    return mod, sc1a, sc1f, Bmod


I32 = mybir.dt.int32
NT = NL + LC
NKB = 66
NKEY = NKB * 128
GROWS = 513


def prog_barrier(P):
    toks = []
    for f in ("pe", "act", "dve", "pool"):
        lst = P.ops[f]
        for idx in range(len(lst) - 1, -1, -1):
            if lst[idx]["fn"] is not None and lst[idx]["dma"] is None and lst[idx].get("coll") is None:
                toks.append(("c", f, idx + 1))
                break
    for j in range(P.N_DMA_SEMS):
        if P.dma_uses[j]:
            toks.append(("d", j, P.dma_uses[j] * 16))
    for e in ENG_NAMES:
        waits = P._filter(e, list(toks))
        P.ops[e].append({"fn": None, "waits": waits, "inc": False, "dma": None})


def build_fused(nstages=6):
    E = Env()
    P = E.P
    nc = E.nc
    xT_d = E.din("xT", [128, KC, NT])
    cT_d = E.din("cT", [128, KC, 2])
    adaw_d = E.din("adaw", [2, 128, KC, 6144])
    adab_d = E.din("adab", [2, 128, 48])
    wall_d = E.din("wall", [2, 128, KC, 4096])
    ropeC_d = E.din("ropeC", [64, NL])
    ropeS_d = E.din("ropeS", [64, NL])
    bc_d = [E.din("nabc%d" % i, [64 * 15, 256]) for i in range(2)]
    idxtab_d = E.din("idxtab", [128, 64], I32)
    selh_d = E.din("selh", [128, 8])
    convw_d = E.din("convw", [2, 128, 2, 3])
    wna_d = E.din("wout_na", [2, 64, 4, D])
    wor_d = E.din("wout_r", [2, 128, 6, D])
    dlam_d = E.din("dlam", [2, 1, 256])
    subln_d = E.din("subln", [2, 128, 1])
    identb_d = E.din("identb", [128, 128], BF16)
    fw1_d = E.din("fw1", [1, 128, KC, D_FF])
    fw3_d = E.din("fw3", [1, 128, KC, D_FF])
    fw2_d = E.din("fw2", [1, 128, D_FF // 128, D])
    NEI = NEXP if nstages >= 6 else 1
    mw1_d = E.din("mw1", [NEI, 128, KC, D_EXP])
    mw3_d = E.din("mw3", [NEI, 128, KC, D_EXP])
    mw2_d = E.din("mw2", [NEI, 128, D_EXP // 128, D])
    wrt_d = E.din("wrt", [128, KC, NEXP])
    sel_d = E.din("sel", [8, 8, 128])
    ident_d = E.din("ident", [128, 128])
    fg_d = E.din("fgain", [128, KC])
    xo_d = E.dout("xo", [128, KC, NL])
    SC = []
    for l in range(2):
        sc = {}
        for nm in ("AK", "AV", "V0", "V1"):
            sc["blob" + nm] = E.dint("blob%s%d" % (nm, l), [NL, 256], BF16)
            sc["G" + nm] = E.dint("G%s%d" % (nm, l), [4 * NL, 256], BF16)
        for nm in ("B0", "B1"):
            sc["blob" + nm] = E.dint("blob%s%d" % (nm, l), [256, NL], BF16)
            sc["G" + nm] = E.dint("G%s%d" % (nm, l), [4 * 256, NL], BF16)
        sc["blobE"] = E.dint("blobE%d" % l, [4, 512], BF16)
        sc["GE"] = E.dint("GE%d" % l, [16, 512], BF16)
        sc["qb"] = E.dint("qb_s%d" % l, [8, 64, NT], BF16)
        sc["qa"] = E.dint("qa_s%d" % l, [128, 2, NT], BF16)
        sc["kvac"] = E.dint("kvac_s%d" % l, [LC, 512], BF16)
        sc["kbc"] = E.dint("kbc_s%d" % l, [8, 64, LC], BF16)
        sc["vbc"] = E.dint("vbc_s%d" % l, [LC, 512], BF16)
        sc["cgx"] = E.dint("cgx_s%d" % l, [128, 2, NT], F32)
        sc["bg"] = E.dint("bg_s%d" % l, [128, 2, NT], F32)
        for k in list(sc.keys()):
            sc["B" + k] = Buf(k)
        SC.append(sc)
    E.psum()
    ps_bf = E.ps.bitcast(BF16)
    ones, Bones = emit_consts(E)
    tl = tiles_of(NT)
    x = E.sb("x", [128, KC, NT], F32)
    Bx = [Buf("x%d" % i) for i in range(len(tl))]
    for ti, (t0, w) in enumerate(tl):
        P.dma("sp", x[:, :, t0:t0 + w], xT_d[:, :, t0:t0 + w], writes=[Bx[ti]])
    mo = E.sb("mo", [128, 2, 48, 2], F32)
    sc1 = E.sb("sc1", [128, 2, 2, 8, 2], F32)
    Bmod = Buf("mod")
    idb = E.sb("idb", [128, 128], BF16)
    Bid = Buf("idb")
    P.dma("sp", idb[:], identb_d, writes=[Bid])

    def phase():
        return contextlib.ExitStack()

    with phase() as ph:
        cf = E.sb("cf", [128, KC, 2], F32, ph)
        cs = E.sb("cs", [128, KC, 2], BF16, ph)
        ab = E.sb("ab", [128, 2, 48], F32, ph)
        slots = [E.sb("aw", [128, KC, 2048], BF16, ph) for i in range(3)]
        Bs = [Buf("aw%d" % i) for i in range(3)]
        Bc, Bab = Buf("c"), Buf("ab")
        P.dma("sp", cf[:], cT_d, writes=[Bc])
        for l in range(2):
            P.dma("sp", ab[:, l, :], adab_d[l], writes=[Bab])
        P.op("act", "activation", reads=[Bc], writes=[Bc], out=cs[:], in_=cf[:], func=AF.Silu)
        for l in range(2):
            b = E.bank()
            for th in range(3):
                s = (l * 3 + th) % 3
                P.dma("pool", slots[s][:], adaw_d[l, :, :, th * 2048:(th + 1) * 2048], writes=[Bs[s]])
                for m in range(16):
                    mc = th * 16 + m
                    for k in range(KC):
                        P.op("pe", "matmul", reads=[Bs[s], Bc], writes=[E.psb[b]], out=E.ps[:, b, 2 * mc:2 * mc + 2], lhsT=slots[s][:, k, m * 128:(m + 1) * 128],
                             rhs=cs[:, k, :], start=(k == 0), stop=(k == KC - 1))
            for j in range(2):
                P.op("dve", "tensor_tensor", reads=[E.psb[b], Bab], writes=[Bmod], out=mo[:, l, :, j], in0=E.ps[:, b, j:96:2], in1=ab[:, l, :], op=ALU.add)
            P.op("dve", "tensor_scalar", reads=[Bmod], writes=[Bmod], out=sc1[:, l, 0, :, :], in0=mo[:, l, 8:16, :], scalar1=1.0, scalar2=None, op0=ALU.add)
            P.op("dve", "tensor_scalar", reads=[Bmod], writes=[Bmod], out=sc1[:, l, 1, :, :], in0=mo[:, l, 32:40, :], scalar1=1.0, scalar2=None, op0=ALU.add)
        prog_barrier(P)

    def norm_scr(ph):
        scr = {}
        scr["sq"] = E.sb("n_sq", [128, KC, 512], BF16, ph)
        scr["Bsq"] = Buf("sq")
        scr["rstd"] = E.sb("n_rstd", [128, 512], F32, ph)
        scr["Brstd"] = Buf("rstd")
        scr["tmp"] = [E.sb("n_tmp", [128, 512], F32, ph) for i in range(2)]
        scr["Btmp"] = [Buf("tmp%d" % i) for i in range(2)]
        return scr

    evac_rr = [0]

    def evac_copy(dst, src, reads, writes):
        if evac_rr[0] % 2 == 0:
            P.op("act", "activation", reads=reads, writes=writes, out=dst, in_=src, func=AF.Copy)
        else:
            P.op("dve", "tensor_copy", reads=reads, writes=writes, out=dst, in_=src)
        evac_rr[0] += 1

    def stage_L1(l):
        sc = SC[l]
        with phase() as ph0:
            hT = E.sb("hT", [128, KC, NT], BF16, ph0)
            Bh = [Buf("h%d" % i) for i in range(len(tl))]
            with phase() as ph:
                scr = norm_scr(ph)
                for ti, (t0, w) in enumerate(tl):
                    j = 0 if t0 < NL else 1
                    emit_norm_mod_tile(E, ones, Bones, x[:, :, t0:t0 + w], Bx[ti], w, mo[:, l, 0:8, j], sc1[:, l, 0, :, j], Bmod,
                                       hT[:, :, t0:t0 + w], Bh[ti], scr)
                prog_barrier(P)
            with phase() as ph:
                rC = E.sb("rC", [64, NL], F32, ph)
                rS = E.sb("rS", [64, NL], F32, ph)
                Brope = Buf("rope")
                P.dma("sp", rC[:], ropeC_d, writes=[Brope])
                P.dma("sp", rS[:], ropeS_d, writes=[Brope])
                NW = 3
                wsl = [E.sb("wsl", [128, KC, 512], BF16, ph) for i in range(NW)]
                Bw = [Buf("wsl%d" % i) for i in range(NW)]
                stg = [E.sb("stg", [128, NT], F32, ph) for i in range(2)]
                Bstg = [Buf("stg%d" % i) for i in range(2)]
                t1 = [E.sb("rt", [64, 512], F32, ph) for i in range(4)]
                Bt1 = [Buf("rt%d" % i) for i in range(4)]
                xin_t = [E.sb("xin", [128, 512], F32, ph) for i in range(2)]
                Bxin = [Buf("xin%d" % i) for i in range(2)]
                kvst = E.sb("kvst", [128, 9, 512], BF16, ph)
                Bkvst = Buf("kvst")
                egw = E.sb("egw", [128, 2, 2], BF16, ph)
                Begw = Buf("egw")
                stg_rr = [0]
                wrr = [0]

                def load_slab(s):
                    slot = wrr[0] % NW
                    wrr[0] += 1
                    P.dma("pool", wsl[slot][:], wall_d[l, :, :, s * 512:(s + 1) * 512], writes=[Bw[slot]])
                    return slot

                def next_stage():
                    i = stg_rr[0]
                    stg_rr[0] = (i + 1) % 2
                    return i

                def roped(sa, sr, dests):
                    for hm in range(8):
                        si = next_stage()
                        sview = stg[si].bitcast(BF16)
                        for ti, (t0, w) in enumerate(tl):
                            ba = E.bank()
                            for k in range(KC):
                                P.op("pe", "matmul", reads=[Bw[sa], Bh[ti]], writes=[E.psb[ba]], out=E.ps[0:64, ba, 0:w], lhsT=wsl[sa][:, k, hm * 64:(hm + 1) * 64],
                                     rhs=hT[:, k, t0:t0 + w], start=(k == 0), stop=(k == KC - 1))
                            if t0 < NL:
                                bb = E.bank()
                                for k in range(KC):
                                    P.op("pe", "matmul", reads=[Bw[sr], Bh[ti]], writes=[E.psb[bb]], out=E.ps[0:64, bb, 0:w], lhsT=wsl[sr][:, k, hm * 64:(hm + 1) * 64],
                                         rhs=hT[:, k, t0:t0 + w], start=(k == 0), stop=(k == KC - 1))
                                ia = (2 * ti) % 4
                                ib = (2 * ti + 1) % 4
                                P.op("dve", "tensor_tensor", reads=[E.psb[ba], Brope], writes=[Bt1[ia]], out=t1[ia][:, 0:w], in0=E.ps[0:64, ba, 0:w], in1=rC[:, t0:t0 + w], op=ALU.mult)
                                P.op("dve", "tensor_tensor", reads=[E.psb[bb], Brope], writes=[Bt1[ib]], out=t1[ib][:, 0:w], in0=E.ps[0:64, bb, 0:w], in1=rS[:, t0:t0 + w], op=ALU.mult)
                                P.op("pool", "tensor_tensor", reads=[Bt1[ia], Bt1[ib]], writes=[Bstg[si]], out=sview[0:64, t0:t0 + w], in0=t1[ia][:, 0:w], in1=t1[ib][:, 0:w], op=ALU.add)
                            else:
                                evac_copy(sview[0:64, t0:t0 + w], E.ps[0:64, ba, 0:w], [E.psb[ba]], [Bstg[si]])
                        for (dap, c0, ncol, Bd) in dests(hm):
                            P.dma("sp", dap, sview[0:64, c0:c0 + ncol], reads=[Bstg[si]], writes=[Bd])

                def plain_chunk(slot, c, dap, Bd, dt):
                    si = next_stage()
                    sview = stg[si].bitcast(BF16) if dt == BF16 else stg[si]
                    for ti, (t0, w) in enumerate(tl):
                        b = E.bank()
                        for k in range(KC):
                            P.op("pe", "matmul", reads=[Bw[slot], Bh[ti]], writes=[E.psb[b]], out=E.ps[:, b, 0:w], lhsT=wsl[slot][:, k, c * 128:(c + 1) * 128],
                                 rhs=hT[:, k, t0:t0 + w], start=(k == 0), stop=(k == KC - 1))
                        evac_copy(sview[:, t0:t0 + w], E.ps[:, b, 0:w], [E.psb[b]], [Bstg[si]])
                    P.dma("sp", dap, sview[:, 0:NT], reads=[Bstg[si]], writes=[Bd])

                def tokmajor(slot, nm0, nm1, ctx_ap, Bctx):
                    bv = [sc["blob" + nm0].rearrange("(j p) c -> p j c", p=128), sc["blob" + nm1].rearrange("(j p) c -> p j c", p=128)]
                    Bb = [sc["Bblob" + nm0], sc["Bblob" + nm1]]
                    ctx_v = ctx_ap.rearrange("(j p) c -> p j c", p=128)
                    for half in range(2):
                        for jj in range(9):
                            blk = half * 9 + jj
                            b = E.bank()
                            ti = min(blk // 4, 4)
                            for k in range(KC):
                                P.op("pe", "matmul", reads=[Bw[slot], Bh[ti]], writes=[E.psb[b]], out=E.ps[:, b, :], lhsT=hT[:, k, blk * 128:(blk + 1) * 128],
                                     rhs=wsl[slot][:, k, :], start=(k == 0), stop=(k == KC - 1))
                            evac_copy(kvst[:, jj, :], E.ps[:, b, :], [E.psb[b]], [Bkvst])
                        for hh in range(2):
                            if half == 0:
                                P.dma("sp", bv[hh][:, 0:9, :], kvst[:, 0:9, hh * 256:(hh + 1) * 256], reads=[Bkvst], writes=[Bb[hh]])
                            else:
                                P.dma("sp", bv[hh][:, 9:16, :], kvst[:, 0:7, hh * 256:(hh + 1) * 256], reads=[Bkvst], writes=[Bb[hh]])
                        if half == 1:
                            P.dma("sp", ctx_v[:, 0:2, :], kvst[:, 7:9, :], reads=[Bkvst], writes=[Bctx])

                sa = load_slab(1)
                sr = load_slab(2)
                roped(sa, sr, lambda hm: [(sc["blobB%d" % (hm // 4)][(hm % 4) * 64:(hm % 4 + 1) * 64, :], 0, NL, sc["BblobB%d" % (hm // 4)]),
                                          (sc["kbc"][hm], NL, LC, sc["Bkbc"])])
                AGKW = dict(kind="AllGather", op=ALU.bypass, replica_groups=[[0, 1, 2, 3], [4, 5, 6, 7]])
                for nm in ("B0", "B1"):
                    P.collective(reads=[sc["Bblob" + nm]], writes=[sc["BG" + nm]], ins=[sc["blob" + nm]], outs=[sc["G" + nm]], **AGKW)
                s7 = load_slab(7)
                for c in range(2):
                    si = next_stage()
                    for ti, (t0, w) in enumerate(tl):
                        bx = E.bank()
                        for k in range(KC):
                            P.op("pe", "matmul", reads=[Bw[s7], Bh[ti]], writes=[E.psb[bx]], out=E.ps[:, bx, 0:w], lhsT=wsl[s7][:, k, c * 128:(c + 1) * 128],
                                 rhs=hT[:, k, t0:t0 + w], start=(k == 0), stop=(k == KC - 1))
                        bc = E.bank()
                        for k in range(KC):
                            P.op("pe", "matmul", reads=[Bw[s7], Bh[ti]], writes=[E.psb[bc]], out=E.ps[:, bc, 0:w], lhsT=wsl[s7][:, k, (2 + c) * 128:(3 + c) * 128],
                                 rhs=hT[:, k, t0:t0 + w], start=(k == 0), stop=(k == KC - 1))
                        xi = ti % 2
                        P.op("act", "activation", reads=[E.psb[bx]], writes=[Bxin[xi]], out=xin_t[xi][:, 0:w], in_=E.ps[:, bx, 0:w], func=AF.Copy)
                        P.op("dve", "tensor_tensor", reads=[E.psb[bc], Bxin[xi]], writes=[Bstg[si]], out=stg[si][:, t0:t0 + w], in0=E.ps[:, bc, 0:w], in1=xin_t[xi][:, 0:w], op=ALU.mult)
                    P.op("dve", "tensor_copy", reads=[Bstg[si]], writes=[Begw], out=egw[:, c, 0:1], in_=stg[si][:, 0:1])
                    P.op("dve", "tensor_copy", reads=[Bstg[si]], writes=[Begw], out=egw[:, c, 1:2], in_=stg[si][:, NL - 1:NL])
                    P.dma("sp", sc["cgx"][:, c, :], stg[si][:, 0:NT], reads=[Bstg[si]], writes=[sc["Bcgx"]])
                P.dma("sp", sc["blobE"][0, 0:512].rearrange("(c p j) -> p c j", c=2, p=128, j=2), egw[:], reads=[Begw], writes=[sc["BblobE"]],
                      allow_slow_non_contiguous=True)
                P.collective(reads=[sc["BblobE"]], writes=[sc["BGE"]], ins=[sc["blobE"]], outs=[sc["GE"]], **AGKW)
                s3 = load_slab(3)
                tokmajor(s3, "V0", "V1", sc["vbc"], sc["Bvbc"])
                for nm in ("V0", "V1"):
                    P.collective(reads=[sc["Bblob" + nm]], writes=[sc["BG" + nm]], ins=[sc["blob" + nm]], outs=[sc["G" + nm]], **AGKW)
                sa = load_slab(4)
                sr = load_slab(5)
                roped(sa, sr, lambda hm: [(sc["qb"][hm], 0, NT, sc["Bqb"])])
                s0 = load_slab(0)
                tokmajor(s0, "AK", "AV", sc["kvac"], sc["Bkvac"])
                for nm in ("AK", "AV"):
                    P.collective(reads=[sc["Bblob" + nm]], writes=[sc["BG" + nm]], ins=[sc["blob" + nm]], outs=[sc["G" + nm]], **AGKW)
                s6 = load_slab(6)
                plain_chunk(s6, 0, sc["qa"][:, 0, :], sc["Bqa"], BF16)
                plain_chunk(s6, 1, sc["qa"][:, 1, :], sc["Bqa"], BF16)
                plain_chunk(s6, 2, sc["bg"][:, 0, :], sc["Bbg"], F32)
                plain_chunk(s6, 3, sc["bg"][:, 1, :], sc["Bbg"], F32)
                prog_barrier(P)

    def stage_L2(l, with_ctx):
        sc = SC[l]
        NQ = NT if with_ctx else NL
        lambda_init = 0.8 - 0.6 * math.exp(-0.3 * l)
        with phase() as ph0:
            mixd = E.sb("mixd", [128, 4, NQ], BF16, ph0)
            Bmixd = [Buf("mixd%d" % i) for i in range(4)]
            dl = E.sb("dl", [1, 256], F32, ph0)
            lt = E.sb("lt", [1, 128], F32, ph0)
            ls = E.sb("ls", [1, 4], F32, ph0)
            ones1 = E.sb("ones1", [1, 128], F32, ph0)
            nl = E.sb("nl", [128, 1], F32, ph0)
            gsc = E.sb("gsc", [128, 1], F32, ph0)
            Bl = Buf("lam")
            P.dma("sp", dl[:], dlam_d[l], writes=[Bl])
            P.dma("sp", gsc[:], subln_d[l], writes=[Bl])
            P.op("pool", "memset", writes=[Bl], ap=ones1[:], constant=1.0)
            P.op("dve", "tensor_tensor", reads=[Bl], writes=[Bl], out=lt[:, 0:64], in0=dl[:, 0:64], in1=dl[:, 64:128], op=ALU.mult)
            P.op("dve", "tensor_tensor", reads=[Bl], writes=[Bl], out=lt[:, 64:128], in0=dl[:, 128:192], in1=dl[:, 192:256], op=ALU.mult)
            P.op("dve", "reduce_sum", reads=[Bl], writes=[Bl], out=ls[:, 0:1], in_=lt[:, 0:64], axis=AX.X)
            P.op("dve", "reduce_sum", reads=[Bl], writes=[Bl], out=ls[:, 1:2], in_=lt[:, 64:128], axis=AX.X)
            P.op("act", "activation", reads=[Bl], writes=[Bl], out=ls[:, 0:2], in_=ls[:, 0:2], func=AF.Exp)
            P.op("dve", "tensor_tensor", reads=[Bl], writes=[Bl], out=ls[:, 2:3], in0=ls[:, 1:2], in1=ls[:, 0:1], op=ALU.subtract)
            P.op("dve", "tensor_scalar", reads=[Bl], writes=[Bl], out=ls[:, 3:4], in0=ls[:, 2:3], scalar1=-lambda_init, scalar2=None, op0=ALU.add)
            b = E.bank()
            P.op("pe", "matmul", reads=[Bl], writes=[E.psb[b]], out=E.ps[:, b, 0:1], lhsT=ones1[:], rhs=ls[:, 3:4], start=True, stop=True)
            P.op("dve", "tensor_copy", reads=[E.psb[b]], writes=[Bl], out=nl[:], in_=E.ps[:, b, 0:1])
            P.op("dve", "tensor_scalar", reads=[Bl], writes=[Bl], out=gsc[:], in0=gsc[:], scalar1=(1.0 - lambda_init), scalar2=None, op0=ALU.mult)

            with phase() as ph:
                NKS = 3
                kt = [E.sb("kt", [65, NKEY], BF16, ph) for i in range(NKS)]
                vt = [E.sb("vt", [128, NKB, 128], BF16, ph) for i in range(2)]
                qt = [E.sb("qt", [65, 2, NQ], BF16, ph) for i in range(1)]
                Bk = [Buf("kt%d" % i) for i in range(NKS)]
                Bv = [Buf("vt%d" % i) for i in range(2)]
                Bq = [Buf("qt%d" % i) for i in range(1)]
                Baug = Buf("aug")
                for i in range(NKS):
                    P.op("pool", "memset", writes=[Baug], ap=kt[i][64:65, :], constant=1.0)
                for i in range(1):
                    P.op("pool", "memset", writes=[Baug], ap=qt[i][64:65, :, :], constant=0.0)
                accD = [E.sb("accD", [128, 512], F32, ph) for i in range(2)]
                accP = [E.sb("accP", [128, 512], F32, ph) for i in range(2)]
                BaD = [Buf("accD%d" % i) for i in range(2)]
                BaP = [Buf("accP%d" % i) for i in range(2)]
                ones32 = E.sb("ones32", [128, 128], F32, ph)
                Bo32 = Buf("ones32")
                P.op("pool", "memset", writes=[Bo32], ap=ones32[:], constant=1.0)
                pT = [E.sb("pT", [128, 512], BF16, ph) for i in range(4)]
                BpT = [Buf("pT%d" % i) for i in range(4)]
                rr = [E.sb("rr", [128, 512], F32, ph) for i in range(2)]
                Brr = [Buf("rr%d" % i) for i in range(2)]
                osq = E.sb("osq", [128, 512], BF16, ph)
                Bosq = Buf("osq")
                GVv = [sc["GV0"].rearrange("(j p) c -> p j c", p=128), sc["GV1"].rearrange("(j p) c -> p j c", p=128)]
                vbcv = sc["vbc"].rearrange("(j p) c -> p j c", p=128)

                def load_map(m):
                    s = m % NKS
                    P.dma("sp", kt[s][0:64, 0:LC], sc["kbc"][m], reads=[sc["Bkbc"]], writes=[Bk[s]])
                    for r in range(4):
                        P.dma("sp", kt[s][0:64, LC + r * NL:LC + (r + 1) * NL], sc["GB%d" % (m // 4)][r * 256 + (m % 4) * 64:r * 256 + (m % 4 + 1) * 64, :],
                              reads=[sc["BGB%d" % (m // 4)]], writes=[Bk[s]])

                def load_vq(dh):
                    s = dh % 2
                    P.dma("sp", vt[s][:, 0:2, :], vbcv[:, :, dh * 128:(dh + 1) * 128], reads=[sc["Bvbc"]], writes=[Bv[s]])
                    P.dma("sp", vt[s][:, 2:NKB, :], GVv[dh // 2][:, :, (dh % 2) * 128:(dh % 2 + 1) * 128], reads=[sc["BGV%d" % (dh // 2)]], writes=[Bv[s]])

                def load_q(dh):
                    for mi in range(2):
                        P.dma("sp", qt[0][0:64, mi, :], sc["qb"][2 * dh + mi][:, 0:NQ], reads=[sc["Bqb"]], writes=[Bq[0]])

                qtiles = [(t0, w, NKB) for (t0, w) in tiles_of(NL)] + ([(NL, LC, 2)] if with_ctx else [])
                OB = (0, 1)
                ZB = (2, 3)
                SBK = (4, 5, 6, 7)
                load_map(0)
                load_map(1)
                load_vq(0)
                load_q(0)
                for dh in range(4):
                    s = dh % 2
                    if dh + 1 < 4:
                        load_map(2 * dh + 2)
                        load_vq(dh + 1)
                    for qi, (q0, qw, nkb) in enumerate(qtiles):
                        if dh + 1 < 4 and qi == len(qtiles) - 1:
                            pass
                        steps = [(kb, mi) for kb in range(nkb) for mi in range(2)]

                        def emit_S(i):
                            kb, mi = steps[i]
                            sbk = SBK[i % 4]
                            ks = (2 * dh + mi) % NKS
                            P.op("pe", "matmul", reads=[Bk[ks], Bq[0], Baug], writes=[E.psb[sbk]], out=E.ps[:, sbk, 0:qw], lhsT=kt[ks][0:65, kb * 128:(kb + 1) * 128],
                                 rhs=qt[0][0:65, mi, q0:q0 + qw], start=True, stop=True)

                        emit_S(0)
                        emit_S(1)
                        for i, (kb, mi) in enumerate(steps):
                            sbk = SBK[i % 4]
                            pi = i % 4
                            P.op("act", "activation", reads=[E.psb[sbk]], writes=[BpT[pi]], out=pT[pi][:, 0:qw], in_=E.ps[:, sbk, 0:qw], func=AF.Exp, scale=0.125)
                            if i + 2 < len(steps):
                                emit_S(i + 2)
                            first = (kb == 0)
                            last = (kb == nkb - 1)
                            P.op("pe", "matmul", reads=[Bv[s], BpT[pi]], writes=[E.psb[OB[mi]]], out=E.ps[:, OB[mi], 0:qw], lhsT=vt[s][:, kb, :], rhs=pT[pi][:, 0:qw], start=first, stop=last)
                            if kb % 3 != 0:
                                if kb == 1:
                                    P.op("dve", "tensor_copy", reads=[BpT[pi]], writes=[BaD[mi]], out=accD[mi][:, 0:qw], in_=pT[pi][:, 0:qw])
                                else:
                                    P.op("dve", "tensor_tensor", reads=[BpT[pi], BaD[mi]], writes=[BaD[mi]], out=accD[mi][:, 0:qw], in0=accD[mi][:, 0:qw], in1=pT[pi][:, 0:qw], op=ALU.add)
                            else:
                                P.op("pe", "matmul", reads=[Bones, BpT[pi]], writes=[E.psb[ZB[mi]]], out=E.ps[:, ZB[mi], 0:qw], lhsT=ones[:], rhs=pT[pi][:, 0:qw], start=(kb == 0), stop=False)
                        for mi in range(2):
                            P.op("pe", "matmul", reads=[Bo32, BaD[mi]], writes=[E.psb[ZB[mi]]], out=E.ps[:, ZB[mi], 0:qw], lhsT=ones32[:], rhs=accD[mi][:, 0:qw], start=False, stop=True)
                        for mi in range(2):
                            P.op("dve", "reciprocal", reads=[E.psb[ZB[mi]]], writes=[Brr[mi]], out=rr[mi][:, 0:qw], in_=E.ps[:, ZB[mi], 0:qw])
                            P.op("dve", "tensor_tensor", reads=[E.psb[OB[mi]], Brr[mi]], writes=[Brr[mi]], out=rr[mi][:, 0:qw], in0=E.ps[:, OB[mi], 0:qw], in1=rr[mi][:, 0:qw], op=ALU.mult)
                        P.op("dve", "scalar_tensor_tensor", reads=[Brr[0], Brr[1], Bl], writes=[Brr[0]], out=rr[0][:, 0:qw], in0=rr[1][:, 0:qw], scalar=nl[:, 0:1], in1=rr[0][:, 0:qw],
                             op0=ALU.mult, op1=ALU.add)
                        P.op("act", "activation", reads=[Brr[0]], writes=[Bosq], out=osq[:, 0:qw], in_=rr[0][:, 0:qw], func=AF.Square)
                        bz = ZB[0]
                        P.op("pe", "matmul", reads=[Bones, Bosq], writes=[E.psb[bz]], out=E.ps[:, bz, 0:qw], lhsT=ones[:], rhs=osq[:, 0:qw], start=True, stop=True)
                        P.op("act", "activation", reads=[E.psb[bz]], writes=[Brr[1]], out=rr[1][:, 0:qw], in_=E.ps[:, bz, 0:qw], func=AF.Sqrt, bias=EPS, scale=1.0 / 128)
                        P.op("dve", "reciprocal", reads=[Brr[1]], writes=[Brr[1]], out=rr[1][:, 0:qw], in_=rr[1][:, 0:qw])
                        P.op("dve", "scalar_tensor_tensor", reads=[Brr[0], Brr[1], Bl], writes=[Bmixd[dh]], out=mixd[:, dh, q0:q0 + qw], in0=rr[0][:, 0:qw], scalar=gsc[:, 0:1], in1=rr[1][:, 0:qw],
                             op0=ALU.mult, op1=ALU.mult)
                    if 2 * dh + 3 < 8:
                        load_map(2 * dh + 3)
                    if dh + 1 < 4:
                        load_q(dh + 1)
                prog_barrier(P)

            mixn = E.sb("mixn", [64, 4, NQ], BF16, ph0)
            Bmixn = [Buf("mixn%d" % i) for i in range(4)]
            with phase() as ph:
                qa = E.sb("qa", [128, 2, NQ], BF16, ph)
                kvac = E.sb("kvac", [128, 2, 512], BF16, ph)
                kacT = E.sb("kacT", [128, 2, LC], BF16, ph)
                Bna, Bkvac, BkacT = Buf("na_in"), Buf("kvac"), Buf("kacT")
                P.dma("sp", qa[:], sc["qa"][:, :, 0:NQ], reads=[sc["Bqa"]], writes=[Bna])
                P.dma("sp", kvac[:], sc["kvac"].rearrange("(j p) c -> p j c", p=128), reads=[sc["Bkvac"]], writes=[Bkvac])
                bt = E.bank()
                for j in range(2):
                    for hp in range(2):
                        P.op("pe", "transpose", reads=[Bkvac, Bid], writes=[E.psb[bt]], out=ps_bf[:, bt, hp * 256 + j * 128:hp * 256 + (j + 1) * 128], in_=kvac[:, j, hp * 128:(hp + 1) * 128], identity=idb[:])
                P.op("act", "activation", reads=[E.psb[bt]], writes=[BkacT], out=kacT[:].rearrange("p a b -> p (a b)"), in_=ps_bf[:, bt, 0:512], func=AF.Copy)
                kvwin = [E.sb("kwin", [128, 4, 256], BF16, ph) for i in range(2)]
                vwin = [E.sb("vwin", [128, 4, 256], BF16, ph) for i in range(2)]
                kwT = [E.sb("kwT", [128, 2, 512], BF16, ph) for i in range(2)]
                nab = [E.sb("nab", [64, 8, 4, 64], F32, ph) for i in range(2)]
                Bkv = [Buf("kvwin%d" % i) for i in range(2)]
                BkwT = [Buf("kwT%d" % i) for i in range(2)]
                Bnab = [Buf("nab%d" % i) for i in range(2)]
                sbuf_s = [E.sb("nas", [64, 768], F32, ph) for i in range(2)]
                Bs = [Buf("nas%d" % i) for i in range(2)]
                p32 = [E.sb("nap", [64, 768], F32, ph) for i in range(2)]
                Bp32 = [Buf("nap%d" % i) for i in range(2)]
                pn = [E.sb("napn", [64, 768], BF16, ph) for i in range(2)]
                Bpn = [Buf("napn%d" % i) for i in range(2)]
                ptsb = [E.sb("napt", [128, 384], BF16, ph) for i in range(2)]
                Bpt = [Buf("napt%d" % i) for i in range(2)]
                st4 = [E.sb("nast", [64, 4], F32, ph) for i in range(2)]
                Bst4 = [Buf("nast%d" % i) for i in range(2)]
                GAK, GAV = sc["GAK"], sc["GAV"]
                idxt = E.sb("idxt", [128, 64], I32, ph)
                Bidx = Buf("idxt")
                P.dma("sp", idxt[:], idxtab_d, writes=[Bidx])

                def load_row(rl):
                    s = rl % 2
                    P.custom("pool", (lambda eng, rl=rl, s=s: eng.indirect_dma_start(
                        out=kvwin[s][:].rearrange("p a b -> p (a b)"), out_offset=None, in_=GAK[:, :],
                        in_offset=bass.IndirectOffsetOnAxis(ap=idxt[:, rl:rl + 1], axis=0))), reads=[sc["BGAK"], Bidx], writes=[Bkv[s]], dma=True)
                    P.custom("pool", (lambda eng, rl=rl, s=s: eng.indirect_dma_start(
                        out=vwin[s][:].rearrange("p a b -> p (a b)"), out_offset=None, in_=GAV[:, :],
                        in_offset=bass.IndirectOffsetOnAxis(ap=idxt[:, rl:rl + 1], axis=0))), reads=[sc["BGAV"], Bidx], writes=[Bkv[s]], dma=True)
                    P.custom("pool", (lambda eng, rl=rl, s=s: eng.indirect_dma_start(
                        out=nab[s][:].rearrange("p a b c -> p (a b c)"), out_offset=None, in_=bc_d[l][:, :],
                        in_offset=bass.IndirectOffsetOnAxis(ap=idxt[0:64, 32 + rl:32 + rl + 1], axis=0))), reads=[Bidx], writes=[Bnab[s]], dma=True)
                    bt = E.bank()
                    for j in range(4):
                        for hp in range(2):
                            P.op("pe", "transpose", reads=[Bkv[s], Bid], writes=[E.psb[bt]], out=ps_bf[:, bt, hp * 512 + j * 128:hp * 512 + (j + 1) * 128], in_=kvwin[s][:, j, hp * 128:(hp + 1) * 128], identity=idb[:])
                    P.op("act", "activation", reads=[E.psb[bt]], writes=[BkwT[s]], out=kwT[s][:].rearrange("p a b -> p (a b)"), in_=ps_bf[:, bt, 0:1024], func=AF.Copy)

                it = [0]

                def na_block(q0, h, rl):
                    i = it[0] % 2
                    it[0] += 1
                    hp, hb = h // 2, (h % 2) * 64
                    nk = 768 if rl is not None else 256
                    lhs = qa[hb:hb + 64, hp, q0:q0 + 64]
                    if rl is not None:
                        s = rl % 2
                        ba = E.bank()
                        P.op("pe", "matmul", reads=[Bna, BkwT[s]], writes=[E.psb[ba]], out=E.ps[0:64, ba, 0:512], lhsT=lhs, rhs=kwT[s][hb:hb + 64, hp, :], start=True, stop=True)
                        for jj in range(4):
                            P.op("dve", "scalar_tensor_tensor", reads=[E.psb[ba], Bnab[s]], writes=[Bs[i]],
                                 out=sbuf_s[i][:, jj * 128:(jj + 1) * 128].rearrange("p (a m) -> p a m", a=8),
                                 in0=E.ps[0:64, ba, jj * 128:(jj + 1) * 128].rearrange("p (a m) -> p a m", a=8), scalar=0.125,
                                 in1=nab[s][:, :, h, jj:64:4], op0=ALU.mult, op1=ALU.add)
                        c0 = 512
                    else:
                        c0 = 0
                    bb = E.bank()
                    P.op("pe", "matmul", reads=[Bna, BkacT], writes=[E.psb[bb]], out=E.ps[0:64, bb, 0:256], lhsT=lhs, rhs=kacT[hb:hb + 64, hp, :], start=True, stop=True)
                    P.op("act", "activation", reads=[E.psb[bb]], writes=[Bs[i]], out=sbuf_s[i][:, c0:c0 + 256], in_=E.ps[0:64, bb, 0:256], func=AF.Copy, scale=0.125)
                    P.op("dve", "reduce_max", reads=[Bs[i]], writes=[Bst4[i]], out=st4[i][:, 0:1], in_=sbuf_s[i][:, 0:nk], axis=AX.X)
                    P.op("dve", "tensor_scalar", reads=[Bst4[i]], writes=[Bst4[i]], out=st4[i][:, 1:2], in0=st4[i][:, 0:1], scalar1=-1.0, scalar2=None, op0=ALU.mult)
                    P.op("act", "activation", reads=[Bs[i], Bst4[i]], writes=[Bp32[i]], out=p32[i][:, 0:nk], in_=sbuf_s[i][:, 0:nk], func=AF.Exp, bias=st4[i][:, 1:2], scale=1.0)
                    P.op("dve", "reduce_sum", reads=[Bp32[i]], writes=[Bst4[i]], out=st4[i][:, 2:3], in_=p32[i][:, 0:nk], axis=AX.X)
                    P.op("dve", "reciprocal", reads=[Bst4[i]], writes=[Bst4[i]], out=st4[i][:, 3:4], in_=st4[i][:, 2:3])
                    P.op("dve", "tensor_scalar", reads=[Bp32[i], Bst4[i]], writes=[Bpn[i]], out=pn[i][:, 0:nk], in0=p32[i][:, 0:nk], scalar1=st4[i][:, 3:4], scalar2=None, op0=ALU.mult)
                    nb = nk // 128
                    bt = E.bank()
                    for j in range(nb):
                        P.op("pe", "transpose", reads=[Bpn[i], Bid], writes=[E.psb[bt]], out=ps_bf[:, bt, j * 64:(j + 1) * 64], in_=pn[i][:, j * 128:(j + 1) * 128], identity=idb[0:64, 0:64])
                    P.op("act", "activation", reads=[E.psb[bt]], writes=[Bpt[i]], out=ptsb[i][:, 0:nb * 64], in_=ps_bf[:, bt, 0:nb * 64], func=AF.Copy)
                    bo = E.bank()
                    for j in range(nb):
                        if rl is not None and j < 4:
                            lv = vwin[rl % 2][:, j, h * 64:(h + 1) * 64]
                            rd = [Bkv[rl % 2], Bpt[i]]
                        else:
                            lv = kvac[:, j - (4 if rl is not None else 0), 256 + h * 64:256 + (h + 1) * 64]
                            rd = [Bkvac, Bpt[i]]
                        P.op("pe", "matmul", reads=rd, writes=[E.psb[bo]], out=E.ps[0:64, bo, 0:64], lhsT=lv, rhs=ptsb[i][:, j * 64:(j + 1) * 64], start=(j == 0), stop=(j == nb - 1))
                    P.op("dve", "tensor_copy", reads=[E.psb[bo]], writes=[Bmixn[h]], out=mixn[0:64, h, q0:q0 + 64], in_=E.ps[0:64, bo, 0:64])

                load_row(0)
                for rl in range(32):
                    if rl + 1 < 32:
                        load_row(rl + 1)
                    for h in range(4):
                        na_block(rl * 64, h, rl)
                if with_ctx:
                    for qb in range(4):
                        for h in range(4):
                            na_block(NL + qb * 64, h, None)
                prog_barrier(P)

            with phase() as ph:
                mixc = E.sb("mixc", [128, 2, NQ], BF16, ph)
                Bmixc = [Buf("mixc%d" % i) for i in range(2)]
                cg = E.sb("cgx", [128, 2, NT + 4], F32, ph)
                bg = E.sb("bg", [128, 2, NT], F32, ph)
                cw = E.sb("cw", [128, 2, 3], F32, ph)
                ca = E.sb("ca", [128, NL], F32, ph)
                eg = E.sb("eg", [128, 4, 2, 2], BF16, ph)
                selh = E.sb("selh", [128, 8], F32, ph)
                egt = E.sb("egt", [128, 4], F32, ph)
                Bcv, Bca, Beg = Buf("conv_in"), Buf("ca"), Buf("eg")
                P.op("pool", "memset", writes=[Bcv], ap=cg[:], constant=0.0)
                P.dma("sp", cg[:, :, 1:NL + 1], sc["cgx"][:, :, 0:NL], reads=[sc["Bcgx"]], writes=[Bcv])
                P.dma("sp", cg[:, :, NL + 3:NL + 3 + LC], sc["cgx"][:, :, NL:NT], reads=[sc["Bcgx"]], writes=[Bcv])
                P.dma("sp", bg[:], sc["bg"], reads=[sc["Bbg"]], writes=[Bcv])
                P.dma("sp", cw[:], convw_d[l], writes=[Bcv])
                P.dma("sp", selh[:], selh_d, writes=[Beg])
                for r in range(4):
                    P.dma("sp", eg[:, r, :, :], sc["GE"][r * 4, 0:512].rearrange("(c p j) -> p c j", c=2, p=128, j=2), reads=[sc["BGE"]], writes=[Beg],
                          allow_slow_non_contiguous=True)
                for c in range(2):
                    P.op("dve", "tensor_tensor", reads=[Beg], writes=[Beg], out=egt[:], in0=eg[:, :, c, 1], in1=selh[:, 0:4], op=ALU.mult)
                    P.op("dve", "reduce_sum", reads=[Beg, Bcv], writes=[Bcv], out=cg[:, c, 0:1], in_=egt[:], axis=AX.X)
                    P.op("dve", "tensor_tensor", reads=[Beg], writes=[Beg], out=egt[:], in0=eg[:, :, c, 0], in1=selh[:, 4:8], op=ALU.mult)
                    P.op("dve", "reduce_sum", reads=[Beg, Bcv], writes=[Bcv], out=cg[:, c, NL + 1:NL + 2], in_=egt[:], axis=AX.X)
                segs = [(0, 0, NL)] + ([(NL + 2, NL, LC)] if with_ctx else [])
                for c in range(2):
                    for (u0, o0, n) in segs:
                        P.op("dve", "tensor_scalar", reads=[Bcv], writes=[Bca], out=ca[:, 0:n], in0=cg[:, c, u0 + 1:u0 + 1 + n], scalar1=cw[:, c, 1:2], scalar2=None, op0=ALU.mult)
                        P.op("dve", "scalar_tensor_tensor", reads=[Bcv, Bca], writes=[Bca], out=ca[:, 0:n], in0=cg[:, c, u0:u0 + n], scalar=cw[:, c, 0:1], in1=ca[:, 0:n],
                             op0=ALU.mult, op1=ALU.add)
                        P.op("dve", "scalar_tensor_tensor", reads=[Bcv, Bca], writes=[Bca], out=ca[:, 0:n], in0=cg[:, c, u0 + 2:u0 + 2 + n], scalar=cw[:, c, 2:3], in1=ca[:, 0:n],
                             op0=ALU.mult, op1=ALU.add)
                        P.op("dve", "tensor_tensor", reads=[Bcv, Bca], writes=[Bmixc[c]], out=mixc[:, c, o0:o0 + n], in0=ca[:, 0:n], in1=bg[:, c, o0:o0 + n], op=ALU.mult)
                wna = E.sb("wna", [64, 4, D], BF16, ph)
                wr = E.sb("wr", [128, 6, D], BF16, ph)
                Bw = Buf("wout")
                P.dma("pool", wna[:], wna_d[l], writes=[Bw])
                P.dma("pool", wr[:], wor_d[l], writes=[Bw])
                for ti, (t0, w) in enumerate(tiles_of(NQ)):
                    j = 0 if t0 < NL else 1
                    for m in range(KC):
                        b = E.bank()
                        for kc in range(10):
                            if kc < 4:
                                lh, rh, Br = wna[:, kc, m * 128:(m + 1) * 128], mixn[0:64, kc, t0:t0 + w], Bmixn[kc]
                            elif kc < 8:
                                lh, rh, Br = wr[:, kc - 4, m * 128:(m + 1) * 128], mixd[:, kc - 4, t0:t0 + w], Bmixd[kc - 4]
                            else:
                                lh, rh, Br = wr[:, kc - 4, m * 128:(m + 1) * 128], mixc[:, kc - 8, t0:t0 + w], Bmixc[kc - 8]
                            P.op("pe", "matmul", reads=[Bw, Br], writes=[E.psb[b]], out=E.ps[:, b, 0:w], lhsT=lh, rhs=rh, start=(kc == 0), stop=(kc == 9))
                        P.op("dve", "scalar_tensor_tensor", reads=[E.psb[b], Bmod, Bx[ti]], writes=[Bx[ti]], out=x[:, m, t0:t0 + w], in0=E.ps[:, b, 0:w], scalar=mo[:, l, 16 + m, j:j + 1],
                             in1=x[:, m, t0:t0 + w], op0=ALU.mult, op1=ALU.add)
                prog_barrier(P)

    def stage_L3(l, NTK, moe, final):
        G = 2
        F = D_EXP if moe else D_FF
        NE = NEXP if moe else 1
        w1, w3, w2 = (mw1_d, mw3_d, mw2_d) if moe else (fw1_d, fw3_d, fw2_d)
        tlk = tiles_of(NTK)
        with phase() as ph0:
            hT = E.sb("hT3", [128, KC, NTK], BF16, ph0)
            Bh = [Buf("h%d" % i) for i in range(len(tlk))]
            if moe:
                gatesT = E.sb("gatesT", [8, NTK], F32, ph0)
                Bgt = Buf("gatesT")
                sel = E.sb("sel", [8, 8, 128], F32, ph0)
                Bc = Buf("moeconst")
                P.dma("sp", sel[:], sel_d, writes=[Bc])
            with phase() as ph:
                scr = norm_scr(ph)
                if moe:
                    h32 = E.sb("h32", [128, KC, 512], F32, ph)
                    Bh32 = Buf("h32")
                    wr = E.sb("wrt", [128, KC, NEXP], F32, ph)
                    ident = E.sb("ident", [128, 128], F32, ph)
                    P.dma("sp", wr[:], wrt_d, writes=[Bc])
                    P.dma("sp", ident[:], ident_d, writes=[Bc])
                    lg = E.sb("lg", [128, 8], F32, ph)
                    l2 = E.sb("l2", [128, 8], F32, ph)
                    ew = E.sb("ew", [128, 8], F32, ph)
                    sm = E.sb("sm", [128, 8], F32, ph)
                    Bg = Buf("gate_scr")
                for ti, (t0, w) in enumerate(tlk):
                    j = 0 if t0 < NL else 1
                    emit_norm_mod_tile(E, ones, Bones, x[:, :, t0:t0 + w], Bx[ti], w, mo[:, l, 24:32, j], sc1[:, l, 1, :, j], Bmod,
                                       hT[:, :, t0:t0 + w], Bh[ti], scr,
                                       h32dst=(h32[:, :, 0:w] if moe else None), Bh32=(Bh32 if moe else None))
                    if moe:
                        for blk in range(w // 128):
                            b = E.bank()
                            for k in range(KC):
                                P.op("pe", "matmul", reads=[Bh32, Bc], writes=[E.psb[b]], out=E.ps[:, b, 0:8], lhsT=h32[:, k, blk * 128:(blk + 1) * 128], rhs=wr[:, k, :],
                                     start=(k == 0), stop=(k == KC - 1))
                            P.op("dve", "tensor_copy", reads=[E.psb[b]], writes=[Bg], out=lg[:], in_=E.ps[:, b, 0:8])
                            P.op("dve", "reduce_max", reads=[Bg], writes=[Bg], out=sm[:, 0:1], in_=lg[:], axis=AX.X)
                            P.op("dve", "tensor_scalar", reads=[Bg], writes=[Bg], out=sm[:, 1:2], in0=sm[:, 0:1], scalar1=-1.0, scalar2=None, op0=ALU.mult)
                            P.op("dve", "tensor_scalar", reads=[Bg], writes=[Bg], out=l2[:], in0=lg[:], scalar1=sm[:, 0:1], scalar2=-1e30, op0=ALU.is_equal, op1=ALU.mult)
                            P.op("dve", "tensor_tensor", reads=[Bg], writes=[Bg], out=l2[:], in0=l2[:], in1=lg[:], op=ALU.add)
                            P.op("dve", "reduce_max", reads=[Bg], writes=[Bg], out=sm[:, 2:3], in_=l2[:], axis=AX.X)
                            P.op("dve", "tensor_scalar", reads=[Bg], writes=[Bg], out=l2[:], in0=lg[:], scalar1=sm[:, 2:3], scalar2=None, op0=ALU.is_ge)
                            P.op("act", "activation", reads=[Bg], writes=[Bg], out=ew[:], in_=lg[:], func=AF.Exp, bias=sm[:, 1:2], scale=1.0)
                            P.op("dve", "tensor_tensor", reads=[Bg], writes=[Bg], out=ew[:], in0=ew[:], in1=l2[:], op=ALU.mult)
                            P.op("dve", "reduce_sum", reads=[Bg], writes=[Bg], out=sm[:, 3:4], in_=ew[:], axis=AX.X)
                            P.op("dve", "reciprocal", reads=[Bg], writes=[Bg], out=sm[:, 4:5], in_=sm[:, 3:4])
                            P.op("dve", "tensor_scalar", reads=[Bg], writes=[Bg], out=ew[:], in0=ew[:], scalar1=sm[:, 4:5], scalar2=None, op0=ALU.mult)
                            b2 = E.bank()
                            P.op("pe", "transpose", reads=[Bg, Bc], writes=[E.psb[b2]], out=E.ps[0:8, b2, 0:128], in_=ew[:], identity=ident[:])
                            c0 = t0 + blk * 128
                            P.op("dve", "tensor_copy", reads=[E.psb[b2]], writes=[Bgt], out=gatesT[:, c0:c0 + 128], in_=E.ps[0:8, b2, 0:128])
                prog_barrier(P)
            with phase() as ph:
                NS = 3
                w1s = [E.sb("w1s", [128, KC, 128 * G], BF16, ph) for i in range(NS)]
                w3s = [E.sb("w3s", [128, KC, 128 * G], BF16, ph) for i in range(NS)]
                w2s = [E.sb("w2s", [128, G, D], BF16, ph) for i in range(NS)]
                Bws = [Buf("ws%d" % i) for i in range(NS)]
                ut = [E.sb("ut", [128, G, 512], BF16, ph) for i in range(2)]
                But = [Buf("ut%d" % i) for i in range(2)]
                st_ = [E.sb("st", [128, 512], F32, ph) for i in range(4)]
                Bst = [Buf("st%d" % i) for i in range(4)]
                tt_ = [E.sb("tt", [128, 512], F32, ph) for i in range(4)]
                Btt = [Buf("tt%d" % i) for i in range(4)]
                if moe:
                    gbc = [E.sb("gbc", [128, NTK], F32, ph) for i in range(2)]
                    Bgbc = [Buf("gbc%d" % i) for i in range(2)]
                ngrp = F // (128 * G)
                groups = [(ex, g) for ex in range(NE) for g in range(ngrp)]

                def load_group(gi):
                    ex, g = groups[gi]
                    s = gi % NS
                    P.dma("pool", w1s[s][:], w1[ex, :, :, g * 128 * G:(g + 1) * 128 * G], writes=[Bws[s]])
                    P.dma("pool", w3s[s][:], w3[ex, :, :, g * 128 * G:(g + 1) * 128 * G], writes=[Bws[s]])
                    P.dma("pool", w2s[s][:], w2[ex, :, g * G:(g + 1) * G, :], writes=[Bws[s]])

                load_group(0)
                if len(groups) > 1:
                    load_group(1)
                items = [(gi, ti) for gi in range(len(groups)) for ti in range(len(tlk))]
                dn_rr = [0]

                def up(ii):
                    gi, ti = items[ii]
                    ex, g = groups[gi]
                    s = gi % NS
                    t0, w = tlk[ti]
                    ui = ii % 2
                    if ti == 0:
                        if moe and g == 0:
                            gs = ex % 2
                            for tj, (u0, uw) in enumerate(tlk):
                                b_ = 4 + dn_rr[0] % 4
                                dn_rr[0] += 1
                                P.op("pe", "matmul", reads=[Bc, Bgt], writes=[E.psb[b_]], out=E.ps[:, b_, 0:uw], lhsT=sel[:, ex, :], rhs=gatesT[:, u0:u0 + uw], start=True, stop=True)
                                P.op("act", "activation", reads=[E.psb[b_]], writes=[Bgbc[gs]], out=gbc[gs][:, u0:u0 + uw], in_=E.ps[:, b_, 0:uw], func=AF.Copy)
                    for fc in range(G):
                        ba, bb = 2 * fc, 2 * fc + 1
                        for k in range(KC):
                            P.op("pe", "matmul", reads=[Bws[s], Bh[ti]], writes=[E.psb[ba]], out=E.ps[:, ba, 0:w], lhsT=w1s[s][:, k, fc * 128:(fc + 1) * 128],
                                 rhs=hT[:, k, t0:t0 + w], start=(k == 0), stop=(k == KC - 1))
                        for k in range(KC):
                            P.op("pe", "matmul", reads=[Bws[s], Bh[ti]], writes=[E.psb[bb]], out=E.ps[:, bb, 0:w], lhsT=w3s[s][:, k, fc * 128:(fc + 1) * 128],
                                 rhs=hT[:, k, t0:t0 + w], start=(k == 0), stop=(k == KC - 1))
                        si = (ii % 2) * 2 + fc
                        P.op("act", "activation", reads=[E.psb[ba]], writes=[Bst[si]], out=st_[si][:, 0:w], in_=E.ps[:, ba, 0:w], func=AF.Silu)
                        if moe:
                            gs = ex % 2
                            P.op("dve", "tensor_tensor", reads=[E.psb[bb], Bgbc[gs]], writes=[Btt[si]], out=tt_[si][:, 0:w], in0=E.ps[:, bb, 0:w], in1=gbc[gs][:, t0:t0 + w], op=ALU.mult)
                            P.op("dve", "tensor_tensor", reads=[Btt[si], Bst[si]], writes=[But[ui]], out=ut[ui][:, fc, 0:w], in0=tt_[si][:, 0:w], in1=st_[si][:, 0:w], op=ALU.mult)
                        else:
                            P.op("dve", "tensor_tensor", reads=[E.psb[bb], Bst[si]], writes=[But[ui]], out=ut[ui][:, fc, 0:w], in0=E.ps[:, bb, 0:w], in1=st_[si][:, 0:w], op=ALU.mult)

                def down(ii):
                    gi, ti = items[ii]
                    s = gi % NS
                    t0, w = tlk[ti]
                    ui = ii % 2
                    j = 0 if t0 < NL else 1
                    for m in range(KC):
                        bc = 4 + dn_rr[0] % 4
                        dn_rr[0] += 1
                        for fc in range(G):
                            P.op("pe", "matmul", reads=[Bws[s], But[ui]], writes=[E.psb[bc]], out=E.ps[:, bc, 0:w], lhsT=w2s[s][:, fc, m * 128:(m + 1) * 128],
                                 rhs=ut[ui][:, fc, 0:w], start=(fc == 0), stop=(fc == G - 1))
                        P.op("dve", "scalar_tensor_tensor", reads=[E.psb[bc], Bmod, Bx[ti]], writes=[Bx[ti]], out=x[:, m, t0:t0 + w], in0=E.ps[:, bc, 0:w], scalar=mo[:, l, 40 + m, j:j + 1],
                             in1=x[:, m, t0:t0 + w], op0=ALU.mult, op1=ALU.add)
                    if ti == len(tlk) - 1 and gi + 3 < len(groups):
                        load_group(gi + 3)

                if len(groups) > 2:
                    load_group(2)
                up(0)
                for ii in range(len(items)):
                    if ii + 1 < len(items):
                        up(ii + 1)
                    down(ii)
                prog_barrier(P)
        if final:
            with phase() as ph:
                fg = E.sb("fg", [128, KC], F32, ph)
                sq = E.sb("fsq", [128, KC, 512], BF16, ph)
                rstd = E.sb("frstd", [128, 512], F32, ph)
                Bfg, Bsq, Brstd = Buf("fg"), Buf("fsq"), Buf("frstd")
                P.dma("sp", fg[:], fg_d, writes=[Bfg])
                for ti, (t0, w) in enumerate(tlk):
                    P.op("act", "activation", reads=[Bx[ti]], writes=[Bsq], out=sq[:, :, 0:w], in_=x[:, :, t0:t0 + w], func=AF.Square)
                    b = E.bank()
                    for k in range(KC):
                        P.op("pe", "matmul", reads=[Bones, Bsq], writes=[E.psb[b]], out=E.ps[:, b, 0:w], lhsT=ones[:], rhs=sq[:, k, 0:w], start=(k == 0), stop=(k == KC - 1))
                    P.op("act", "activation", reads=[E.psb[b]], writes=[Brstd], out=rstd[:, 0:w], in_=E.ps[:, b, 0:w], func=AF.Sqrt, bias=EPS, scale=1.0 / D)
                    P.op("dve", "reciprocal", reads=[Brstd], writes=[Brstd], out=rstd[:, 0:w], in_=rstd[:, 0:w])
                    for k in range(KC):
                        P.op("dve", "scalar_tensor_tensor", reads=[Bx[ti], Brstd, Bfg], writes=[Bx[ti]], out=x[:, k, t0:t0 + w], in0=x[:, k, t0:t0 + w], scalar=fg[:, k:k + 1], in1=rstd[:, 0:w],
                             op0=ALU.mult, op1=ALU.mult)
                    P.dma("sp", xo_d[:, :, t0:t0 + w], x[:, :, t0:t0 + w], reads=[Bx[ti]], is_output=True)

    stages = [lambda: stage_L1(0), lambda: stage_L2(0, True), lambda: stage_L3(0, NT, False, False),
              lambda: stage_L1(1), lambda: stage_L2(1, False), lambda: stage_L3(1, NL, True, True)]
    for i in range(nstages):
        stages[i]()
    if nstages < 6:
        for ti, (t0, w) in enumerate(tiles_of(NL)):
            P.dma("sp", xo_d[:, :, t0:t0 + w], x[:, :, t0:t0 + w], reads=[Bx[ti]], is_output=True)
    return E.done()


def fm(a):
    T = a.shape[0]
    return np.ascontiguousarray(a.T.reshape(-1, 128, T).transpose(1, 0, 2))


def unfm(a):
    return np.ascontiguousarray(a.transpose(1, 0, 2).reshape(-1, a.shape[2]).T)


def wl(w):
    K, C = w.shape
    return np.ascontiguousarray(w.reshape(K // 128, 128, C).transpose(1, 0, 2))


def rope_tables(q):
    t = np.arange(q * NL, (q + 1) * NL, dtype=np.int32)
    row = (t // GRID_W).astype(np.float32)
    col = (t % GRID_W).astype(np.float32)
    inv = (np.float32(10000.0) ** (-np.arange(16, dtype=np.float32) / np.float32(16))).astype(np.float32)
    ang = np.concatenate([row[:, None] * inv, col[:, None] * inv], axis=-1).astype(np.float32)
    c = np.cos(ang).astype(np.float32).T
    s = np.sin(ang).astype(np.float32).T
    return np.ascontiguousarray(np.concatenate([c, c], 0)), np.ascontiguousarray(np.concatenate([-s, s], 0))


def rot_cols(w):
    return np.ascontiguousarray(w.reshape(w.shape[0], -1, 2, 32)[:, :, ::-1, :].reshape(w.shape[0], -1))


def make_wall(W):
    qB, kB = W[:, 256:768], W[:, 1024:1536]
    return wl(np.concatenate([W[:, 768:1024], W[:, 1536:1792], kB, rot_cols(kB), W[:, 1792:2304], qB, rot_cols(qB),
                              W[:, 0:256], W[:, 2560:2816], W[:, 2304:2560], W[:, 2816:3072]], axis=1))


def na_bias_table(rpb):
    qc = np.arange(64)
    kc = np.arange(64)
    win_start = np.clip(qc - 8, 0, 48)
    valid = (kc[None, :] >= win_start[:, None]) & (kc[None, :] < win_start[:, None] + 16)
    dcol = np.clip(kc[None, :] - qc[:, None] + 15, 0, 30)
    t = rpb[:, :, dcol]
    t = np.where(valid[None, None], t, np.float32(-1e30)).astype(np.float32)
    return np.ascontiguousarray(t.transpose(2, 1, 0, 3).reshape(64 * 15, 256))


def build_in_maps(inp, nstages=6):
    adaw = np.ascontiguousarray(inp["ada_w"].reshape(2, 8, 128, 6144).transpose(0, 2, 1, 3))
    adab = np.ascontiguousarray(inp["ada_b"].reshape(2, 48, 128).transpose(0, 2, 1))
    wall = np.stack([make_wall(inp["w_in"][l]) for l in range(2)])
    nabc = np.stack([na_bias_table(inp["na_rpb"][l]) for l in range(2)])
    convw = np.stack([np.ascontiguousarray(inp["conv_w"][l].T.reshape(2, 128, 3).transpose(1, 0, 2)) for l in range(2)])
    wna = np.stack([np.ascontiguousarray(inp["w_out"][l][0:256].reshape(4, 64, D).transpose(1, 0, 2)) for l in range(2)])
    wor = np.stack([wl(inp["w_out"][l][256:]) for l in range(2)])
    dlam = np.ascontiguousarray(inp["diff_lambda"].reshape(2, 1, 256))
    subln = np.ascontiguousarray(inp["diff_subln"].reshape(2, 128, 1))
    identb = np.eye(128, dtype=np.float32).astype(NPBF)
    fw1 = wl(inp["ffn_w1"][0])[None]
    fw3 = wl(inp["ffn_w3"][0])[None]
    fw2 = wl(inp["ffn_w2"][0])[None]
    NEI = NEXP if nstages >= 6 else 1
    mw1 = np.stack([wl(inp["moe_w1"][0, e]) for e in range(NEI)])
    mw3 = np.stack([wl(inp["moe_w3"][0, e]) for e in range(NEI)])
    mw2 = np.stack([wl(inp["moe_w2"][0, e]) for e in range(NEI)])
    wrt = wl(inp["router_w"][0])
    sel = np.zeros((8, 8, 128), np.float32)
    for e in range(8):
        sel[e, e, :] = 1.0
    ident = np.eye(128, dtype=np.float32)
    fg = np.ascontiguousarray(inp["final_gain"].reshape(8, 128).T)
    ropes = [rope_tables(q) for q in range(4)]
    maps = []
    for core in range(NCORES):
        b, q = core // 4, core % 4
        cT = np.ascontiguousarray(np.stack([inp["c"][b], inp["c_ctx"]], -1).reshape(8, 128, 2).transpose(1, 0, 2))
        r = 32 * q + np.arange(32)
        rs_ = np.clip(r - 4, 0, 120)
        idxtab = np.zeros((128, 64), np.int32)
        pp = np.arange(128)
        for rl in range(32):
            idxtab[:, rl] = rs_[rl] * 64 + 4 * pp
            idxtab[:, 32 + rl] = (pp % 64) * 15 + (rs_[rl] - r[rl] + 7)
        selh = np.zeros((128, 8), np.float32)
        if q > 0:
            selh[:, q - 1] = 1.0
        if q < 3:
            selh[:, 4 + q + 1] = 1.0
        maps.append({
            "xT": fm(np.concatenate([inp["x"][b, q * NL:(q + 1) * NL], inp["ctx"][b]], 0)), "cT": cT, "adaw": adaw, "adab": adab, "wall": wall,
            "ropeC": ropes[q][0], "ropeS": ropes[q][1], "nabc0": nabc[0], "nabc1": nabc[1], "idxtab": idxtab, "selh": selh, "convw": convw, "wout_na": wna, "wout_r": wor,
            "dlam": dlam, "subln": subln, "identb": identb, "fw1": fw1, "fw3": fw3, "fw2": fw2, "mw1": mw1, "mw3": mw3, "mw2": mw2, "wrt": wrt,
            "sel": sel, "ident": ident, "fgain": fg,
        })
    return maps


_NC = []


def kernel(x, c, ctx, c_ctx, ada_w, ada_b, w_in, w_out, na_rpb, diff_lambda, diff_subln, conv_w,
           ffn_w1, ffn_w3, ffn_w2, router_w, moe_w1, moe_w3, moe_w2, final_gain):
    inp = {k: np.asarray(v) for k, v in dict(
        x=x, c=c, ctx=ctx, c_ctx=c_ctx, ada_w=ada_w, ada_b=ada_b, w_in=w_in, w_out=w_out, na_rpb=na_rpb,
        diff_lambda=diff_lambda, diff_subln=diff_subln, conv_w=conv_w, ffn_w1=ffn_w1, ffn_w3=ffn_w3, ffn_w2=ffn_w2,
        router_w=router_w, moe_w1=moe_w1, moe_w3=moe_w3, moe_w2=moe_w2, final_gain=final_gain).items()}
    import os
    nst = int(os.environ.get("KF_NSTAGES", "6"))
    if not _NC:
        _NC.append(build_fused(nst))
    res = run_bass_kernel_spmd(_NC[0], build_in_maps(inp, nst), core_ids=list(range(NCORES)))
    out = np.zeros((2, 4 * NL, D), np.float32)
    for core in range(NCORES):
        b, q = core // 4, core % 4
        out[b, q * NL:(q + 1) * NL] = unfm(np.asarray(res.results[core]["xo"]))
    return out
```
